# Optimizing a Trainium2 kernel written in Bass

```python
import math
import jax
import jax.numpy as jnp
from jax import lax
import numpy as np

D_MODEL = 1024
BATCH = 4
SEQ = 8192
DEPTH = 1

CTX_LEN = 256
GRID_W = 64

HY_WIDTH = 512
HY_BANDS = 16
HY_EMB = 2 * HY_BANDS + 1
HY_FILTER_ORDER = 64
HY_DECAY_TARGET = 1e-2
HY_DECAY_SHORT = 0.3
HY_DECAY_LONG = 1.5

RW_HEADS = 8
RW_HEAD = 64
RW_WIDTH = RW_HEADS * RW_HEAD
LORA_W = 64
LORA_A = 64
LORA_G = 128
RW_LN_EPS = 64e-5
SCAN_DIRECTIONS = (False, True)

HY_COLS = 3 * HY_WIDTH
RW_COLS = 3 * RW_WIDTH + LORA_W + LORA_A + LORA_G
GATE_COLS = 2 * D_MODEL
IN_COLS = HY_COLS + RW_COLS + GATE_COLS
RW_SPLITS = (RW_WIDTH, 2 * RW_WIDTH, 3 * RW_WIDTH, 3 * RW_WIDTH + LORA_W, 3 * RW_WIDTH + LORA_W + LORA_A)
STATE_LO = RW_WIDTH
STATE_HI = 3 * RW_WIDTH + LORA_W + LORA_A

N_EXPERTS = 16
D_EXPERT = 2048
EC_CAPACITY = 2
NORM_EPS = 1e-6

kernel_name = 'hyena_rwkv7_ec_moe_diffusion_block'


def rmsnorm(x, g):
    x32 = x.astype(jnp.float32)
    y = x32 * lax.rsqrt(jnp.mean(x32 * x32, axis=-1, keepdims=True) + NORM_EPS)
    return y.astype(x.dtype) * g


def modulation(cvec, p):
    return jnp.split(jax.nn.silu(cvec) @ p['mod_w'] + p['mod_b'], 6, axis=-1)


def shift_prev(z):
    return jnp.pad(z, ((0, 0), (1, 0), (0, 0)))[:, :-1]


def shift_next(z):
    return jnp.pad(z, ((0, 0), (0, 1), (0, 0)))[:, 1:]


def centred_mix(z, mu):
    return z + mu[0] * (shift_prev(z) - z) + mu[1] * (shift_next(z) - z)


def heads(z):
    return z.reshape(z.shape[:-1] + (RW_HEADS, RW_HEAD))


def hyena_filter_spectrum(L, p):
    f32 = jnp.float32
    t = jnp.linspace(0.0, 1.0, L, dtype=f32)[:, None]
    ang = (2.0 * math.pi / L) * jnp.arange(L, dtype=f32)[:, None]
    bands = jnp.linspace(1e-4, HY_BANDS - 1, HY_BANDS, dtype=f32)
    feat = jnp.concatenate([t, jnp.cos(bands * ang), -jnp.sin(bands * ang)], axis=-1)
    freq = p['hy_freq'].astype(f32)
    hid = jnp.sin(freq * (feat @ p['hy_ffn_w1'].astype(f32) + p['hy_ffn_b1'].astype(f32)))
    hid = jnp.sin(freq * (hid @ p['hy_ffn_w2'].astype(f32) + p['hy_ffn_b2'].astype(f32)))
    filt = (hid @ p['hy_ffn_w3'].astype(f32)).reshape(L, 2, HY_WIDTH)
    deltas = jnp.abs(jnp.linspace(math.log(HY_DECAY_TARGET) / HY_DECAY_LONG,
                                  math.log(HY_DECAY_TARGET) / HY_DECAY_SHORT, HY_WIDTH, dtype=f32))
    filt = filt * jnp.exp(-t[:, :, None] * deltas)
    circ = jnp.concatenate([filt[:, 0], jnp.zeros((1, HY_WIDTH), f32), filt[:0:-1, 1]], axis=0)
    circ = circ / jnp.sum(jnp.abs(circ), axis=0, keepdims=True)
    return jnp.fft.rfft(circ, axis=0)


def long_conv(z, spec):
    L = z.shape[1]
    zf = jnp.fft.rfft(z.astype(jnp.float32), n=2 * L, axis=1)
    return jnp.fft.irfft(zf * spec, n=2 * L, axis=1)[:, :L].astype(z.dtype)


def hyena_branch(cols, p):
    w = p['hy_conv_w']
    u = w[0] * shift_prev(cols) + w[1] * cols + w[2] * shift_next(cols) + p['hy_conv_b']
    x0, x1, v = jnp.split(u, 3, axis=-1)
    z = v * x1
    z = long_conv(z, hyena_filter_spectrum(cols.shape[1], p)) + p['hy_bias'] * z
    return z * x0


def rwkv_direction(k, lw, la, p, d):
    w = -jax.nn.softplus(-(p['rw_w0'][d] + jnp.tanh(lw) @ p['rw_w_up'][d])) - 0.5
    decay = jnp.exp(-jnp.exp(w.astype(jnp.float32)))
    a = jax.nn.sigmoid(p['rw_a0'][d] + la @ p['rw_a_up'][d])
    kk = heads(k * p['rw_k_k']).astype(jnp.float32)
    kk = kk * lax.rsqrt(jnp.maximum(jnp.sum(kk * kk, axis=-1, keepdims=True), 1e-24))
    k_mod = heads(k * (1.0 + (a - 1.0) * p['rw_k_a']))
    return heads(decay), k_mod, -kk, kk * heads(a).astype(jnp.float32)


def rwkv_scan(decay, k, v, a_vec, b_vec, r, state0, reverse):
    emit = r is not None
    xs = (decay, k, v, a_vec, b_vec) + ((r,) if emit else ())
    xs = tuple(jnp.moveaxis(t.astype(jnp.float32), 1, 0) for t in xs)

    def step(S, inp):
        w_t, k_t, v_t, a_t, b_t = inp[:5]
        sa = jnp.einsum('bhij,bhj->bhi', S, a_t)
        S = S * w_t[:, :, None, :] + sa[..., None] * b_t[:, :, None, :] + v_t[..., None] * k_t[:, :, None, :]
        y = jnp.einsum('bhij,bhj->bhi', S, inp[5]) if emit else None
        return S, y

    S, ys = lax.scan(step, state0, xs, reverse=reverse)
    return S, (jnp.moveaxis(ys, 0, 1) if emit else None)


def head_groupnorm(y, g, b):
    y32 = y.astype(jnp.float32)
    mu = jnp.mean(y32, axis=-1, keepdims=True)
    var = jnp.mean(jnp.square(y32 - mu), axis=-1, keepdims=True)
    yn = (y32 - mu) * lax.rsqrt(var + RW_LN_EPS)
    return yn.reshape(y.shape[:-2] + (RW_WIDTH,)).astype(g.dtype) * g + b


def rwkv_branch(cols, p, states0):
    z = centred_mix(cols, p['rw_mu'])
    r, k, v, lw, la, lg = jnp.split(z, RW_SPLITS, axis=-1)
    r_h, v_h = heads(r), heads(v)
    outs, bonuses, finals = [], [], []
    for d, reverse in enumerate(SCAN_DIRECTIONS):
        decay, k_mod, a_vec, b_vec = rwkv_direction(k, lw, la, p, d)
        S, y_d = rwkv_scan(decay, k_mod, v_h, a_vec, b_vec, r_h, states0[d], reverse)
        outs.append(y_d)
        finals.append(S)
        bonuses.append(jnp.sum(r_h * k_mod * p['rw_r_k'], axis=-1, keepdims=True) * v_h)
    y = head_groupnorm(outs[0] + outs[1], p['rw_ln_g'], p['rw_ln_b'])
    y = y + (bonuses[0] + bonuses[1]).reshape(cols.shape[:-1] + (RW_WIDTH,))
    g = jax.nn.sigmoid(lg) @ p['rw_g_up']
    return y * g, finals


def rwkv_context_states(cols, p, state0):
    z = centred_mix(cols, p['rw_mu'][:, STATE_LO:STATE_HI])
    k, v, lw, la = jnp.split(z, (RW_WIDTH, 2 * RW_WIDTH, 2 * RW_WIDTH + LORA_W), axis=-1)
    v_h = heads(v)
    finals = []
    for d, reverse in enumerate(SCAN_DIRECTIONS):
        decay, k_mod, a_vec, b_vec = rwkv_direction(k, lw, la, p, d)
        S, _ = rwkv_scan(decay, k_mod, v_h, a_vec, b_vec, None, state0, reverse)
        finals.append(S)
    return finals


def token_mixer(h, p, states0):
    cols = h @ p['w_in']
    hy_cols, rw_cols, gate_cols = jnp.split(cols, (HY_COLS, HY_COLS + RW_COLS), axis=-1)
    y_hy = hyena_branch(hy_cols, p) @ p['hy_proj']
    y_rw, finals = rwkv_branch(rw_cols, p, states0)
    g_hy, g_rw = jnp.split(jax.nn.sigmoid(gate_cols), 2, axis=-1)
    mixed = (g_hy * y_hy + g_rw * (y_rw @ p['rw_proj'])) @ p['w_out']
    return mixed, finals


def expert_choice_moe(h, p):
    B, T, D = h.shape
    cap = EC_CAPACITY * T // N_EXPERTS
    affinity = jax.nn.softmax((h @ p['router_w']).astype(jnp.float32), axis=-1)
    gates, idx = lax.top_k(jnp.swapaxes(affinity, 1, 2), cap)
    xe = jax.vmap(lambda hb, ib: hb[ib])(h, idx)
    hid = jax.nn.silu(jnp.einsum('becd,edf->becf', xe, p['exp_w1'])) * jnp.einsum('becd,edf->becf', xe, p['exp_w3'])
    ye = jnp.einsum('becf,efd->becd', hid, p['exp_w2']) * gates[..., None].astype(h.dtype)
    return jax.vmap(lambda ib, yb: jnp.zeros((T, D), yb.dtype).at[ib.reshape(-1)].add(yb.reshape(-1, D)))(idx, ye)


def layer(x, ctx, c, c_ctx, p, ctx_out):
    m_lat = modulation(c[:, None, :], p)
    m_ctx = modulation(c_ctx, p)
    zero_state = jnp.zeros((ctx.shape[0], RW_HEADS, RW_HEAD, RW_HEAD), jnp.float32)
    h_ctx = rmsnorm(ctx, p['norm1_pre']) * (1.0 + m_ctx[1]) + m_ctx[0]
    if ctx_out:
        mixed_ctx, ctx_states = token_mixer(h_ctx, p, (zero_state, zero_state))
        ctx = ctx + m_ctx[2] * rmsnorm(mixed_ctx, p['norm1_post'])
        h2_ctx = rmsnorm(ctx, p['norm2_pre']) * (1.0 + m_ctx[4]) + m_ctx[3]
        ctx = ctx + m_ctx[5] * rmsnorm(expert_choice_moe(h2_ctx, p), p['norm2_post'])
    else:
        cols = h_ctx @ p['w_in'][:, HY_COLS + STATE_LO:HY_COLS + STATE_HI]
        ctx_states = rwkv_context_states(cols, p, zero_state)
    h = rmsnorm(x, p['norm1_pre']) * (1.0 + m_lat[1]) + m_lat[0]
    mixed, _ = token_mixer(h, p, ctx_states)
    x = x + m_lat[2] * rmsnorm(mixed, p['norm1_post'])
    h2 = rmsnorm(x, p['norm2_pre']) * (1.0 + m_lat[4]) + m_lat[3]
    x = x + m_lat[5] * rmsnorm(expert_choice_moe(h2, p), p['norm2_post'])
    return x, ctx


def setup_inputs(seed: int = 0) -> dict:
    key = jax.random.key(seed)
    ks = iter(jax.random.split(key, 64))
    f32 = jnp.float32

    def nrm(shape, scale=1.0):
        return scale * jax.random.normal(next(ks), shape, f32)

    def unif(shape, lo, hi):
        return jax.random.uniform(next(ks), shape, f32, lo, hi)

    L, D, E = DEPTH, D_MODEL, N_EXPERTS
    return {
        'x': nrm((BATCH, SEQ, D)),
        'c': nrm((BATCH, D)),
        'ctx': nrm((BATCH, CTX_LEN, D)),
        'c_ctx': nrm((D,)),
        'mod_w': nrm((L, D, 6 * D), 0.5 * D ** -0.5),
        'mod_b': nrm((L, 6 * D), 0.02),
        'norm1_pre': 1.0 + nrm((L, D), 0.1),
        'norm1_post': 1.0 + nrm((L, D), 0.1),
        'norm2_pre': 1.0 + nrm((L, D), 0.1),
        'norm2_post': 1.0 + nrm((L, D), 0.1),
        'w_in': nrm((L, D, IN_COLS), D ** -0.5),
        'hy_conv_w': nrm((L, 3, HY_COLS), 0.5),
        'hy_conv_b': nrm((L, HY_COLS), 0.02),
        'hy_ffn_w1': nrm((L, HY_EMB, HY_FILTER_ORDER), HY_EMB ** -0.5),
        'hy_ffn_b1': nrm((L, HY_FILTER_ORDER), 0.1),
        'hy_ffn_w2': nrm((L, HY_FILTER_ORDER, HY_FILTER_ORDER), HY_FILTER_ORDER ** -0.5),
        'hy_ffn_b2': nrm((L, HY_FILTER_ORDER), 0.1),
        'hy_ffn_w3': nrm((L, HY_FILTER_ORDER, 2 * HY_WIDTH), HY_FILTER_ORDER ** -0.5),
        'hy_freq': 1.0 + nrm((L, HY_FILTER_ORDER), 0.1),
        'hy_bias': nrm((L, HY_WIDTH), 0.5),
        'hy_proj': nrm((L, HY_WIDTH, D), HY_WIDTH ** -0.5),
        'rw_mu': unif((L, 2, RW_COLS), 0.0, 0.5),
        'rw_w0': unif((L, 2, RW_WIDTH), -6.0, 0.0),
        'rw_w_up': nrm((L, 2, LORA_W, RW_WIDTH), 0.1),
        'rw_a0': nrm((L, 2, RW_WIDTH), 0.3),
        'rw_a_up': nrm((L, 2, LORA_A, RW_WIDTH), 0.1),
        'rw_g_up': nrm((L, LORA_G, RW_WIDTH), LORA_G ** -0.5),
        'rw_k_k': 0.85 + nrm((L, RW_WIDTH), 0.1),
        'rw_k_a': 1.0 + nrm((L, RW_WIDTH), 0.1),
        'rw_r_k': nrm((L, RW_HEADS, RW_HEAD), 0.1),
        'rw_ln_g': 1.0 + nrm((L, RW_WIDTH), 0.1),
        'rw_ln_b': nrm((L, RW_WIDTH), 0.02),
        'rw_proj': nrm((L, RW_WIDTH, D), RW_WIDTH ** -0.5),
        'w_out': nrm((L, D, D), D ** -0.5),
        'router_w': nrm((L, D, E), D ** -0.5),
        'exp_w1': nrm((L, E, D, D_EXPERT), D ** -0.5),
        'exp_w3': nrm((L, E, D, D_EXPERT), D ** -0.5),
        'exp_w2': nrm((L, E, D_EXPERT, D), D_EXPERT ** -0.5),
    }


def reference(x, c, ctx, c_ctx, mod_w, mod_b, norm1_pre, norm1_post, norm2_pre, norm2_post, w_in,
              hy_conv_w, hy_conv_b, hy_ffn_w1, hy_ffn_b1, hy_ffn_w2, hy_ffn_b2, hy_ffn_w3, hy_freq,
              hy_bias, hy_proj, rw_mu, rw_w0, rw_w_up, rw_a0, rw_a_up, rw_g_up, rw_k_k, rw_k_a,
              rw_r_k, rw_ln_g, rw_ln_b, rw_proj, w_out, router_w, exp_w1, exp_w3, exp_w2):
    for l in range(DEPTH):
        p = {
            'mod_w': mod_w[l], 'mod_b': mod_b[l],
            'norm1_pre': norm1_pre[l], 'norm1_post': norm1_post[l],
            'norm2_pre': norm2_pre[l], 'norm2_post': norm2_post[l],
            'w_in': w_in[l],
            'hy_conv_w': hy_conv_w[l], 'hy_conv_b': hy_conv_b[l],
            'hy_ffn_w1': hy_ffn_w1[l], 'hy_ffn_b1': hy_ffn_b1[l],
            'hy_ffn_w2': hy_ffn_w2[l], 'hy_ffn_b2': hy_ffn_b2[l],
            'hy_ffn_w3': hy_ffn_w3[l], 'hy_freq': hy_freq[l],
            'hy_bias': hy_bias[l], 'hy_proj': hy_proj[l],
            'rw_mu': rw_mu[l], 'rw_w0': rw_w0[l], 'rw_w_up': rw_w_up[l],
            'rw_a0': rw_a0[l], 'rw_a_up': rw_a_up[l], 'rw_g_up': rw_g_up[l],
            'rw_k_k': rw_k_k[l], 'rw_k_a': rw_k_a[l], 'rw_r_k': rw_r_k[l],
            'rw_ln_g': rw_ln_g[l], 'rw_ln_b': rw_ln_b[l], 'rw_proj': rw_proj[l],
            'w_out': w_out[l], 'router_w': router_w[l],
            'exp_w1': exp_w1[l], 'exp_w3': exp_w3[l], 'exp_w2': exp_w2[l],
        }
        x, ctx = layer(x, ctx, c, c_ctx, p, l < DEPTH - 1)
    return x
```

```python
import contextlib
import math
import numpy as np
import concourse.bass as bass
import concourse.mybir as mybir
from concourse.bass_utils import run_bass_kernel_spmd
from concourse.ap import AP

F32 = mybir.dt.float32
BF16 = mybir.dt.bfloat16
I32 = mybir.dt.int32
U32 = mybir.dt.uint32
ALU = mybir.AluOpType
AF = mybir.ActivationFunctionType
AX = mybir.AxisListType

D = 1024
T = 8192
TC = 256
NCOL = 5376
HYW = 512
RWW = 512
NE = 16
DE = 2048
CAP = 1024
CH = 64
EPS = 1e-6
LN_EPS = 64e-5
ROWP = 1026

ENGS = ("pe", "act", "dve", "pool", "sp")
NDSEM = 12


def _res(x):
    if isinstance(x, str):
        return x
    if isinstance(x, AP):
        return x.tensor.name
    return x.name


class Prog:
    def __init__(self, nc, semctx):
        self.nc = nc
        self.semctx = semctx
        self.sems = {}
        self.q = {e: [] for e in ENGS}
        self.cnt = {e: 0 for e in ENGS}
        self.known = {e: {} for e in ENGS}
        self.lastw = {}
        self.readers = {}
        self.dcount = {}
        self.dnext = {e: 0 for e in ENGS}
        self.ninstr = 0
        self.excl = set()
        self._regs = {}
        self.phase = 0

    def _waits(self, eng, reads, writes):
        need = {}

        def add(tok):
            if tok is None:
                return
            sk, val, teng = tok
            if teng == "pe" and eng == "pe":
                return
            if self.known[eng].get(sk, 0) >= val:
                return
            if need.get(sk, 0) < val:
                need[sk] = val

        for r in reads:
            add(self.lastw.get(r))
        for w in writes:
            add(self.lastw.get(w))
            for t in self.readers.get(w, ()):
                add(t)
        for sk, val in need.items():
            self.known[eng][sk] = val
        return list(need.items())

    def _commit(self, tok, reads, writes):
        for r in reads:
            self.readers.setdefault(r, []).append(tok)
        for w in writes:
            self.lastw[w] = tok
            self.readers[w] = []

    def op(self, eng, fn, R=(), W=()):
        reads = [_res(r) for r in R]
        writes = [_res(w) for w in W]
        if self.excl:
            writes = writes + [r for r in reads if r in self.excl and r not in writes]
            reads = [r for r in reads if r not in self.excl]
        waits = self._waits(eng, reads, writes)
        self.cnt[eng] += 1
        tok = (eng, self.cnt[eng], eng)
        self.q[eng].append((fn, waits, (eng, 1)))
        self._commit(tok, reads, writes)
        self.ninstr += 1
        return tok

    def dma(self, eng, fn, R=(), W=()):
        reads = [_res(r) for r in R]
        writes = [_res(w) for w in W]
        slot = self.dnext[eng]
        self.dnext[eng] = (slot + 1) % NDSEM
        sk = "d_%s_%d" % (eng, slot)
        prev = self.dcount.get(sk, 0)
        waits = self._waits(eng, reads, writes)
        if prev > 0 and self.known[eng].get(sk, 0) < 16 * prev:
            waits.append((sk, 16 * prev))
            self.known[eng][sk] = 16 * prev
        self.dcount[sk] = prev + 1
        tok = (sk, 16 * (prev + 1), "dma")
        self.q[eng].append((fn, waits, (sk, 16)))
        self._commit(tok, reads, writes)
        self.ninstr += 1
        return tok

    def reg(self, en, value):
        key = (self.phase, value)
        if key not in self._regs:
            self._regs[key] = en.to_reg(value)
        return self._regs[key]

    def E(self, eng, meth, *a, R=(), W=(), **kw):
        return self.op(eng, lambda e: getattr(e, meth)(*a, **kw), R, W)

    def DMA(self, eng, out, in_, R=(), W=(), **kw):
        return self.dma(eng, lambda e: e.dma_start(out=out, in_=in_, **kw), R, W)

    def flush(self, final=False):
        nc = self.nc
        for k in list(ENGS) + list(self.dcount.keys()):
            if k not in self.sems:
                self.sems[k] = self.semctx.enter_context(nc.semaphore("s_" + k))
        sems = self.sems
        full = {e: self.cnt[e] for e in ENGS}
        for sk, c in self.dcount.items():
            full[sk] = 16 * c
        if final:
            for e in ENGS:
                self.q[e].append((None, [(sk, v) for sk, v in full.items() if v > 0], None))
        with nc.Block() as block:
            engmap = {"pe": block.tensor, "act": block.scalar, "dve": block.vector,
                      "pool": block.gpsimd, "sp": block.sync}
            for e in ENGS:
                lst = self.q[e]
                if not lst:
                    continue

                def body(engobj, lst=lst):
                    for fn, waits, inc in lst:
                        for sk, val in waits:
                            engobj.wait_ge(sems[sk], val)
                        if fn is not None:
                            ins = fn(engobj)
                            if inc is not None:
                                ins.then_inc(sems[inc[0]], inc[1])
                engmap[e](body)
        self.q = {e: [] for e in ENGS}
        self.phase += 1
        for e in ENGS:
            waits = [(sk, v) for sk, v in full.items() if v > 0 and self.known[e].get(sk, 0) < v]
            if waits:
                self.q[e].append((None, waits, None))
                for sk, v in waits:
                    self.known[e][sk] = v


def bc(ap_, shape_pairs):
    return AP(ap_.tensor, ap_.offset, [list(ap_.ap[0])] + [list(x) for x in shape_pairs])


def host_consts():
    c = {}
    L = T
    f32 = np.float32
    t = np.linspace(0.0, 1.0, L, dtype=f32)
    ang = (f32(2.0 * math.pi / L) * np.arange(L, dtype=f32)).astype(f32)
    bands = np.linspace(1e-4, 16 - 1, 16, dtype=f32)
    ph = (bands[None, :] * ang[:, None]).astype(f32)
    feat = np.concatenate([t[:, None], np.cos(ph), -np.sin(ph)], axis=-1).astype(f32)
    c["featT"] = np.ascontiguousarray(feat.T)
    c["trow"] = t.reshape(1, L).copy()
    deltas = np.abs(np.linspace(math.log(1e-2) / 1.5, math.log(1e-2) / 0.3, HYW, dtype=f32))
    nd = np.zeros((128, 8), f32)
    for j in range(8):
        nd[:, j] = -deltas[(j % 4) * 128:(j % 4) * 128 + 128]
    c["negdelta"] = nd
    n = np.arange(128)
    Fm = np.exp(-2j * np.pi * np.outer(n, n) / 128)
    Tw = np.exp(-2j * np.pi * np.outer(n, n) / (2 * L))
    Fr = Fm.real.astype(f32); Fi = Fm.imag.astype(f32)
    dft = np.stack([Fr, Fi, -Fi, Fr, -Fi, Fi, Fr], axis=1).astype(f32)
    c["dft"] = np.ascontiguousarray(dft)
    tw = np.stack([Tw.real, Tw.imag, -Tw.imag], axis=1).astype(f32)
    c["tw"] = np.ascontiguousarray(tw)
    return c


def build(taps=(), upto=99):
    nc = bass.Bass("TRN2", target_bir_lowering=False)
    G = contextlib.ExitStack()
    semctx = contextlib.ExitStack()
    P = Prog(nc, semctx)

    def din(name, shape, dt=F32):
        return nc.dram_tensor(name, list(shape), dt, kind="ExternalInput").ap()

    def dscr(name, shape, dt):
        kind = "ExternalOutput" if name in taps else "Internal"
        return nc.dram_tensor(name, list(shape), dt, kind=kind).ap()

    def gsb(name, shape, dt):
        return G.enter_context(nc.sbuf_tensor(name, list(shape), dt))

    x_in = din("x", [T, D]); ctx_in = din("ctx", [TC, D]); cvec = din("cvec", [128, 16])
    mod_w = din("mod_w", [D, 6 * D]); mod_b = din("mod_b", [1, 6 * D])
    norms = din("norms", [4, D])
    w_in = din("w_in", [D, NCOL])
    hcw = din("hcw", [128, 12, 4])
    hy_w1 = din("hy_w1", [33, 64]); hy_w2 = din("hy_w2", [64, 64]); hy_w3 = din("hy_w3", [64, 1024])
    hyp = din("hyp", [64, 3]); hy_biasc = din("hy_biasc", [128, 4])
    hy_proj = din("hy_proj", [HYW, D]); rw_proj = din("rw_proj", [RWW, D]); w_out = din("w_out", [D, D])
    router_w = din("router_w", [D, NE])
    mucol = din("mucol", [128, 14, 2]); rw_w0a0 = din("rw_w0a0", [128, 4, 4])
    rw_up = din("rw_up", [128, 2, 512]); rw_g_up = din("rw_g_up", [128, 512]); rwc = din("rwc", [128, 4, 5])
    exp_w1 = din("exp_w1", [NE, D, DE]); exp_w3 = din("exp_w3", [NE, D, DE]); exp_w2 = din("exp_w2", [NE, DE, D])
    featT = din("featT", [33, T]); trow = din("trow", [1, T]); negdelta = din("negdelta", [128, 8])
    dft_in = din("dft", [128, 7, 128]); tw_in = din("tw", [128, 3, 128])
    y_out = nc.dram_tensor("y_out", [T, D], F32, kind="ExternalOutput").ap()

    colsT = dscr("colsT", [NCOL, T], BF16)
    ccT = dscr("ccT", [1792, TC], BF16)

    identb = gsb("identb", [128, 128], BF16)
    identf = gsb("identf", [128, 128], F32)
    nhalf = gsb("nhalf", [128, 1], F32)
    invn = gsb("invn", [128, 4], F32)
    blk64 = gsb("blk64", [128, 128], F32)
    AFF = gsb("AFF", [128, T // 128, NE], F32)
    thrB = gsb("thrB", [128, NE], F32)
    pidx = gsb("pidx", [128, 1], F32)
    SLOT = gsb("SLOT", [128, T // 128, NE], I32)
    rows = {"F": gsb("RF", [128, D], F32)}
    G2 = contextlib.ExitStack()
    for k in ("A", "B", "C", "D", "E", "Ac", "Bc"):
        rows[k] = G2.enter_context(nc.sbuf_tensor("R" + k, [128, D], F32))

    P.E("pool", "memset", identf[:], 1.0, W=[identf])
    P.E("pool", "affine_select", identf[:], identf[:], [[-1, 128]], ALU.is_equal, 0.0, base=0,
        channel_multiplier=1, R=[identf], W=[identf])
    P.E("pool", "tensor_copy", identb[:], identf[:], R=[identf], W=[identb])
    P.E("pool", "memset", nhalf[:], -0.5, W=[nhalf])

    def run_pipe(genfn, n, maxlive):
        live = []
        nxt = 0
        while live or nxt < n:
            if nxt < n and len(live) < maxlive:
                live.append(genfn(nxt)); nxt += 1
            nl = []
            for g_ in live:
                try:
                    next(g_); nl.append(g_)
                except StopIteration:
                    pass
            live = nl

    def dram_bcast(ap2d, ncols):
        return AP(ap2d.tensor, ap2d.offset, [[0, 128], [1, ncols]])

    with contextlib.ExitStack() as ph:
        def sb(name, shape, dt):
            return ph.enter_context(nc.sbuf_tensor(name, list(shape), dt))

        def psum(name, shape, dt):
            P.excl.add(name)
            return ph.enter_context(nc.psum_tensor(name, list(shape), dt))
        cv = sb("A_cv", [128, 16], F32); sc = sb("A_sc", [128, 16], F32)
        SC = sb("A_SC", [128, 16, 128], F32)
        MB = sb("A_MB", [128, 6 * D], F32); M = sb("A_M", [128, 6 * D], F32); Mc = sb("A_Mc", [128, 2 * D], F32)
        Gn = sb("A_G", [128, 4, D], F32)
        wts = [sb("A_w%d" % i, [128, 8, 512], F32) for i in range(2)]
        pss = [psum("A_p%d" % i, [128, 512], F32) for i in range(4)]
        P.DMA("sp", cv[:], cvec[:, :], W=[cv])
        P.DMA("sp", MB[:], dram_bcast(mod_b[0:1, :], 6 * D), W=[MB])
        for i in range(4):
            P.DMA("sp", Gn[:, i, :], dram_bcast(norms[i:i + 1, :], D), W=[Gn])
        P.E("act", "activation", sc[:], cv[:], AF.Silu, R=[cv], W=[sc])
        P.E("dve", "tensor_copy", SC[:], bc(sc[:], [[1, 16], [0, 128]]), R=[sc], W=[SC])
        pi = 0
        for cb in range(12):
            wt = wts[cb % 2]
            P.DMA("sp", wt[:], mod_w[:, cb * 512:(cb + 1) * 512].rearrange("(k p) n -> p k n", p=128), W=[wt])
            for which in range(2 if cb < 4 else 1):
                ps = pss[pi % 4]; pi += 1
                for k in range(8):
                    P.E("pe", "matmul", ps[:], SC[:, which * 8 + k, :], wt[:, k, :], start=(k == 0), stop=(k == 7),
                        R=[SC, wt], W=[ps])
                dstM = M if which == 0 else Mc
                P.E("dve", "tensor_tensor", dstM[:, cb * 512:(cb + 1) * 512], ps[:], MB[:, cb * 512:(cb + 1) * 512],
                    ALU.add, R=[ps, MB], W=[dstM])

        def seg(t_, i):
            return t_[:, i * D:(i + 1) * D]
        P.E("dve", "scalar_tensor_tensor", rows["A"][:], seg(M, 1), 1.0, Gn[:, 0, :], ALU.add, ALU.mult, R=[M, Gn], W=[rows["A"]])
        P.E("act", "activation", rows["B"][:], seg(M, 0), AF.Copy, R=[M], W=[rows["B"]])
        P.E("dve", "tensor_tensor", rows["C"][:], seg(M, 2), Gn[:, 1, :], ALU.mult, R=[M, Gn], W=[rows["C"]])
        P.E("dve", "scalar_tensor_tensor", rows["D"][:], seg(M, 4), 1.0, Gn[:, 2, :], ALU.add, ALU.mult, R=[M, Gn], W=[rows["D"]])
        P.E("act", "activation", rows["E"][:], seg(M, 3), AF.Copy, R=[M], W=[rows["E"]])
        P.E("dve", "tensor_tensor", rows["F"][:], seg(M, 5), Gn[:, 3, :], ALU.mult, R=[M, Gn], W=[rows["F"]])
        P.E("dve", "scalar_tensor_tensor", rows["Ac"][:], seg(Mc, 1), 1.0, Gn[:, 0, :], ALU.add, ALU.mult, R=[Mc, Gn], W=[rows["Ac"]])
        P.E("act", "activation", rows["Bc"][:], seg(Mc, 0), AF.Copy, R=[Mc], W=[rows["Bc"]])
        if "dbg_rows" in taps:
            dbg_rows = dscr("dbg_rows", [8, D], F32)
            for i, k in enumerate(("A", "B", "C", "D", "E", "F", "Ac", "Bc")):
                P.DMA("sp", dbg_rows[i:i + 1, :], rows[k][0:1, :], R=[rows[k]])
        P.flush()
    if upto <= 0:
        P.flush(final=True); semctx.close(); G2.close(); G.close(); return nc

    with contextlib.ExitStack() as ph:
        def sb(name, shape, dt):
            return ph.enter_context(nc.sbuf_tensor(name, list(shape), dt))

        def psum(name, shape, dt):
            P.excl.add(name)
            return ph.enter_context(nc.psum_tensor(name, list(shape), dt))
        WIN = sb("B_win", [128, 8, NCOL], BF16)
        for k in range(8):
            for c0 in range(0, NCOL, 1792):
                P.DMA("pool", WIN[:, k, c0:c0 + 1792], w_in[k * 128:(k + 1) * 128, c0:c0 + 1792], W=[WIN])
        xts = [sb("B_x%d" % i, [128, D], F32) for i in range(2)]
        junk = sb("B_junk", [128, D], BF16)
        t1s = [sb("B_t1%d" % i, [128, D], F32) for i in range(2)]
        hbs = [sb("B_hb%d" % i, [128, D], BF16) for i in range(2)]
        sss = [sb("B_ss%d" % i, [128, 4], F32) for i in range(2)]
        hTs = [sb("B_hT%d" % i, [128, 8, 512], BF16) for i in range(2)]
        stg = [sb("B_st%d" % i, [128, 512], BF16) for i in range(4)]
        pTs = [psum("B_pT%d" % i, [128, 8, 128], BF16) for i in range(2)]
        pos = [psum("B_po%d" % i, [128, 512], F32) for i in range(4)]
        cnt = {"tile": 0, "blk": 0, "ev": 0}

        hb4 = [sb("B_hb4_%d" % i, [128, D], BF16) for i in range(8)]

        def inproj(src, ntiles, RAx, RBx, chunks, dst, row0):
            nblk = (ntiles + 3) // 4

            def tiles_of(blk):
                return list(range(blk * 4, min(ntiles, blk * 4 + 4)))

            def prep_norm(blk):
                for i, tix in enumerate(tiles_of(blk)):
                    j = cnt["tile"] % 2; cnt["tile"] += 1
                    xt, t1, ss = xts[j], t1s[j], sss[j]
                    hb = hb4[(blk % 2) * 4 + i]
                    P.DMA("sp", xt[:], src[tix * 128:(tix + 1) * 128, :], W=[xt])
                    P.E("act", "activation", junk[:], xt[:], AF.Square, accum_out=ss[:, 0:1], R=[xt], W=[junk, ss])
                    P.E("dve", "tensor_scalar", ss[:, 1:2], ss[:, 0:1], 1.0 / D, EPS, ALU.mult, ALU.add, R=[ss], W=[ss])
                    P.E("pool", "tensor_tensor", ss[:, 2:3], ss[:, 1:2], nhalf[:], ALU.pow, R=[ss, nhalf], W=[ss])
                    P.E("dve", "scalar_tensor_tensor", t1[:], xt[:], ss[:, 2:3], RAx[:], ALU.mult, ALU.mult,
                        R=[xt, ss, RAx], W=[t1])
                    P.E("dve", "tensor_tensor", hb[:], t1[:], RBx[:], ALU.add, R=[t1, RBx], W=[hb])

            def prep_T(blk):
                hT = hTs[blk % 2]
                for i, tix in enumerate(tiles_of(blk)):
                    hb = hb4[(blk % 2) * 4 + i]; pT = pTs[i % 2]
                    for k in range(8):
                        P.E("pe", "transpose", pT[:, k, :], hb[:, k * 128:(k + 1) * 128], identb[:], R=[hb, identb], W=[pT])
                    P.E("act", "activation", hT[:, :, i * 128:(i + 1) * 128], pT[:], AF.Copy, R=[pT], W=[hT])
            prep_norm(0)
            prep_T(0)
            for blk in range(nblk):
                N = 128 * len(tiles_of(blk))
                hT = hTs[blk % 2]
                if blk + 1 < nblk:
                    prep_norm(blk + 1)
                for c in chunks:
                    e = cnt["ev"]; cnt["ev"] += 1
                    po = pos[e % 4]; st = stg[e % 4]
                    for k in range(8):
                        P.E("pe", "matmul", po[:, 0:N], WIN[:, k, c * 128:(c + 1) * 128], hT[:, k, 0:N],
                            start=(k == 0), stop=(k == 7), R=[WIN, hT], W=[po])
                    if c >= 26:
                        P.E("act", "activation", st[:, 0:N], po[:, 0:N], AF.Sigmoid, R=[po], W=[st])
                    elif e % 2 == 0:
                        P.E("dve", "tensor_copy", st[:, 0:N], po[:, 0:N], R=[po], W=[st])
                    else:
                        P.E("act", "activation", st[:, 0:N], po[:, 0:N], AF.Copy, R=[po], W=[st])
                    r0 = (c - row0) * 128
                    P.DMA("sp", dst[r0:r0 + 128, blk * 512:blk * 512 + N], st[:, 0:N], R=[st])
                if blk + 1 < nblk:
                    prep_T(blk + 1)

        inproj(ctx_in, TC // 128, rows["Ac"], rows["Bc"], list(range(12, 26)), ccT, 12)
        inproj(x_in, T // 128, rows["A"], rows["B"], list(range(42)), colsT, 0)
        P.flush()
    if upto <= 1:
        P.flush(final=True); semctx.close(); G2.close(); G.close(); return nc

    zT = dscr("zT", [HYW, T], BF16); x0T = dscr("x0T", [HYW, T], BF16)
    with contextlib.ExitStack() as ph:
        def sb(name, shape, dt):
            return ph.enter_context(nc.sbuf_tensor(name, list(shape), dt))
        NB = 2048
        hc = sb("D_hcw", [128, 12, 4], F32)
        P.DMA("sp", hc[:], hcw[:, :, :], W=[hc])
        raws = [[sb("D_raw%d_%d" % (p_, i), [128, NB + 2], BF16) for i in range(2)] for p_ in range(3)]
        uus = [[sb("D_u%d_%d" % (p_, i), [128, NB], F32) for i in range(2)] for p_ in range(3)]
        zbs = [sb("D_zb%d" % i, [128, NB], BF16) for i in range(2)]
        x0bs = [sb("D_x0b%d" % i, [128, NB], BF16) for i in range(2)]
        it = 0
        for ct in range(4):
            for blk in range(T // NB):
                t0 = blk * NB
                b_ = it % 2; it += 1
                for part in range(3):
                    j = part * 4 + ct
                    raw = raws[part][b_]; u = uus[part][b_]
                    lo = max(t0 - 1, 0); hi = min(t0 + NB + 1, T)
                    P.DMA("sp", raw[:, lo - (t0 - 1):hi - (t0 - 1)], colsT[j * 128:(j + 1) * 128, lo:hi], W=[raw])
                    if blk == 0:
                        P.E("pool", "memset", raw[:, 0:1], 0.0, W=[raw])
                    if hi == T:
                        P.E("pool", "memset", raw[:, NB + 1:NB + 2], 0.0, W=[raw])
                    P.E("act", "activation", u[:], raw[:, 1:NB + 1], AF.Identity, bias=hc[:, j, 3:4], scale=hc[:, j, 1:2],
                        R=[raw, hc], W=[u])
                    P.E("dve", "scalar_tensor_tensor", u[:], raw[:, 0:NB], hc[:, j, 0:1], u[:], ALU.mult, ALU.add,
                        R=[raw, hc, u], W=[u])
                    P.E("dve", "scalar_tensor_tensor", u[:], raw[:, 2:NB + 2], hc[:, j, 2:3], u[:], ALU.mult, ALU.add,
                        R=[raw, hc, u], W=[u])
                zb = zbs[b_]; x0b = x0bs[b_]
                P.E("dve", "tensor_tensor", zb[:], uus[2][b_][:], uus[1][b_][:], ALU.mult, R=[uus[2][b_], uus[1][b_]], W=[zb])
                P.E("act", "activation", x0b[:], uus[0][b_][:], AF.Copy, R=[uus[0][b_]], W=[x0b])
                P.DMA("sp", zT[ct * 128:(ct + 1) * 128, t0:t0 + NB], zb[:], R=[zb])
                P.DMA("sp", x0T[ct * 128:(ct + 1) * 128, t0:t0 + NB], x0b[:], R=[x0b])
        P.flush()
    if upto <= 2:
        P.flush(final=True); semctx.close(); G2.close(); G.close(); return nc

    filtT = dscr("filtT", [2 * HYW, T], BF16)
    HsT = dscr("HsT", [128, HYW, 3, 128], BF16)
    ycT = dscr("ycT", [HYW, T], F32)
    TWO_PI_LO = 6.283184
    with contextlib.ExitStack() as ph:
        def sb(name, shape, dt):
            return ph.enter_context(nc.sbuf_tensor(name, list(shape), dt))

        def psum(name, shape, dt):
            P.excl.add(name)
            return ph.enter_context(nc.psum_tensor(name, list(shape), dt))
        hid2 = sb("E_hid2", [64, T], F32)
        w3 = sb("E_w3", [64, 2 * HYW], F32)
        P.DMA("sp", w3[:], hy_w3[:, :], W=[w3])
        with contextlib.ExitStack() as ph2:
            def sb2(name, shape, dt):
                return ph2.enter_context(nc.sbuf_tensor(name, list(shape), dt))
            ft = sb2("E_ft", [33, T], F32); hid1 = sb2("E_hid1", [64, T], F32)
            w1 = sb2("E_w1", [33, 64], F32); w2 = sb2("E_w2", [64, 64], F32)
            hp_ = sb2("E_hp", [64, 3], F32); cl_ = sb2("E_cl", [64, 3], F32)
            q = sb2("E_q", [64, 2048], F32); qi = sb2("E_qi", [64, 2048], I32); cm = sb2("E_cm", [64, 2048], F32)
            pq = ph2.enter_context(nc.psum_tensor("E_pq", [64, 2048], F32)); P.excl.add("E_pq")
            P.DMA("sp", ft[:], featT[:, :], W=[ft]); P.DMA("sp", w1[:], hy_w1[:, :], W=[w1])
            P.DMA("sp", w2[:], hy_w2[:, :], W=[w2]); P.DMA("sp", hp_[:], hyp[:, :], W=[hp_])
            P.E("dve", "tensor_scalar", cl_[:, 0:1], hp_[:, 0:1], 1.0 / (2.0 * math.pi), None, ALU.mult, R=[hp_], W=[cl_])
            P.E("dve", "tensor_tensor", cl_[:, 1:3], hp_[:, 1:3], bc(cl_[:, 0:1], [[0, 2]]), ALU.mult, R=[hp_, cl_], W=[cl_])
            for layer in range(2):
                src = ft if layer == 0 else hid1
                wl = w1 if layer == 0 else w2
                dsth = hid1 if layer == 0 else hid2
                for blk in range(4):
                    for s4 in range(4):
                        c0 = blk * 2048 + s4 * 512
                        P.E("pe", "matmul", pq[:, s4 * 512:(s4 + 1) * 512], wl[:], src[:, c0:c0 + 512], start=True, stop=True,
                            R=[wl, src], W=[pq])
                    P.E("dve", "tensor_scalar", q[:], pq[:], cl_[:, 0:1], cl_[:, 1 + layer:2 + layer], ALU.mult, ALU.add,
                        R=[pq, cl_], W=[q])
                    P.E("dve", "tensor_copy", qi[:], q[:], R=[q], W=[qi])
                    P.E("dve", "tensor_copy", cm[:], qi[:], R=[qi], W=[cm])
                    P.E("dve", "tensor_tensor", q[:], q[:], cm[:], ALU.subtract, R=[q, cm], W=[q])
                    P.E("dve", "tensor_single_scalar", cm[:], q[:], 0.5, ALU.is_gt, R=[q], W=[cm])
                    P.E("dve", "tensor_tensor", q[:], q[:], cm[:], ALU.subtract, R=[q, cm], W=[q])
                    P.E("dve", "tensor_single_scalar", cm[:], q[:], -0.5, ALU.is_lt, R=[q], W=[cm])
                    P.E("dve", "tensor_tensor", q[:], q[:], cm[:], ALU.add, R=[q, cm], W=[q])
                    P.E("act", "activation", dsth[:, blk * 2048:(blk + 1) * 2048], q[:], AF.Sin, scale=TWO_PI_LO, R=[q], W=[dsth])
            P.flush()
        with contextlib.ExitStack() as ph2:
            def sb2(name, shape, dt):
                return ph2.enter_context(nc.sbuf_tensor(name, list(shape), dt))
            trb = sb2("E_trb", [128, T], F32); win = sb2("E_win", [128, T], F32)
            ndl = sb2("E_ndl", [128, 8], F32)
            asum = sb2("E_asum", [128, 8, 16], F32); asr = sb2("E_asr", [128, 8], F32)
            fwbs = [sb2("E_fwb%d" % i, [128, 512], BF16) for i in range(3)]
            junk2 = sb2("E_junk", [128, 512], BF16)
            pfs = [ph2.enter_context(nc.psum_tensor("E_pf%d" % i, [128, 512], F32)) for i in range(3)]
            P.excl.update("E_pf%d" % i for i in range(3))
            P.DMA("sp", trb[:], dram_bcast(trow[0:1, :], T), W=[trb])
            P.DMA("sp", ndl[:], negdelta[:, :], W=[ndl])
            P.E("pool", "memset", asum[:], 0.0, W=[asum])
            e3 = 0
            for j in range(8):
                P.E("act", "activation", win[:], trb[:], AF.Exp, scale=ndl[:, j:j + 1], R=[trb, ndl], W=[win])
                for blk in range(16):
                    pf = pfs[e3 % 3]; fwb = fwbs[e3 % 3]; e3 += 1
                    P.E("pe", "matmul", pf[:], w3[:, j * 128:(j + 1) * 128], hid2[:, blk * 512:(blk + 1) * 512], start=True, stop=True,
                        R=[w3, hid2], W=[pf])
                    P.E("dve", "tensor_tensor", fwb[:], pf[:], win[:, blk * 512:(blk + 1) * 512], ALU.mult, R=[pf, win], W=[fwb])
                    if j >= 4 and blk == 0:
                        P.E("pool", "memset", fwb[:, 0:1], 0.0, W=[fwb])
                    P.E("dve", "reduce_sum", asum[:, j, blk:blk + 1], fwb[:], AX.X, apply_absolute_value=True,
                        R=[fwb], W=[asum])
                    P.DMA("sp", filtT[j * 128:(j + 1) * 128, blk * 512:(blk + 1) * 512], fwb[:], R=[fwb])
            P.E("dve", "reduce_sum", asr[:], asum[:], AX.X, R=[asum], W=[asr])
            P.E("dve", "tensor_tensor", asr[:, 0:4], asr[:, 0:4], asr[:, 4:8], ALU.add, R=[asr], W=[asr])
            P.E("dve", "reciprocal", invn[:], asr[:, 0:4], R=[asr], W=[invn])
            P.flush()
    if upto <= 3:
        P.flush(final=True); semctx.close(); G2.close(); G.close(); return nc

    with contextlib.ExitStack() as ph:
        def sb(name, shape, dt):
            return ph.enter_context(nc.sbuf_tensor(name, list(shape), dt))

        def psum(name, shape, dt):
            P.excl.add(name)
            return ph.enter_context(nc.psum_tensor(name, list(shape), dt))
        dftb = sb("F_dft", [128, 7, 128], BF16); twt = sb("F_tw", [128, 3, 128], BF16)
        P.DMA("pool", dftb[:], dft_in[:, :, :], W=[dftb])
        P.DMA("pool", twt[:], tw_in[:, :, :], W=[twt])
        Evs = [sb("F_Ev%d" % i, [128, 1024], BF16) for i in range(2)]
        NBUF = 6
        zins = [sb("F_zin%d" % i, [64, 4, 128], BF16) for i in range(NBUF)]
        S1s = [sb("F_S1%d" % i, [128, 1024], BF16) for i in range(2)]
        S2s = [sb("F_S2%d" % i, [128, 1024], BF16) for i in range(2)]
        Aps = [sb("F_Ap%d" % i, [128, 4, 2, 128], BF16) for i in range(NBUF)]
        Hss = [sb("F_Hs%d" % i, [128, 4, 3, 128], BF16) for i in range(NBUF)]
        Yts = [sb("F_Yt%d" % i, [128, 2, 4, 128], BF16) for i in range(NBUF)]
        Bps = [sb("F_Bp%d" % i, [128, 4, 2, 128], BF16) for i in range(NBUF)]
        ysbs = [sb("F_ys%d" % i, [64, 4, 128], F32) for i in range(NBUF)]
        Hos = [sb("F_Ho%d" % i, [128, 2, 3, 128], BF16) for i in range(NBUF)]
        Xcs = [sb("F_Xc%d" % i, [128, 2, 4, 128], F32) for i in range(NBUF)]
        EvA = [sb("F_EvA%d" % i, [128, 1024], BF16) for i in range(2)]
        EvB = [sb("F_EvB%d" % i, [128, 1024], BF16) for i in range(2)]
        EvC = [sb("F_EvC%d" % i, [128, 1024], BF16) for i in range(2)]
        pA = psum("F_pA", [128, 4, 256], F32); pX = psum("F_pX", [128, 2, 512], F32)
        pB = psum("F_pB", [128, 4, 256], F32); pY = psum("F_pY", [64, 512], F32)
        pA2 = pB

        def v4(t_, off, d1, d2, d3):
            base = t_[:]
            return AP(base.tensor, base.offset + off, [list(base.ap[0]), list(d1), list(d2), list(d3)])
        scn = {"n": 0}

        def tw_mul(src, dst, conj):
            S1 = S1s[scn["n"] % 2]; S2 = S2s[scn["n"] % 2]; scn["n"] += 1
            P.E("dve", "tensor_tensor", v4(S1, 0, [256, 4], [128, 2], [1, 128]), v4(src, 0, [256, 4], [128, 2], [1, 128]),
                v4(twt, 0, [0, 4], [0, 2], [1, 128]), ALU.mult, R=[src, twt], W=[S1])
            if not conj:
                twB = v4(twt, 256, [0, 4], [-128, 2], [1, 128])
            else:
                twB = v4(twt, 128, [0, 4], [128, 2], [1, 128])
            P.E("dve", "tensor_tensor", v4(S2, 0, [256, 4], [128, 2], [1, 128]), v4(src, 128, [256, 4], [-128, 2], [1, 128]),
                twB, ALU.mult, R=[src, twt], W=[S2])
            P.E("dve", "tensor_tensor", dst[:].rearrange("p c r k -> p (c r k)"), S1[:], S2[:], ALU.add, R=[S1, S2], W=[dst])

        def fwd_gen(g, zin, Ap, pA=pA):
            yield
            for c in range(4):
                P.E("pe", "matmul", pA[:, c, :], zin[:, c, :], dftb[0:64, 0:2, :], start=True, stop=True, R=[zin, dftb], W=[pA])
            yield
            ev = EvA[g % 2]
            P.E("act", "activation", ev[:], pA[:].rearrange("p a b -> p (a b)"), AF.Copy, R=[pA], W=[ev])
            yield
            tw_mul(ev, Ap, False)
            yield
            Ar = Ap[:, :, 0, :]; Ai = Ap[:, :, 1, :]
            P.E("pe", "matmul", pX[:, 0, :], dftb[:, 0, :], Ar, start=True, stop=False, R=[dftb, Ap], W=[pX])
            P.E("pe", "matmul", pX[:, 0, :], dftb[:, 2, :], Ai, start=False, stop=True, R=[dftb, Ap], W=[pX])
            P.E("pe", "matmul", pX[:, 1, :], dftb[:, 1, :], Ar, start=True, stop=False, R=[dftb, Ap], W=[pX])
            P.E("pe", "matmul", pX[:, 1, :], dftb[:, 0, :], Ai, start=False, stop=True, R=[dftb, Ap], W=[pX])
            yield

        def filt_gen(g):
            b_ = g % NBUF
            zin, Ap, Ho, Xc = zins[b_], Aps[b_], Hos[b_], Xcs[b_]
            c0 = 2 * g
            P.DMA("sp", zin[:, 0:2, :], filtT[c0:c0 + 2, :].rearrange("c (a b) -> a c b", b=128), W=[zin])
            P.DMA("sp", zin[:, 2:4, :], filtT[HYW + c0:HYW + c0 + 2, :].rearrange("c (a b) -> a c b", b=128), W=[zin])
            yield from fwd_gen(g, zin, Ap, pA if g % 2 == 0 else pA2)
            P.E("act", "activation", Xc[:].rearrange("p r c k -> p (r c k)"), pX[:].rearrange("p a b -> p (a b)"), AF.Copy, R=[pX], W=[Xc])
            yield
            P.E("dve", "tensor_tensor", v4(Ho, 0, [384, 2], [1, 128], [0, 1]), v4(Xc, 0, [128, 2], [1, 128], [0, 1]),
                v4(Xc, 256, [128, 2], [1, 128], [0, 1]), ALU.add, R=[Xc], W=[Ho])
            P.E("dve", "tensor_tensor", v4(Ho, 128, [384, 2], [1, 128], [0, 1]), v4(Xc, 512, [128, 2], [1, 128], [0, 1]),
                v4(Xc, 768, [128, 2], [1, 128], [0, 1]), ALU.subtract, R=[Xc], W=[Ho])
            yield
            P.E("act", "activation", v4(Ho, 256, [384, 2], [1, 128], [0, 1]), v4(Ho, 128, [384, 2], [1, 128], [0, 1]),
                AF.Copy, scale=-1.0, R=[Ho], W=[Ho])
            P.DMA("sp", HsT[:, c0:c0 + 2, :, :], Ho[:], R=[Ho])
            yield

        def conv_gen(g):
            b_ = g % NBUF
            zin, Ap, Hs, Yt, Bp, ysb = zins[b_], Aps[b_], Hss[b_], Yts[b_], Bps[b_], ysbs[b_]
            c0 = 4 * g
            P.DMA("sp", zin[:], zT[c0:c0 + 4, :].rearrange("c (a b) -> a c b", b=128), W=[zin])
            gen = fwd_gen(g, zin, Ap)
            hop = 0
            for _ in gen:
                hop += 1
                if hop == 3:
                    P.DMA("sp", Hs[:], HsT[:, c0:c0 + 4, :, :], W=[Hs])
                yield
            Xe = EvB[g % 2]
            P.E("act", "activation", Xe[:], pX[:].rearrange("p a b -> p (a b)"), AF.Copy, R=[pX], W=[Xe])
            yield
            S1 = S1s[scn["n"] % 2]; S2 = S2s[scn["n"] % 2]; scn["n"] += 1
            P.E("dve", "tensor_tensor", v4(S1, 0, [512, 2], [128, 4], [1, 128]), v4(Xe, 0, [512, 2], [128, 4], [1, 128]),
                v4(Hs, 0, [0, 2], [384, 4], [1, 128]), ALU.mult, R=[Xe, Hs], W=[S1])
            P.E("dve", "tensor_tensor", v4(S2, 0, [512, 2], [128, 4], [1, 128]), v4(Xe, 512, [-512, 2], [128, 4], [1, 128]),
                v4(Hs, 256, [-128, 2], [384, 4], [1, 128]), ALU.mult, R=[Xe, Hs], W=[S2])
            P.E("dve", "tensor_tensor", Yt[:].rearrange("p r c k -> p (r c k)"), S1[:], S2[:], ALU.add, R=[S1, S2], W=[Yt])
            yield
            for c in range(4):
                P.E("pe", "matmul", pB[:, c, :], Yt[:, 0, c, :], dftb[:, 3:5, :], start=True, stop=False, R=[Yt, dftb], W=[pB])
                P.E("pe", "matmul", pB[:, c, :], Yt[:, 1, c, :], dftb[:, 5:7, :], start=False, stop=True, R=[Yt, dftb], W=[pB])
            yield
            ev = EvC[g % 2]
            P.E("act", "activation", ev[:], pB[:].rearrange("p a b -> p (a b)"), AF.Copy, R=[pB], W=[ev])
            yield
            tw_mul(ev, Bp, True)
            yield
            P.E("pe", "matmul", pY[:], dftb[:, 0, 0:64], Bp[:, :, 0, :], start=True, stop=False, R=[dftb, Bp], W=[pY])
            P.E("pe", "matmul", pY[:], dftb[:, 1, 0:64], Bp[:, :, 1, :], start=False, stop=True, R=[dftb, Bp], W=[pY])
            yield
            P.E("act", "activation", ysb[:].rearrange("p c k -> p (c k)"), pY[:], AF.Copy, scale=1.0 / (2 * T), R=[pY], W=[ysb])
            P.DMA("sp", ycT[c0:c0 + 4, :].rearrange("c (a b) -> a c b", b=128), ysb[:], R=[ysb])
            yield

        def run_pipeline(genfn, ngroups, maxlive):
            live = []
            nxt = 0
            while live or nxt < ngroups:
                if nxt < ngroups and len(live) < maxlive:
                    live.append(genfn(nxt)); nxt += 1
                nl = []
                for g_ in live:
                    try:
                        next(g_); nl.append(g_)
                    except StopIteration:
                        pass
                live = nl

        run_pipeline(filt_gen, HYW // 2, 6)
        P.flush()
        if upto > 4:
            run_pipeline(conv_gen, HYW // 4, 6)
            P.flush()
    if upto <= 5:
        P.flush(final=True); semctx.close(); G2.close(); G.close(); return nc

    hyT = dscr("hyT", [HYW, T], BF16)
    with contextlib.ExitStack() as ph:
        def sb(name, shape, dt):
            return ph.enter_context(nc.sbuf_tensor(name, list(shape), dt))
        NB = 2048
        hbias = sb("G_hb", [128, 4], F32)
        P.DMA("sp", hbias[:], hy_biasc[:, :], W=[hbias])
        ycs = [sb("G_yc%d" % i, [128, NB], F32) for i in range(2)]
        zs = [sb("G_z%d" % i, [128, NB], BF16) for i in range(2)]
        x0s = [sb("G_x0%d" % i, [128, NB], BF16) for i in range(2)]
        hys = [sb("G_hy%d" % i, [128, NB], BF16) for i in range(2)]
        it = 0
        for ct in range(4):
            for blk in range(T // NB):
                b_ = it % 2; it += 1
                sl_ = (slice(ct * 128, (ct + 1) * 128), slice(blk * NB, (blk + 1) * NB))
                P.DMA("sp", ycs[b_][:], ycT[sl_], W=[ycs[b_]])
                P.DMA("sp", zs[b_][:], zT[sl_], W=[zs[b_]])
                P.DMA("sp", x0s[b_][:], x0T[sl_], W=[x0s[b_]])
                P.E("act", "activation", ycs[b_][:], ycs[b_][:], AF.Identity, scale=invn[:, ct:ct + 1], R=[ycs[b_], invn], W=[ycs[b_]])
                P.E("dve", "scalar_tensor_tensor", ycs[b_][:], zs[b_][:], hbias[:, ct:ct + 1], ycs[b_][:], ALU.mult, ALU.add,
                    R=[zs[b_], hbias, ycs[b_]], W=[ycs[b_]])
                P.E("dve", "tensor_tensor", hys[b_][:], ycs[b_][:], x0s[b_][:], ALU.mult, R=[ycs[b_], x0s[b_]], W=[hys[b_]])
                P.DMA("sp", hyT[sl_], hys[b_][:], R=[hys[b_]])
        P.flush()
    if upto <= 6:
        P.flush(final=True); semctx.close(); G2.close(); G.close(); return nc

    C0 = math.exp(-0.5)
    seqs = {}
    for sk, Tn in (("c", TC), ("m", T)):
        dd = {"T": Tn}
        for nm in ("Af", "Bf", "Kf", "Rf"):
            dd[nm] = [dscr("%s%d%s" % (nm, d, sk), [RWW, Tn], BF16) for d in range(2)]
        dd["V"] = dscr("VT" + sk, [RWW, Tn], BF16)
        dd["gC"] = [dscr("gC%d%s" % (d, sk), [RWW, Tn // CH], F32) for d in range(2)]
        seqs[sk] = dd
    bonT = dscr("bonT", [RWW, T], F32); grwT = dscr("grwT", [RWW, T], BF16)
    P.E("pool", "memset", blk64[:], 0.0, W=[blk64])
    P.E("pool", "memset", blk64[0:64, 0:64], 1.0, W=[blk64])
    P.E("pool", "memset", blk64[64:128, 64:128], 1.0, W=[blk64])
    with contextlib.ExitStack() as ph:
        def sb(name, shape, dt):
            return ph.enter_context(nc.sbuf_tensor(name, list(shape), dt))

        def psum(name, shape, dt):
            P.excl.add(name)
            return ph.enter_context(nc.psum_tensor(name, list(shape), dt))
        NK = 512
        muc = sb("H_mu", [128, 14, 2], F32); c0c = sb("H_c0", [128, 14], F32)
        wa = sb("H_wa", [128, 4, 4], F32); rc = sb("H_rc", [128, 4, 5], F32)
        UPb = sb("H_up", [128, 2, 512], BF16); gup = sb("H_gup", [128, 512], BF16)
        rmask = sb("H_rmask", [128, NK], F32); nhb = sb("H_nhb", [128, NK], F32)
        P.DMA("sp", muc[:], mucol[:, :, :], W=[muc]); P.DMA("sp", wa[:], rw_w0a0[:, :, :], W=[wa])
        P.DMA("sp", rc[:], rwc[:, :, :], W=[rc])
        P.DMA("pool", UPb[:], rw_up[:, :, :], W=[UPb]); P.DMA("pool", gup[:], rw_g_up[:, :], W=[gup])
        P.E("pool", "memset", rmask[:], 1.0, W=[rmask])
        P.E("pool", "memset", rmask[:].rearrange("p (c t) -> p c t", t=CH)[:, :, 0:1], 0.0, W=[rmask])
        P.E("pool", "memset", nhb[:], -0.5, W=[nhb])
        P.E("dve", "tensor_tensor", c0c[:], muc[:, :, 0], muc[:, :, 1], ALU.add, R=[muc], W=[c0c])
        P.E("dve", "tensor_scalar", c0c[:], c0c[:], -1.0, 1.0, ALU.mult, ALU.add, R=[c0c], W=[c0c])

        raw_l = [sb("H_rawl%d" % i, [128, NK + 2], BF16) for i in range(2)]
        raw_t = [sb("H_rawt%d" % i, [128, NK + 2], BF16) for i in range(4)]
        m12 = sb("H_m12", [128, NK], F32); m13 = sb("H_m13", [128, NK], F32)
        lwa = sb("H_lwa", [128, NK], BF16); lgs = sb("H_lgs", [128, NK], BF16)
        CT = [[sb("H_%s%d" % (nm, i), [128, NK], F32) for nm in ("rm", "km", "vm", "kkr", "sq", "ssb", "kk")] for i in range(2)]
        sg_ = [sb("H_sg%d" % i, [128, NK], F32) for i in range(2)]; ad_ = [sb("H_ad%d" % i, [128, NK], F32) for i in range(2)]
        cs_ = [sb("H_cs%d" % i, [128, NK], F32) for i in range(2)]; ce_ = [sb("H_ce%d" % i, [128, NK], F32) for i in range(2)]
        ci_ = [sb("H_ci%d" % i, [128, NK], F32) for i in range(2)]
        EA_ = [sb("H_EA%d" % i, [128, NK], F32) for i in range(2)]; EI_ = [sb("H_EI%d" % i, [128, NK], F32) for i in range(2)]
        EN_ = [sb("H_EN%d" % i, [128, NK], F32) for i in range(2)]
        kb_ = [sb("H_kb%d" % i, [128, NK], F32) for i in range(2)]; tt_ = [sb("H_tt%d" % i, [128, NK], F32) for i in range(2)]
        kmod_ = [sb("H_kmod%d" % i, [128, NK], F32) for i in range(2)]
        rk_ = [sb("H_rk%d" % i, [128, NK], F32) for i in range(2)]; bon = sb("H_bon", [128, NK], F32)
        gct = [sb("H_gct%d" % i, [128, 8], F32) for i in range(2)]
        ob = {nm: [sb("H_o%s%d" % (nm, i), [128, NK], BF16) for i in range(2)] for nm in ("Af", "Bf", "Kf", "Rf", "V", "g")}
        ps_w = [psum("H_pw%d" % i, [128, NK], F32) for i in range(2)]
        ps_a = [psum("H_pa%d" % i, [128, NK], F32) for i in range(2)]
        ps_ss = psum("H_pss", [128, NK], F32); ps_g = psum("H_pg", [128, NK], F32); ps_bon = psum("H_pbon", [128, NK], F32)
        cn = {"o": 0, "l": 0, "t": 0}

        def load_mix(src, row, chunk14, t0, N, Tn, raw, out, func=None):
            lo = max(t0 - 1, 0); hi = min(t0 + N + 1, Tn)
            P.DMA("sp", raw[:, lo - (t0 - 1):hi - (t0 - 1)], src[row:row + 128, lo:hi], W=[raw])
            if t0 == 0:
                P.E("pool", "memset", raw[:, 0:1], 0.0, W=[raw])
            if hi == Tn:
                P.E("pool", "memset", raw[:, N + 1:N + 2], 0.0, W=[raw])
            P.E("act", "activation", out[:, 0:N], raw[:, 1:N + 1], AF.Identity, scale=c0c[:, chunk14:chunk14 + 1], R=[raw, c0c], W=[out])
            P.E("dve", "scalar_tensor_tensor", out[:, 0:N], raw[:, 0:N], muc[:, chunk14, 0:1], out[:, 0:N], ALU.mult, ALU.add,
                R=[raw, muc, out], W=[out])
            P.E("dve", "scalar_tensor_tensor", out[:, 0:N], raw[:, 2:N + 2], muc[:, chunk14, 1:2], out[:, 0:N], ALU.mult, ALU.add,
                R=[raw, muc, out], W=[out])

        def rw_prep(src, row0, sk):
            dd = seqs[sk]; Tn = dd["T"]; main = sk == "m"
            for blk in range((Tn + NK - 1) // NK):
                t0 = blk * NK; N = min(NK, Tn - t0); nch = N // CH
                tsl = slice(t0, t0 + N)
                load_mix(src, row0 + 1536, 12, t0, N, Tn, raw_l[0], m12)
                P.E("act", "activation", lwa[0:64, 0:N], m12[0:64, 0:N], AF.Tanh, R=[m12], W=[lwa])
                P.E("dve", "tensor_copy", lwa[64:128, 0:N], m12[64:128, 0:N], R=[m12], W=[lwa])
                if main:
                    load_mix(src, row0 + 1664, 13, t0, N, Tn, raw_l[1], m13)
                    P.E("act", "activation", lgs[:, 0:N], m13[:, 0:N], AF.Sigmoid, R=[m13], W=[lgs])
                def common_gen(hp, par):
                    rm, km, vm, kkr, sq, ssb, kk = CT[par]
                    rsl = slice(hp * 128, (hp + 1) * 128)
                    if main:
                        load_mix(src, row0 + hp * 128, hp, t0, N, Tn, raw_t[0], rm)
                    load_mix(src, row0 + 512 + hp * 128, 4 + hp, t0, N, Tn, raw_t[1], km)
                    yield
                    load_mix(src, row0 + 1024 + hp * 128, 8 + hp, t0, N, Tn, raw_t[2], vm)
                    o = cn["o"] % 2; cn["o"] += 1
                    P.E("act", "activation", ob["V"][o][:, 0:N], vm[:, 0:N], AF.Copy, R=[vm], W=[ob["V"][o]])
                    P.DMA("sp", dd["V"][rsl, tsl], ob["V"][o][:, 0:N], R=[ob["V"][o]])
                    yield
                    P.E("dve", "tensor_scalar", kkr[:, 0:N], km[:, 0:N], rc[:, hp, 0:1], None, ALU.mult, R=[km, rc], W=[kkr])
                    P.E("act", "activation", sq[:, 0:N], kkr[:, 0:N], AF.Square, R=[kkr], W=[sq])
                    yield
                    P.E("pe", "matmul", ps_ss[:, 0:N], blk64[:], sq[:, 0:N], start=True, stop=True, R=[blk64, sq], W=[ps_ss])
                    P.E("dve", "tensor_scalar", ssb[:, 0:N], ps_ss[:, 0:N], 1e-24, None, ALU.max, R=[ps_ss], W=[ssb])
                    P.E("act", "activation", ssb[:, 0:N], ssb[:, 0:N], AF.Sqrt, R=[ssb], W=[ssb])
                    yield
                    P.E("dve", "reciprocal", ssb[:, 0:N], ssb[:, 0:N], R=[ssb], W=[ssb])
                    P.E("dve", "tensor_tensor", kk[:, 0:N], kkr[:, 0:N], ssb[:, 0:N], ALU.mult, R=[kkr, ssb], W=[kk])
                    if main:
                        P.E("pe", "matmul", ps_g[:, 0:N], gup[:, rsl], lgs[:, 0:N], start=True, stop=True, R=[gup, lgs], W=[ps_g])
                        P.E("act", "activation", ob["g"][o][:, 0:N], ps_g[:, 0:N], AF.Copy, R=[ps_g], W=[ob["g"][o]])
                        P.DMA("sp", grwT[rsl, tsl], ob["g"][o][:, 0:N], R=[ob["g"][o]])
                    yield
                for _ in common_gen(0, 0):
                    pass
                for hp in range(4):
                    par = hp % 2
                    rsl = slice(hp * 128, (hp + 1) * 128)
                    o = 0
                    def dgen(d, hp=hp, N=N, nch=nch, rsl=rsl, tsl=tsl, t0=t0, o=o, par=par):
                        pw = ps_w[d]; pa = ps_a[d]
                        rm, km, vm, kkr, sq, ssb, kk = CT[par]
                        P.E("pe", "matmul", pw[:, 0:N], UPb[0:64, d, rsl], lwa[0:64, 0:N], start=True, stop=True, R=[UPb, lwa], W=[pw])
                        P.E("pe", "matmul", pa[:, 0:N], UPb[64:128, d, rsl], lwa[64:128, 0:N], start=True, stop=True, R=[UPb, lwa], W=[pa])
                        yield
                        P.E("act", "activation", sg_[d][:, 0:N], pw[:, 0:N], AF.Sigmoid, bias=wa[:, hp, d:d + 1], R=[pw, wa], W=[sg_[d]])
                        P.E("act", "activation", ad_[d][:, 0:N], pa[:, 0:N], AF.Sigmoid, bias=wa[:, hp, 2 + d:3 + d], R=[pa, wa], W=[ad_[d]])
                        yield
                        P.E("dve", "tensor_tensor_scan", cs_[d][:, 0:N], rmask[:, 0:N], sg_[d][:, 0:N], 0.0, ALU.mult, ALU.add, R=[rmask, sg_[d]], W=[cs_[d]])
                        if d == 0:
                            cis = cs_[d]
                            P.E("dve", "tensor_tensor", ce_[d][:, 0:N], cs_[d][:, 0:N], sg_[d][:, 0:N], ALU.subtract, R=[cs_[d], sg_[d]], W=[ce_[d]])
                        else:
                            cis = ci_[d]
                            tot = bc(cs_[d][:, CH - 1:CH], [[CH, nch], [0, CH]])
                            P.E("dve", "tensor_tensor", ce_[d][:, 0:N].rearrange("p (c t) -> p c t", t=CH), tot,
                                cs_[d][:, 0:N].rearrange("p (c t) -> p c t", t=CH), ALU.subtract, R=[cs_[d]], W=[ce_[d]])
                            P.E("dve", "tensor_tensor", ci_[d][:, 0:N], ce_[d][:, 0:N], sg_[d][:, 0:N], ALU.add, R=[ce_[d], sg_[d]], W=[ci_[d]])
                        g_ = gct[d]
                        P.E("act", "activation", g_[:, 0:nch], bc(cs_[d][:, CH - 1:CH], [[CH, nch]]), AF.Exp, scale=-C0, R=[cs_[d]], W=[g_])
                        P.DMA("sp", dd["gC"][d][rsl, t0 // CH:t0 // CH + nch], g_[:, 0:nch], R=[g_])
                        yield
                        P.E("act", "activation", EA_[d][:, 0:N], ce_[d][:, 0:N], AF.Exp, scale=-C0, R=[ce_[d]], W=[EA_[d]])
                        P.E("act", "activation", EN_[d][:, 0:N], cis[:, 0:N], AF.Exp, scale=C0, R=[cis], W=[EN_[d]])
                        yield
                        if main:
                            P.E("act", "activation", EI_[d][:, 0:N], cis[:, 0:N], AF.Exp, scale=-C0, R=[cis], W=[EI_[d]])
                        oA, oB, oK, oR = ob["Af"][d], ob["Bf"][d], ob["Kf"][d], ob["Rf"][d]
                        P.E("dve", "scalar_tensor_tensor", oA[:, 0:N], kk[:, 0:N], -1.0, EA_[d][:, 0:N], ALU.mult, ALU.mult, R=[kk, EA_[d]], W=[oA])
                        P.E("dve", "tensor_tensor", kb_[d][:, 0:N], kk[:, 0:N], ad_[d][:, 0:N], ALU.mult, R=[kk, ad_[d]], W=[kb_[d]])
                        P.E("dve", "tensor_tensor", oB[:, 0:N], kb_[d][:, 0:N], EN_[d][:, 0:N], ALU.mult, R=[kb_[d], EN_[d]], W=[oB])
                        P.E("dve", "tensor_scalar", tt_[d][:, 0:N], ad_[d][:, 0:N], -1.0, rc[:, hp, 1:2], ALU.add, ALU.mult, R=[ad_[d], rc], W=[tt_[d]])
                        P.E("dve", "scalar_tensor_tensor", kmod_[d][:, 0:N], tt_[d][:, 0:N], 1.0, km[:, 0:N], ALU.add, ALU.mult, R=[tt_[d], km], W=[kmod_[d]])
                        P.E("dve", "tensor_tensor", oK[:, 0:N], kmod_[d][:, 0:N], EN_[d][:, 0:N], ALU.mult, R=[kmod_[d], EN_[d]], W=[oK])
                        P.DMA("sp", dd["Af"][d][rsl, tsl], oA[:, 0:N], R=[oA])
                        P.DMA("sp", dd["Bf"][d][rsl, tsl], oB[:, 0:N], R=[oB])
                        P.DMA("sp", dd["Kf"][d][rsl, tsl], oK[:, 0:N], R=[oK])
                        if main:
                            P.E("dve", "tensor_tensor", oR[:, 0:N], rm[:, 0:N], EI_[d][:, 0:N], ALU.mult, R=[rm, EI_[d]], W=[oR])
                            P.DMA("sp", dd["Rf"][d][rsl, tsl], oR[:, 0:N], R=[oR])
                            P.E("dve", "scalar_tensor_tensor", rk_[d][:, 0:N], rm[:, 0:N], rc[:, hp, 2:3], kmod_[d][:, 0:N], ALU.mult, ALU.mult,
                                R=[rm, rc, kmod_[d]], W=[rk_[d]])
                            P.E("pe", "matmul", ps_bon[:, 0:N], blk64[:], rk_[d][:, 0:N], start=(d == 0), stop=(d == 1), R=[blk64, rk_[d]], W=[ps_bon])
                    gens = [dgen(0), dgen(1)] + ([common_gen(hp + 1, 1 - par)] if hp + 1 < 4 else [])
                    alive = True
                    while alive:
                        alive = False
                        for g_ in gens:
                            try:
                                next(g_); alive = True
                            except StopIteration:
                                pass
                    vm = CT[par][2]
                    if main:
                        P.E("dve", "tensor_tensor", bon[:, 0:N], ps_bon[:, 0:N], vm[:, 0:N], ALU.mult, R=[ps_bon, vm], W=[bon])
                        P.DMA("sp", bonT[rsl, tsl], bon[:, 0:N], R=[bon])

        rw_prep(ccT, 0, "c")
        rw_prep(colsT, 1536, "m")
        P.flush()
    if upto <= 7:
        P.flush(final=True); semctx.close(); G2.close(); G.close(); return nc

    yT = [dscr("yT%d" % d, [RWW, T], F32) for d in range(2)]
    SCH = 8
    with contextlib.ExitStack() as ph:
        def sb(name, shape, dt):
            return ph.enter_context(nc.sbuf_tensor(name, list(shape), dt))

        def psum(name, shape, dt):
            P.excl.add(name)
            return ph.enter_context(nc.psum_tensor(name, list(shape), dt))
        ones_f = sb("I_ones", [128, 256], F32)
        P.E("pool", "memset", ones_f[:], 1.0, W=[ones_f])
        MA = [sb("I_MA%d" % d, [128, 256], F32) for d in range(2)]
        MB = [sb("I_MB%d" % d, [128, 256], F32) for d in range(2)]
        MC = [sb("I_MC%d" % d, [128, 2, 64], F32) for d in range(2)]

        def asel(out, inp, pat, cm, cmp, res):
            P.E("pool", "affine_select", out, inp, pat, cmp, 0.0, base=0, channel_multiplier=cm, R=[ones_f], W=[res])
        for d in range(2):
            pf, cf = ([[1, 128]], -1) if d == 0 else ([[-1, 128]], 1)
            pr, cr = ([[-1, 128]], 1) if d == 0 else ([[1, 128]], -1)
            asel(MA[d][:, 0:128], ones_f[:, 0:128], pf, cf, ALU.is_gt, MA[d])
            asel(MA[d][:, 128:256], ones_f[:, 0:128], pr, cr, ALU.is_gt, MA[d])
            P.E("pool", "memset", MB[d][:, 0:128], 1.0, W=[MB[d]])
            asel(MB[d][:, 128:256], ones_f[:, 0:128], pr, cr, ALU.is_gt, MB[d])
            pi_, ci_ = ([[1, 64]], -1) if d == 0 else ([[-1, 64]], 1)
            for half in range(2):
                for w_ in range(2):
                    asel(MC[d][half * 64:(half + 1) * 64, w_, :], ones_f[half * 64:(half + 1) * 64, 0:64], pi_, ci_, ALU.is_ge, MC[d])

        NS = SCH * CH
        lanes = []
        for l in range(4):
            L_ = {"d": l % 2}
            L_["st"] = [{nm: sb("I_st%s%d_%d" % (nm, l, i), [128, NS], BF16) for nm in ("Rf",)} for i in range(2)]
            L_["bd"] = [{nm: sb("I_bd%s%d_%d" % (nm, l, i), [128, SCH, 128], BF16) for nm in ("Af", "Bf", "Kf", "V")} for i in range(2)]
            L_["gc"] = [sb("I_gc%d_%d" % (l, i), [128, SCH], F32) for i in range(2)]
            L_["yb"] = [sb("I_yb%d_%d" % (l, i), [128, NS], F32) for i in range(2)]
            L_["XX"] = [sb("I_XX%d_%d" % (l, i), [128, 2, 128], BF16) for i in range(2)]
            L_["YY"] = [sb("I_YY%d_%d" % (l, i), [128, 2, 128], BF16) for i in range(2)]
            L_["TM"] = [sb("I_TM%d_%d" % (l, i), [128, 3, 128], BF16) for i in range(2)]
            L_["BRKR"] = [sb("I_BK%d_%d" % (l, i), [128, 2, 64], BF16) for i in range(2)]
            L_["GQ"] = [sb("I_GQ%d_%d" % (l, i), [128, 192], BF16) for i in range(2)]
            L_["HZ"] = [sb("I_HZ%d_%d" % (l, i), [128, 192], BF16) for i in range(2)]
            L_["S"] = [sb("I_S%d_%d" % (l, i), [128, 128], BF16) for i in range(2)]
            L_["ps"] = [psum("I_ps%d_%d" % (l, i), [128, 512], F32) for i in range(2)]
            for i in range(2):
                for nm in ("Af", "Bf", "Kf", "V"):
                    P.E("pool", "memset", L_["bd"][i][nm][:], 0.0, W=[L_["bd"][i][nm]])
            lanes.append(L_)

        def load_super(L_, hp, sk, s_idx, setidx):
            dd = seqs[sk]; d = L_["d"]; main = sk == "m"
            Tn = dd["T"]; N = min(NS, Tn); nch = N // CH
            t0 = s_idx * NS
            rsl = slice(hp * 128, (hp + 1) * 128)
            st = L_["st"][setidx]; bd = L_["bd"][setidx]
            for nm in ("Af", "Bf", "Kf", "V"):
                srcT = dd["V"] if nm == "V" else dd[nm][d]
                for half in range(2):
                    r0 = hp * 128 + half * 64
                    P.DMA("sp", bd[nm][half * 64:(half + 1) * 64, 0:nch, half * 64:(half + 1) * 64],
                          srcT[r0:r0 + 64, t0:t0 + N].rearrange("p (c t) -> p c t", t=CH), W=[bd[nm]])
            if main:
                P.DMA("sp", st["Rf"][:, 0:N], dd["Rf"][d][rsl, t0:t0 + N], W=[st["Rf"]])
            P.DMA("sp", L_["gc"][setidx][:, 0:nch], dd["gC"][d][rsl, t0 // CH:t0 // CH + nch], W=[L_["gc"][setidx]])

        cstate = [{"n": 0, "s": 0} for _ in range(4)]

        def chunk_gen(L_, l, sk, setidx, c, main):
            d = L_["d"]; cs_ = cstate[l]
            par = cs_["n"] % 2; cs_["n"] += 1
            st = L_["st"][setidx]; bd = L_["bd"][setidx]
            psA, psB = L_["ps"]
            rA0 = rA1 = psA; rB0 = rB1 = psB; rC1 = psA; rC0 = psA
            Af = bd["Af"][:, c, :]; Bf = bd["Bf"][:, c, :]; Kf = bd["Kf"][:, c, :]; Vf = bd["V"][:, c, :]
            Rfc = st["Rf"][:, c * CH:(c + 1) * CH]
            XX = L_["XX"]; YY = L_["YY"]; TM = L_["TM"][par]; BRKR = L_["BRKR"][par]; GQ = L_["GQ"][par]; HZ = L_["HZ"][par]
            mm = lambda out, lhsT, rhs, st_, sp_, R, W: P.E("pe", "matmul", out, lhsT, rhs, start=st_, stop=sp_, R=R, W=W)
            mm(psA[:, 0:128], Bf, Af, True, True, [bd["Bf"], bd["Af"]], [rA0])
            mm(psA[:, 128:256], Af, Bf, True, True, [bd["Bf"], bd["Af"]], [rA0])
            mm(psA[:, 256:384], Af, identb[:], True, True, [bd["Af"], identb], [rA1])
            mm(psA[:, 384:512], Af, Kf, True, True, [bd["Af"], bd["Kf"]], [rA1])
            if main:
                mm(psB[:, 0:64], Bf, Rfc, True, True, [bd["Bf"], st["Rf"]], [rB0])
                mm(psB[:, 64:128], Kf, Rfc, True, True, [bd["Kf"], st["Rf"]], [rB0])
            mm(psB[:, 128:256], Bf, identb[:], True, True, [bd["Bf"], identb], [rB1])
            mm(psB[:, 256:384], Kf, identb[:], True, True, [bd["Kf"], identb], [rB1])
            mm(psB[:, 384:512], Vf, identb[:], True, True, [bd["V"], identb], [rB1])
            x0 = XX[0]; y0 = YY[0]
            P.E("dve", "tensor_tensor", x0[:].rearrange("p a b -> p (a b)"), psA[:, 0:256], MA[d][:], ALU.mult, R=[rA0, MA[d]], W=[x0])
            P.E("dve", "tensor_tensor", y0[:].rearrange("p a b -> p (a b)"), psA[:, 256:512], MB[d][:], ALU.mult, R=[rA1, MB[d]], W=[y0])
            if main:
                P.E("dve", "tensor_tensor", BRKR[:].rearrange("p a b -> p (a b)"), psB[:, 0:128], MC[d][:].rearrange("p a b -> p (a b)"),
                    ALU.mult, R=[rB0, MC[d]], W=[BRKR])
            P.E("act", "activation", TM[:].rearrange("p a b -> p (a b)"), psB[:, 128:512], AF.Copy, R=[rB1], W=[TM])
            yield
            for k in range(6):
                xk = XX[k % 2]; xn = XX[(k + 1) % 2]; yk = YY[k % 2]; yn = YY[(k + 1) % 2]
                ykf = yk[:].rearrange("p a b -> p (a b)")
                mm(psA[:, 0:256], xk[:, 0, :], ykf, True, k < 5, [xk, yk], [rC1])
                if k == 5:
                    mm(psA[:, 0:256], identb[:], ykf, False, True, [identb, yk], [rC1])
                if k < 5:
                    mm(psA[:, 256:384], xk[:, 1, :], xk[:, 0, :], True, True, [xk], [rC0])
                    if k < 4:
                        mm(psA[:, 384:512], xk[:, 0, :], xk[:, 1, :], True, True, [xk], [rC0])
                if k < 5:
                    P.E("dve", "tensor_tensor", yn[:].rearrange("p a b -> p (a b)"), psA[:, 0:256], ykf, ALU.add, R=[rC1, yk], W=[yn])
                else:
                    P.E("act", "activation", yn[:].rearrange("p a b -> p (a b)"), psA[:, 0:256], AF.Copy, R=[rC1], W=[yn])
                if k < 5:
                    w_ = 256 if k < 4 else 128
                    P.E("act", "activation", xn[:].rearrange("p a b -> p (a b)")[:, 0:w_], psA[:, 256:256 + w_], AF.Copy, R=[rC0], W=[xn])
                yield
            yf = YY[0]
            ApT = yf[:, 0, :]; WT = yf[:, 1, :]
            Bt = TM[:, 0, :]; Kt = TM[:, 1, :]; Vt = TM[:, 2, :]
            mm(psB[:, 0:128], ApT, Bt, True, False, [yf, TM], [psB])
            mm(psB[:, 0:128], identb[:], identb[:], False, True, [identb], [psB])
            if main:
                mm(psB[:, 128:192], ApT, BRKR[:, 0, :], True, False, [yf, BRKR], [psB])
                mm(psB[:, 128:192], identb[:], Rfc, False, True, [identb, st["Rf"]], [psB])
            mm(psB[:, 192:320], WT, Bt, True, False, [yf, TM], [psB])
            mm(psB[:, 192:320], identb[:], Kt, False, True, [identb, TM], [psB])
            if main:
                mm(psB[:, 320:384], WT, BRKR[:, 0, :], True, False, [yf, BRKR], [psB])
                mm(psB[:, 320:384], identb[:], BRKR[:, 1, :], False, True, [identb, BRKR], [psB])
            wq = 192 if main else 128
            P.E("act", "activation", GQ[:, 0:wq], psB[:, 0:wq], AF.Copy, R=[psB], W=[GQ])
            P.E("dve", "tensor_copy", HZ[:, 0:wq], psB[:, 192:192 + wq], R=[psB], W=[HZ])
            yield
            Sc = L_["S"][cs_["s"] % 2]; Sn = L_["S"][(cs_["s"] + 1) % 2]; cs_["s"] += 1
            if main:
                mm(psA[:, 0:64], Sc[:], GQ[:, 128:192], True, False, [Sc, GQ], [psA])
                mm(psA[:, 0:64], Vt, HZ[:, 128:192], False, True, [TM, HZ], [psA])
                yb = L_["yb"][setidx]
                P.E("dve", "tensor_copy", yb[:, c * CH:(c + 1) * CH], psA[:, 0:64], R=[psA], W=[yb])
            mm(psB[:, 384:512], GQ[:, 0:128], Sc[:], True, False, [GQ, Sc], [psB])
            mm(psB[:, 384:512], HZ[:, 0:128], Vt, False, True, [HZ, TM], [psB])
            P.E("act", "activation", Sn[:], psB[:, 384:512], AF.Identity, scale=L_["gc"][setidx][:, c:c + 1], R=[psB, L_["gc"][setidx]], W=[Sn])
            yield

        import os
        for hpp in range(int(os.environ.get('SCAN_NHP', 2))):
            sched = []
            for l in range(4):
                items = []
                nsup = T // NS
                if l % 2 == 0:
                    items.append(("c", 0, list(range(TC // CH))))
                    for s_ in range(nsup):
                        items.append(("m", s_, list(range(SCH))))
                else:
                    items.append(("c", 0, list(range(TC // CH - 1, -1, -1))))
                    for s_ in range(nsup - 1, -1, -1):
                        items.append(("m", s_, list(range(SCH - 1, -1, -1))))
                sched.append(items)
                cstate[l]["s"] = 0
                P.E("pool", "memset", lanes[l]["S"][0][:], 0.0, W=[lanes[l]["S"][0]])
            hps = [2 * hpp + l // 2 for l in range(4)]
            nitems = min(len(sched[0]), int(os.environ.get('SCAN_NITEMS', 999)))
            for l in range(4):
                load_super(lanes[l], hps[l], sched[l][0][0], sched[l][0][1], 0)
            for it_ in range(nitems):
                setidx = it_ % 2
                if it_ + 1 < nitems:
                    for l in range(4):
                        load_super(lanes[l], hps[l], sched[l][it_ + 1][0], sched[l][it_ + 1][1], (it_ + 1) % 2)
                nchunks = len(sched[0][it_][2])
                for ci_ in range(nchunks):
                    gens = []
                    for l in range(4):
                        sk, s_idx, order = sched[l][it_]
                        gens.append(chunk_gen(lanes[l], l, sk, setidx, order[ci_], sk == "m"))
                    alive = True
                    while alive:
                        alive = False
                        for g_ in gens:
                            try:
                                next(g_); alive = True
                            except StopIteration:
                                pass
                for l in range(4):
                    sk, s_idx, order = sched[l][it_]
                    if sk == "m":
                        rsl = slice(hps[l] * 128, (hps[l] + 1) * 128)
                        P.DMA("sp", yT[l % 2][rsl, s_idx * NS:(s_idx + 1) * NS], lanes[l]["yb"][setidx][:], R=[lanes[l]["yb"][setidx]])
        P.flush()
    if upto <= 8:
        P.flush(final=True); semctx.close(); G2.close(); G.close(); return nc

    xe = [dscr("xe%d" % e, [CAP, ROWP], BF16) for e in range(NE)]; acc = dscr("acc", [T, D], F32)
    rwoT = dscr("rwoT", [RWW, T], BF16)
    with contextlib.ExitStack() as ph:
        def sb(name, shape, dt):
            return ph.enter_context(nc.sbuf_tensor(name, list(shape), dt))

        def psum(name, shape, dt):
            P.excl.add(name)
            return ph.enter_context(nc.psum_tensor(name, list(shape), dt))
        NK = 512
        rc = sb("J_rc", [128, 4, 5], F32); nhb = sb("J_nhb", [128, NK], F32)
        P.DMA("sp", rc[:], rwc[:, :, :], W=[rc]); P.E("pool", "memset", nhb[:], -0.5, W=[nhb])
        zb = sb("K_zb", [128, 8 * ROWP], BF16); zf = sb("K_zf", [128, 4096], F32)
        P.E("pool", "memset", zb[:], 0.0, W=[zb]); P.E("pool", "memset", zf[:], 0.0, W=[zf])
        acc_flat = acc.rearrange("t d -> (t d)").rearrange("(p q) -> p q", p=128)
        for i in range(16):
            P.DMA("act", xe[i].rearrange("s r -> (s r)").rearrange("(p q) -> p q", p=128), zb[:], R=[zb])
            P.DMA("act", acc_flat[:, i * 4096:(i + 1) * 4096], zf[:], R=[zf])

        pm = psum("J_pm", [128, NK], F32); pq2 = psum("J_pq", [128, NK], F32)
        NR = 3
        y0s = [sb("J_y0r%d" % i, [128, NK], F32) for i in range(NR)]; y1s = [sb("J_y1r%d" % i, [128, NK], F32) for i in range(NR)]
        bns = [sb("J_bnr%d" % i, [128, NK], F32) for i in range(NR)]; gts = [sb("J_gtr%d" % i, [128, NK], BF16) for i in range(NR)]
        outs = [sb("J_outr%d" % i, [128, NK], BF16) for i in range(NR)]
        sqs = [sb("J_sqr%d" % i, [128, NK], F32) for i in range(NR)]; mns = [sb("J_mnr%d" % i, [128, NK], F32) for i in range(NR)]
        msqs = [sb("J_msqr%d" % i, [128, NK], F32) for i in range(NR)]; vars_ = [sb("J_varr%d" % i, [128, NK], F32) for i in range(NR)]
        yns = [sb("J_ynr%d" % i, [128, NK], F32) for i in range(NR)]
        nbj = T // NK

        def j_gen(it):
            hp = it // nbj; blk = it % nbj; b_ = it % NR
            sl_ = (slice(hp * 128, (hp + 1) * 128), slice(blk * NK, (blk + 1) * NK))
            y0, y1, bn, gt, ot = y0s[b_], y1s[b_], bns[b_], gts[b_], outs[b_]
            sq, mn, msq, var, yn = sqs[b_], mns[b_], msqs[b_], vars_[b_], yns[b_]
            P.DMA("sp", y0[:], yT[0][sl_], W=[y0]); P.DMA("sp", y1[:], yT[1][sl_], W=[y1])
            P.DMA("sp", bn[:], bonT[sl_], W=[bn]); P.DMA("sp", gt[:], grwT[sl_], W=[gt])
            yield
            P.E("dve", "tensor_tensor", y0[:], y0[:], y1[:], ALU.add, R=[y0, y1], W=[y0])
            yield
            P.E("pe", "matmul", pm[:], blk64[:], y0[:], start=True, stop=True, R=[blk64, y0], W=[pm])
            P.E("act", "activation", sq[:], y0[:], AF.Square, R=[y0], W=[sq])
            yield
            P.E("pe", "matmul", pq2[:], blk64[:], sq[:], start=True, stop=True, R=[blk64, sq], W=[pq2])
            P.E("act", "activation", mn[:], pm[:], AF.Copy, scale=1.0 / 64, R=[pm], W=[mn])
            P.E("act", "activation", msq[:], pm[:], AF.Square, scale=1.0 / 64, R=[pm], W=[msq])
            yield
            P.E("dve", "scalar_tensor_tensor", var[:], pq2[:], 1.0 / 64, msq[:], ALU.mult, ALU.subtract, R=[pq2, msq], W=[var])
            P.E("dve", "tensor_scalar", var[:], var[:], LN_EPS, None, ALU.add, R=[var], W=[var])
            yield
            P.E("act", "activation", var[:], var[:], AF.Sqrt, R=[var], W=[var])
            yield
            P.E("dve", "reciprocal", var[:], var[:], R=[var], W=[var])
            P.E("dve", "tensor_tensor", yn[:], y0[:], mn[:], ALU.subtract, R=[y0, mn], W=[yn])
            P.E("dve", "tensor_tensor", yn[:], yn[:], var[:], ALU.mult, R=[yn, var], W=[yn])
            yield
            P.E("act", "activation", yn[:], yn[:], AF.Identity, scale=rc[:, hp, 3:4], bias=rc[:, hp, 4:5], R=[yn, rc], W=[yn])
            yield
            P.E("dve", "tensor_tensor", yn[:], yn[:], bn[:], ALU.add, R=[yn, bn], W=[yn])
            P.E("dve", "tensor_tensor", ot[:], yn[:], gt[:], ALU.mult, R=[yn, gt], W=[ot])
            P.DMA("sp", rwoT[sl_], ot[:], R=[ot])
            yield
        run_pipe(j_gen, 4 * nbj, NR)
        P.flush()
    if upto <= 9:
        P.flush(final=True); semctx.close(); G2.close(); G.close(); return nc

    x1d = dscr("x1d", [T, D], F32); h2p = dscr("h2p", [T, ROWP], BF16)
    affd = dscr("affd", [T, NE], F32); affTd = dscr("affTd", [NE, T], F32)
    P.E("pool", "iota", pidx[:], [[0, 1]], base=0, channel_multiplier=1, allow_small_or_imprecise_dtypes=True, W=[pidx])
    with contextlib.ExitStack() as ph:
        def sb(name, shape, dt):
            return ph.enter_context(nc.sbuf_tensor(name, list(shape), dt))

        def psum(name, shape, dt):
            P.excl.add(name)
            return ph.enter_context(nc.psum_tensor(name, list(shape), dt))
        hpj = sb("K_hpj", [128, 4, D], BF16); rpj = sb("K_rpj", [128, 4, D], BF16); wo = sb("K_wo", [128, 8, D], BF16)
        rtw = sb("K_rtw", [128, 8, NE], F32)
        for c in range(4):
            P.DMA("pool", hpj[:, c, :], hy_proj[c * 128:(c + 1) * 128, :], W=[hpj])
            P.DMA("pool", rpj[:, c, :], rw_proj[c * 128:(c + 1) * 128, :], W=[rpj])
        for k in range(8):
            P.DMA("pool", wo[:, k, :], w_out[k * 128:(k + 1) * 128, :], W=[wo])
        P.DMA("sp", rtw[:], router_w.rearrange("(k p) e -> p k e", p=128), W=[rtw])
        hyb = [sb("K_hy%d" % i, [128, 4, 512], BF16) for i in range(2)]
        rwb = [sb("K_rw%d" % i, [128, 4, 512], BF16) for i in range(2)]
        gtb = [sb("K_gt0", [128, 16, 512], BF16)] * 2
        m1 = sb("K_m1", [128, 512], F32); m2 = sb("K_m2", [128, 512], F32)
        mp = [sb("K_mp%d" % i, [128, 8, 512], BF16) for i in range(2)]
        xts = [sb("K_x%d" % i, [128, D], F32) for i in range(2)]
        t1r = [sb("K_t1%d" % i, [128, D], F32) for i in range(2)]; x1tr = [sb("K_x1t%d" % i, [128, D], F32) for i in range(2)]
        h2tr = [sb("K_h2t%d" % i, [128, D], F32) for i in range(2)]
        junkr = [sb("K_junk%d" % i, [128, D], BF16) for i in range(2)]
        h2pt = [sb("K_h2p%d" % i, [128, ROWP], BF16) for i in range(2)]
        ssr = [sb("K_ss%d" % i, [128, 8], F32) for i in range(2)]; smr = [sb("K_sm%d" % i, [128, 4], F32) for i in range(2)]
        h2Tr = [sb("K_h2T%d" % i, [128, 8, 128], F32) for i in range(2)]; e16r = [sb("K_e16%d" % i, [128, NE], F32) for i in range(2)]
        affTs = [sb("K_affT%d" % i, [NE, 128], F32) for i in range(2)]
        ps_h = psum("K_ph", [128, 512], F32); ps_r = psum("K_pr", [128, 512], F32)
        ps_o = psum("K_po", [128, 1024], F32); pTr = psum("K_pTr", [128, 8, 128], F32)
        ps_l = psum("K_pl", [128, 512], F32); psT = psum("K_pT", [128, 512], F32)
        def proj_gen(blk):
            b_ = blk % 2
            tsl = slice(blk * 512, (blk + 1) * 512)
            P.DMA("sp", hyb[b_][:], hyT[:, tsl].rearrange("(c p) t -> p c t", p=128), W=[hyb[b_]])
            P.DMA("sp", rwb[b_][:], rwoT[:, tsl].rearrange("(c p) t -> p c t", p=128), W=[rwb[b_]])
            P.DMA("sp", gtb[b_][:], colsT[3328:5376, tsl].rearrange("(c p) t -> p c t", p=128), W=[gtb[b_]])
            for dc in range(8):
                for c in range(4):
                    P.E("pe", "matmul", ps_h[:], hpj[:, c, dc * 128:(dc + 1) * 128], hyb[b_][:, c, :], start=(c == 0), stop=(c == 3),
                        R=[hpj, hyb[b_]], W=[ps_h])
                for c in range(4):
                    P.E("pe", "matmul", ps_r[:], rpj[:, c, dc * 128:(dc + 1) * 128], rwb[b_][:, c, :], start=(c == 0), stop=(c == 3),
                        R=[rpj, rwb[b_]], W=[ps_r])
                P.E("dve", "tensor_tensor", m1[:], ps_h[:], gtb[b_][:, dc, :], ALU.mult, R=[ps_h, gtb[b_]], W=[m1])
                P.E("dve", "tensor_tensor", m2[:], ps_r[:], gtb[b_][:, 8 + dc, :], ALU.mult, R=[ps_r, gtb[b_]], W=[m2])
                P.E("dve", "tensor_tensor", mp[b_][:, dc, :], m1[:], m2[:], ALU.add, R=[m1, m2], W=[mp[b_]])
                yield
        def tile_gen(tau):
            if True:
                blk = tau // 4; i = tau % 4; b_ = blk % 2; r_ = tau % 2
                t1 = t1r[r_]; x1t = x1tr[r_]; h2t = h2tr[r_]; junkb = junkr[r_]; ss = ssr[r_]; sm = smr[r_]; h2T = h2Tr[r_]; e16 = e16r[r_]
                xt = xts[tau % 2]; hp_ = h2pt[tau % 2]
                rsl = slice(tau * 128, (tau + 1) * 128)
                P.DMA("sp", xt[:], x_in[rsl, :], W=[xt])
                for half in range(2):
                    for k in range(8):
                        P.E("pe", "matmul", ps_o[:, half * 512:(half + 1) * 512], mp[b_][:, k, i * 128:(i + 1) * 128],
                            wo[:, k, half * 512:(half + 1) * 512], start=(k == 0), stop=(k == 7), R=[mp[b_], wo], W=[ps_o])
                P.E("act", "activation", junkb[:], ps_o[:], AF.Square, accum_out=ss[:, 0:1], R=[ps_o], W=[junkb, ss])
                P.E("dve", "tensor_scalar", ss[:, 1:2], ss[:, 0:1], 1.0 / D, EPS, ALU.mult, ALU.add, R=[ss], W=[ss])
                P.E("pool", "tensor_tensor", ss[:, 2:3], ss[:, 1:2], nhalf[:], ALU.pow, R=[ss, nhalf], W=[ss])
                P.E("dve", "scalar_tensor_tensor", t1[:], ps_o[:], ss[:, 2:3], rows["C"][:], ALU.mult, ALU.mult, R=[ps_o, ss, rows["C"]], W=[t1])
                yield
                P.E("dve", "tensor_tensor", x1t[:], t1[:], xt[:], ALU.add, R=[t1, xt], W=[x1t])
                P.DMA("sp", x1d[rsl, :], x1t[:], R=[x1t])
                yield
                P.E("act", "activation", junkb[:], x1t[:], AF.Square, accum_out=ss[:, 3:4], R=[x1t], W=[junkb, ss])
                P.E("dve", "tensor_scalar", ss[:, 4:5], ss[:, 3:4], 1.0 / D, EPS, ALU.mult, ALU.add, R=[ss], W=[ss])
                P.E("pool", "tensor_tensor", ss[:, 5:6], ss[:, 4:5], nhalf[:], ALU.pow, R=[ss, nhalf], W=[ss])
                P.E("dve", "scalar_tensor_tensor", t1[:], x1t[:], ss[:, 5:6], rows["D"][:], ALU.mult, ALU.mult, R=[x1t, ss, rows["D"]], W=[t1])
                P.E("dve", "tensor_tensor", h2t[:], t1[:], rows["E"][:], ALU.add, R=[t1, rows["E"]], W=[h2t])
                yield
                P.E("act", "activation", hp_[:, 0:D], h2t[:], AF.Copy, R=[h2t], W=[hp_])
                P.E("pool", "memset", hp_[:, D:D + 1], float(tau), W=[hp_])
                P.E("pool", "tensor_copy", hp_[:, D + 1:D + 2], pidx[:], R=[pidx], W=[hp_])
                P.DMA("sp", h2p[rsl, :], hp_[:], R=[hp_])
                for k in range(8):
                    P.E("pe", "transpose", pTr[:, k, :], h2t[:, k * 128:(k + 1) * 128], identf[:], R=[h2t, identf], W=[pTr])
                P.E("act", "activation", h2T[:].rearrange("p a b -> p (a b)"), pTr[:].rearrange("p a b -> p (a b)"), AF.Copy, R=[pTr], W=[h2T])
                yield
                for k in range(8):
                    P.E("pe", "matmul", ps_l[:, 0:NE], h2T[:, k, :], rtw[:, k, :], start=(k == 0), stop=(k == 7), R=[h2T, rtw], W=[ps_l])
                P.E("dve", "reduce_max", sm[:, 0:1], ps_l[:, 0:NE], AX.X, R=[ps_l], W=[sm])
                P.E("dve", "tensor_scalar", sm[:, 1:2], sm[:, 0:1], -1.0, None, ALU.mult, R=[sm], W=[sm])
                P.E("act", "activation", e16[:], ps_l[:, 0:NE], AF.Exp, bias=sm[:, 1:2], accum_out=sm[:, 2:3], R=[ps_l, sm], W=[e16, sm])
                P.E("dve", "reciprocal", sm[:, 3:4], sm[:, 2:3], R=[sm], W=[sm])
                P.E("dve", "tensor_scalar", AFF[:, tau, :], e16[:], sm[:, 3:4], None, ALU.mult, R=[e16, sm], W=[AFF])
                yield
                P.DMA("sp", affd[rsl, :], AFF[:, tau, :], R=[AFF])
                P.E("pe", "transpose", psT[0:NE, 0:128], AFF[:, tau, :], identf[:], R=[AFF, identf], W=[psT])
                P.E("act", "activation", affTs[tau % 2][:], psT[0:NE, 0:128], AF.Copy, R=[psT], W=[affTs[tau % 2]])
                P.DMA("sp", affTd[:, tau * 128:(tau + 1) * 128], affTs[tau % 2][:], R=[affTs[tau % 2]])
                yield
        nblk_ = T // 512
        for _ in proj_gen(0):
            pass
        jobs = []
        for blk in range(nblk_):
            if blk + 1 < nblk_:
                jobs.append(("p", blk + 1))
            for i in range(4):
                jobs.append(("t", blk * 4 + i))

        def kjob(n):
            kind, idx = jobs[n]
            return proj_gen(idx) if kind == "p" else tile_gen(idx)
        run_pipe(kjob, len(jobs), 2)
        P.flush()
    if upto <= 10:
        P.flush(final=True); semctx.close(); G2.close(); G.close(); return nc

    with contextlib.ExitStack() as ph:
        def sb(name, shape, dt):
            return ph.enter_context(nc.sbuf_tensor(name, list(shape), dt))

        def psum(name, shape, dt):
            P.excl.add(name)
            return ph.enter_context(nc.psum_tensor(name, list(shape), dt))
        A2 = sb("L_A2", [128, 1024], F32); cmpt = sb("L_cmp", [128, 1024], F32)
        blk8 = sb("L_blk8", [128, 128], F32)
        ri = sb("L_ri", [128, 1], I32); rf = sb("L_rf", [128, 1], F32)
        cix = sb("L_ci", [128, 128], I32); cf = sb("L_cf", [128, 128], F32)
        lo = sb("L_lo", [128, 1], F32); mid = sb("L_mid", [128, 1], F32); cntp = sb("L_cnt", [128, 1], F32); pred = sb("L_pred", [128, 1], F32)
        thrM = sb("L_thrM", [128, 128], F32)
        pc = psum("L_pc", [128, 512], F32)
        P.DMA("sp", A2[:], affTd.rearrange("e (g i) -> (e g) i", g=8), W=[A2])
        P.E("pool", "iota", ri[:], [[0, 1]], base=0, channel_multiplier=1, W=[ri])
        P.E("pool", "iota", cix[:], [[1, 128]], base=0, channel_multiplier=0, W=[cix])
        P.E("dve", "tensor_single_scalar", ri[:], ri[:], 3, ALU.arith_shift_right, R=[ri], W=[ri])
        P.E("dve", "tensor_single_scalar", cix[:], cix[:], 3, ALU.arith_shift_right, R=[cix], W=[cix])
        P.E("dve", "tensor_copy", rf[:], ri[:], R=[ri], W=[rf])
        P.E("dve", "tensor_copy", cf[:], cix[:], R=[cix], W=[cf])
        P.E("dve", "tensor_scalar", blk8[:], cf[:], rf[:, 0:1], None, ALU.is_equal, R=[cf, rf], W=[blk8])
        P.E("pool", "memset", lo[:], 0.0, W=[lo])
        for it_ in range(34):
            wk = 0.5 ** (it_ + 1)
            P.E("dve", "tensor_scalar", mid[:], lo[:], wk, None, ALU.add, R=[lo], W=[mid])
            P.E("dve", "tensor_scalar", cmpt[:], A2[:], mid[:, 0:1], None, ALU.is_ge, R=[A2, mid], W=[cmpt])
            P.E("dve", "reduce_sum", cntp[:], cmpt[:], AX.X, R=[cmpt], W=[cntp])
            P.E("pe", "matmul", pc[:, 0:1], blk8[:], cntp[:], start=True, stop=True, R=[blk8, cntp], W=[pc])
            P.E("dve", "tensor_single_scalar", pred[:], pc[:, 0:1], CAP - 0.5, ALU.is_ge, R=[pc], W=[pred])
            P.E("dve", "scalar_tensor_tensor", lo[:], pred[:], wk, lo[:], ALU.mult, ALU.add, R=[pred, lo], W=[lo])
        P.E("dve", "tensor_copy", thrM[:], bc(lo[:, 0:1], [[0, 128]]), R=[lo], W=[thrM])
        P.E("pe", "matmul", pc[:, 0:NE], thrM[:], bc(identf[:, 0:1], [[8, NE]]), start=True, stop=True, R=[thrM, identf], W=[pc])
        P.E("dve", "tensor_copy", thrB[:], pc[:, 0:NE], R=[pc], W=[thrB])
        if "dbg_thr" in taps:
            dbg_thr = dscr("dbg_thr", [128, NE], F32)
            P.DMA("sp", dbg_thr[:, :], thrB[:], R=[thrB])
        P.flush()
    if upto <= 11:
        P.flush(final=True); semctx.close(); G2.close(); G.close(); return nc

    with contextlib.ExitStack() as ph:
        def sb(name, shape, dt):
            return ph.enter_context(nc.sbuf_tensor(name, list(shape), dt))

        def psum(name, shape, dt):
            P.excl.add(name)
            return ph.enter_context(nc.psum_tensor(name, list(shape), dt))
        triu = sb("M_triu", [128, 128], BF16); onesb = sb("M_ones", [128, 128], BF16)
        onesf = sb("M_onesf", [128, 128], F32)
        Rrun = sb("M_Rrun", [128, NE], BF16)
        maskb = [sb("M_mask%d" % i, [128, NE], BF16) for i in range(2)]
        slotf = [sb("M_slotf%d" % i, [128, NE], F32) for i in range(2)]
        pcs = [psum("M_pc%d" % i, [128, 512], F32) for i in range(2)]
        P.E("pool", "memset", onesf[:], 1.0, W=[onesf])
        P.E("pool", "tensor_copy", onesb[:], onesf[:], R=[onesf], W=[onesb])
        P.E("pool", "affine_select", triu[:], onesf[:], [[1, 128]], ALU.is_ge, 0.0, base=0, channel_multiplier=-1, R=[onesf], W=[triu])
        P.E("pool", "memset", Rrun[:], 0.0, W=[Rrun])
        for tau in range(T // 128):
            b_ = tau % 2
            mk = maskb[b_]; sf = slotf[b_]; pc_ = pcs[b_]
            P.E("dve", "tensor_tensor", mk[:], AFF[:, tau, :], thrB[:], ALU.is_ge, R=[AFF, thrB], W=[mk])
            P.E("pe", "matmul", pc_[:, 0:NE], triu[:], mk[:], start=True, stop=False, R=[triu, mk], W=[pc_])
            P.E("pe", "matmul", pc_[:, 0:NE], onesb[:], Rrun[:], start=False, stop=True, R=[onesb, Rrun], W=[pc_])
            P.E("dve", "scalar_tensor_tensor", sf[:], mk[:], -4096.0, pc_[:, 0:NE], ALU.mult, ALU.add, R=[mk, pc_], W=[sf])
            P.E("dve", "tensor_scalar", sf[:], sf[:], 4095.0, None, ALU.add, R=[sf], W=[sf])
            P.E("dve", "tensor_copy", SLOT[:, tau, :], sf[:], R=[sf], W=[SLOT])
            P.E("dve", "tensor_tensor", Rrun[:], Rrun[:], mk[:], ALU.add, R=[Rrun, mk], W=[Rrun])
        P.flush()
    if upto <= 12:
        P.flush(final=True); semctx.close(); G2.close(); G.close(); return nc

    G2.close()
    with contextlib.ExitStack() as ph:
        def sb(name, shape, dt):
            return ph.enter_context(nc.sbuf_tensor(name, list(shape), dt))

        def psum(name, shape, dt):
            P.excl.add(name)
            return ph.enter_context(nc.psum_tensor(name, list(shape), dt))
        w1b = sb("N_w1", [128, 8, DE], BF16); w3b = sb("N_w3", [128, 8, DE], BF16); w2b = sb("N_w2", [128, 16, D], BF16)
        xeT = sb("N_xeT", [128, 8, CAP], BF16); hidT = sb("N_hidT", [128, 16, CAP], BF16)
        xts = [sb("N_xt%d" % i, [128, ROWP], BF16) for i in range(2)]
        s1s = [sb("N_s1%d" % i, [128, 512], BF16) for i in range(2)]
        yos = [sb("N_yo%d" % i, [128, D], F32) for i in range(2)]
        stg = [sb("N_stg%d" % i, [128, 1024], F32) for i in range(2)]
        ps1 = [psum("N_p1%d" % i, [128, 512], F32) for i in range(2)]
        ps3 = [psum("N_p3%d" % i, [128, 512], F32) for i in range(2)]
        pso = [psum("N_po%d" % i, [128, 512], F32) for i in range(4)]
        lc = {"n": 0}
        dtl = [sb("N_dt%d" % i, [128, ROWP], BF16) for i in range(5)]
        tidfs = [sb("N_tidf%d" % i, [128, 8], F32) for i in range(2)]
        tidis = [sb("N_tidi%d" % i, [128, 8], I32) for i in range(2)]
        garrs = [sb("N_garr%d" % i, [128, 8, NE], F32) for i in range(2)]
        dcount = {"n": 0}
        dtoks = {}

        def load_cast(dst, src, wres):
            i = lc["n"]; lc["n"] += 1
            st = stg[i % 2]
            P.DMA("sp", st[:], src, W=[st])
            if i % 2 == 0:
                P.E("act", "activation", dst, st[:], AF.Copy, R=[st], W=[wres])
            else:
                P.E("dve", "tensor_copy", dst, st[:], R=[st], W=[wres])

        def w13_jobs(e):
            jobs = []
            for k in range(8):
                for h_ in range(2):
                    jobs.append((w1b[:, k, h_ * 1024:(h_ + 1) * 1024], exp_w1[e, k * 128:(k + 1) * 128, h_ * 1024:(h_ + 1) * 1024], w1b))
                    jobs.append((w3b[:, k, h_ * 1024:(h_ + 1) * 1024], exp_w3[e, k * 128:(k + 1) * 128, h_ * 1024:(h_ + 1) * 1024], w3b))
            return jobs

        def w2_jobs(e):
            return [(w2b[:, kk, :], exp_w2[e, kk * 128:(kk + 1) * 128, :], w2b) for kk in range(16)]

        def dispatch_one(e, tau):
            i = dcount["n"]; dcount["n"] += 1
            tl = dtl[i % 5]
            P.DMA("sp", tl[:], h2p[tau * 128:(tau + 1) * 128, :], W=[tl])
            tok = P.dma("pool", (lambda en, e=e, tau=tau, tl=tl: en.indirect_dma_start(
                out=xe[e][:, :], out_offset=bass.IndirectOffsetOnAxis(ap=SLOT[:, tau, e:e + 1], axis=0),
                in_=tl[:], in_offset=None, bounds_check=P.reg(en, CAP - 1), oob_is_err=False)), R=[SLOT.name, tl.name], W=[])
            d_ = dtoks.setdefault(e, {})
            d_[tok[0]] = max(d_.get(tok[0], 0), tok[1])

        def wait_dispatch(e):
            waits = [(sk, v) for sk, v in dtoks.get(e, {}).items() if P.known["sp"].get(sk, 0) < v]
            for sk, v in waits:
                P.known["sp"][sk] = v
            if waits:
                P.q["sp"].append((None, waits, None))

        def prologue_step(e, st_):
            par = e % 2
            tidf = tidfs[par]; tidi = tidis[par]; garr = garrs[par]
            xt = xts[st_ % 2]
            if st_ == 0:
                wait_dispatch(e)
            P.DMA("sp", xt[:], xe[e][st_ * 128:(st_ + 1) * 128, :], W=[xt])
            P.E("dve", "scalar_tensor_tensor", tidf[:, st_:st_ + 1], xt[:, D:D + 1], 128.0, xt[:, D + 1:D + 2], ALU.mult, ALU.add,
                R=[xt], W=[tidf])
            P.E("dve", "tensor_copy", tidi[:, st_:st_ + 1], tidf[:, st_:st_ + 1], R=[tidf], W=[tidi])
            P.dma("pool", (lambda en, st_=st_, garr=garr, tidi=tidi: en.indirect_dma_start(
                out=garr[:, st_, :], out_offset=None, in_=affd[:, :],
                in_offset=bass.IndirectOffsetOnAxis(ap=tidi[:, st_:st_ + 1], axis=0),
                bounds_check=P.reg(en, T - 1), oob_is_err=False)), R=[tidi.name], W=[garr.name])
            for half in range(2):
                pp = ps1[half]
                for k4 in range(4):
                    k = half * 4 + k4
                    P.E("pe", "matmul", pp[:, k4 * 128:(k4 + 1) * 128], xt[:, k * 128:(k + 1) * 128], identb[:], start=True, stop=True,
                        R=[xt, identb], W=[pp])
                dst = xeT[:, half * 4:(half + 1) * 4, st_ * 128:(st_ + 1) * 128]
                if half == 0:
                    P.E("act", "activation", dst, pp[:].rearrange("p (a b) -> p a b", b=128), AF.Copy, R=[pp], W=[xeT])
                else:
                    P.E("dve", "tensor_copy", dst, pp[:].rearrange("p (a b) -> p a b", b=128), R=[pp], W=[xeT])
        import os
        nexp = int(os.environ.get("NEXP", NE))
        for tau in range(T // 128):
            dispatch_one(0, tau)
        for j in w13_jobs(0):
            load_cast(*j)
        for st_ in range(8):
            prologue_step(0, st_)
        ev = 0
        for e in range(nexp):
            par = e % 2
            tidi = tidis[par]; garr = garrs[par]
            jobs2 = w2_jobs(e)
            djobs = [(e + 1, tau) for tau in range(T // 128)] if e + 1 < nexp else []
            for sh in range(2):
                ssl = slice(sh * 512, (sh + 1) * 512)
                for fc in range(16):
                    if jobs2:
                        load_cast(*jobs2.pop(0))
                    for _ in range(2):
                        if djobs:
                            dispatch_one(*djobs.pop(0))
                    p1 = ps1[ev % 2]; p3 = ps3[ev % 2]; s1 = s1s[ev % 2]; ev += 1
                    for k in range(8):
                        P.E("pe", "matmul", p1[:], w1b[:, k, fc * 128:(fc + 1) * 128], xeT[:, k, ssl], start=(k == 0), stop=(k == 7),
                            R=[w1b, xeT], W=[p1])
                    for k in range(8):
                        P.E("pe", "matmul", p3[:], w3b[:, k, fc * 128:(fc + 1) * 128], xeT[:, k, ssl], start=(k == 0), stop=(k == 7),
                            R=[w3b, xeT], W=[p3])
                    P.E("act", "activation", s1[:], p1[:], AF.Silu, R=[p1], W=[s1])
                    P.E("dve", "tensor_tensor", hidT[:, fc, ssl], s1[:], p3[:], ALU.mult, R=[s1, p3], W=[hidT])
            jobs13 = w13_jobs(e + 1) if e + 1 < nexp else []
            for st_ in range(8):
                yo = yos[st_ % 2]
                for dh in range(2):
                    for _ in range(2):
                        if jobs13:
                            load_cast(*jobs13.pop(0))
                    po = pso[(st_ % 2) * 2 + dh]
                    for fc in range(16):
                        P.E("pe", "matmul", po[:], hidT[:, fc, st_ * 128:(st_ + 1) * 128], w2b[:, fc, dh * 512:(dh + 1) * 512],
                            start=(fc == 0), stop=(fc == 15), R=[hidT, w2b], W=[po])
                    if dh == 0:
                        P.E("act", "activation", yo[:, 0:512], po[:], AF.Identity, scale=garr[:, st_, e:e + 1], R=[po, garr], W=[yo])
                    else:
                        P.E("dve", "tensor_scalar", yo[:, 512:1024], po[:], garr[:, st_, e:e + 1], None, ALU.mult, R=[po, garr], W=[yo])
                P.dma("pool", (lambda en, st_=st_, yo=yo, tidi=tidi: en.indirect_dma_start(
                    out=acc[:, :], out_offset=bass.IndirectOffsetOnAxis(ap=tidi[:, st_:st_ + 1], axis=0),
                    in_=yo[:], in_offset=None, bounds_check=P.reg(en, T - 1), oob_is_err=True, compute_op=ALU.add)),
                    R=[yo.name, tidi.name] + (["accE%d_%d" % (e - 1, j_) for j_ in range(8)] if e > 0 else []),
                    W=["accE%d_%d" % (e, st_)])
                if e + 1 < nexp:
                    prologue_step(e + 1, st_)
            for j in jobs13:
                load_cast(*j)
        P.flush()
    if upto <= 13:
        P.flush(final=True); semctx.close(); G2.close(); G.close(); return nc

    with contextlib.ExitStack() as ph:
        def sb(name, shape, dt):
            return ph.enter_context(nc.sbuf_tensor(name, list(shape), dt))
        NR = 4
        ats = [sb("O_a%d" % i, [128, D], F32) for i in range(NR)]
        xs_ = [sb("O_x%d" % i, [128, D], F32) for i in range(NR)]
        os_ = [sb("O_o%d" % i, [128, D], F32) for i in range(NR)]
        junkb = sb("O_junk", [128, D], BF16); sss = [sb("O_ss%d" % i, [128, 4], F32) for i in range(NR)]

        def o_gen(tau):
            b_ = tau % NR; rsl = slice(tau * 128, (tau + 1) * 128)
            at, xt, ot, ss = ats[b_], xs_[b_], os_[b_], sss[b_]
            P.DMA("sp", at[:], acc[rsl, :], W=[at]); P.DMA("act", xt[:], x1d[rsl, :], W=[xt])
            yield
            P.E("act", "activation", junkb[:], at[:], AF.Square, accum_out=ss[:, 0:1], R=[at], W=[junkb, ss])
            yield
            P.E("dve", "tensor_scalar", ss[:, 1:2], ss[:, 0:1], 1.0 / D, EPS, ALU.mult, ALU.add, R=[ss], W=[ss])
            yield
            P.E("pool", "tensor_tensor", ss[:, 2:3], ss[:, 1:2], nhalf[:], ALU.pow, R=[ss, nhalf], W=[ss])
            yield
            P.E("dve", "scalar_tensor_tensor", at[:], at[:], ss[:, 2:3], rows["F"][:], ALU.mult, ALU.mult, R=[at, ss, rows["F"]], W=[at])
            P.E("dve", "tensor_tensor", ot[:], at[:], xt[:], ALU.add, R=[at, xt], W=[ot])
            yield
            P.DMA("sp", y_out[rsl, :], ot[:], R=[ot])
            yield
        run_pipe(o_gen, T // 128, NR)
        P.flush()

    P.flush(final=True)
    semctx.close(); G.close()
    return nc


def prep_inputs(inp, consts):
    f = np.float32
    L0 = lambda k: np.ascontiguousarray(inp[k][0], dtype=f)
    sh = {}
    sh["mod_w"] = L0("mod_w"); sh["mod_b"] = L0("mod_b").reshape(1, -1)
    sh["norms"] = np.stack([L0("norm1_pre"), L0("norm1_post"), L0("norm2_pre"), L0("norm2_post")])
    sh["w_in"] = L0("w_in")
    cw = L0("hy_conv_w"); cb = L0("hy_conv_b")
    hc = np.zeros((128, 12, 4), f)
    for k in range(3):
        hc[:, :, k] = cw[k].reshape(12, 128).T
    hc[:, :, 3] = cb.reshape(12, 128).T
    sh["hcw"] = hc
    sh["hy_w1"] = L0("hy_ffn_w1"); sh["hy_w2"] = L0("hy_ffn_w2"); sh["hy_w3"] = L0("hy_ffn_w3")
    sh["hyp"] = np.stack([L0("hy_freq"), L0("hy_ffn_b1"), L0("hy_ffn_b2")], axis=1)
    sh["hy_biasc"] = np.ascontiguousarray(L0("hy_bias").reshape(4, 128).T)
    sh["hy_proj"] = L0("hy_proj"); sh["rw_proj"] = L0("rw_proj"); sh["w_out"] = L0("w_out")
    sh["router_w"] = L0("router_w")
    mu = L0("rw_mu")
    sh["mucol"] = np.ascontiguousarray(mu.reshape(2, 14, 128).transpose(2, 1, 0))
    w0 = L0("rw_w0"); a0 = L0("rw_a0")
    wa = np.zeros((128, 4, 4), f)
    for d in range(2):
        wa[:, :, d] = w0[d].reshape(4, 128).T
        wa[:, :, 2 + d] = a0[d].reshape(4, 128).T
    sh["rw_w0a0"] = wa
    up = np.zeros((128, 2, 512), f)
    up[0:64] = L0("rw_w_up").transpose(1, 0, 2)
    up[64:128] = L0("rw_a_up").transpose(1, 0, 2)
    sh["rw_up"] = up
    sh["rw_g_up"] = L0("rw_g_up")
    rc = np.zeros((128, 4, 5), f)
    for i, k in enumerate(["rw_k_k", "rw_k_a", "rw_r_k", "rw_ln_g", "rw_ln_b"]):
        rc[:, :, i] = L0(k).reshape(4, 128).T
    sh["rwc"] = rc
    sh["exp_w1"] = L0("exp_w1"); sh["exp_w3"] = L0("exp_w3"); sh["exp_w2"] = L0("exp_w2")
    sh.update(consts)
    maps = []
    for core in range(8):
        b = core // 2
        m = dict(sh)
        m["x"] = np.ascontiguousarray(inp["x"][b], dtype=f)
        m["ctx"] = np.ascontiguousarray(inp["ctx"][b], dtype=f)
        cv = np.zeros((128, 16), f)
        cv[:, 0:8] = np.asarray(inp["c"][b], f).reshape(8, 128).T
        cv[:, 8:16] = np.asarray(inp["c_ctx"], f).reshape(8, 128).T
        m["cvec"] = cv
        maps.append(m)
    return maps


_CACHE = {}


def kernel(**inputs):
    inp = {k: np.asarray(v) for k, v in inputs.items()}
    if "nc" not in _CACHE:
        _CACHE["nc"] = build()
        _CACHE["consts"] = host_consts()
    nc = _CACHE["nc"]
    maps = prep_inputs(inp, _CACHE["consts"])
    res = run_bass_kernel_spmd(nc, maps, core_ids=list(range(8)))
    out = np.stack([np.asarray(res.results[2 * b]["y_out"], dtype=np.float32) for b in range(4)], axis=0)
    return out
```

```python
import contextlib
import math
import numpy as np
import concourse.bass as bass
import concourse.mybir as mybir
from concourse.bass_utils import run_bass_kernel_spmd
from concourse.ap import AP

F32 = mybir.dt.float32
BF16 = mybir.dt.bfloat16
I32 = mybir.dt.int32
U32 = mybir.dt.uint32
ALU = mybir.AluOpType
AF = mybir.ActivationFunctionType
AX = mybir.AxisListType

D = 1024
T = 8192
TC = 256
NCOL = 5376
HYW = 512
RWW = 512
NE = 16
DE = 2048
CAP = 1024
CH = 64
EPS = 1e-6
LN_EPS = 64e-5
ROWP = 1026

ENGS = ("pe", "act", "dve", "pool", "sp")
NDSEM = 12


def _res(x):
    if isinstance(x, str):
        return x
    if isinstance(x, AP):
        return x.tensor.name
    return x.name


class Prog:
    def __init__(self, nc, semctx):
        self.nc = nc
        self.semctx = semctx
        self.sems = {}
        self.q = {e: [] for e in ENGS}
        self.cnt = {e: 0 for e in ENGS}
        self.known = {e: {} for e in ENGS}
        self.lastw = {}
        self.readers = {}
        self.dcount = {}
        self.dnext = {e: 0 for e in ENGS}
        self.ninstr = 0
        self.excl = set()
        self._regs = {}
        self.phase = 0

    def _waits(self, eng, reads, writes):
        need = {}

        def add(tok):
            if tok is None:
                return
            sk, val, teng = tok
            if teng == "pe" and eng == "pe":
                return
            if self.known[eng].get(sk, 0) >= val:
                return
            if need.get(sk, 0) < val:
                need[sk] = val

        for r in reads:
            add(self.lastw.get(r))
        for w in writes:
            add(self.lastw.get(w))
            for t in self.readers.get(w, ()):
                add(t)
        for sk, val in need.items():
            self.known[eng][sk] = val
        return list(need.items())

    def _commit(self, tok, reads, writes):
        for r in reads:
            self.readers.setdefault(r, []).append(tok)
        for w in writes:
            self.lastw[w] = tok
            self.readers[w] = []

    def op(self, eng, fn, R=(), W=()):
        reads = [_res(r) for r in R]
        writes = [_res(w) for w in W]
        if self.excl:
            writes = writes + [r for r in reads if r in self.excl and r not in writes]
            reads = [r for r in reads if r not in self.excl]
        waits = self._waits(eng, reads, writes)
        self.cnt[eng] += 1
        tok = (eng, self.cnt[eng], eng)
        self.q[eng].append((fn, waits, (eng, 1)))
        self._commit(tok, reads, writes)
        self.ninstr += 1
        return tok

    def dma(self, eng, fn, R=(), W=()):
        reads = [_res(r) for r in R]
        writes = [_res(w) for w in W]
        slot = self.dnext[eng]
        self.dnext[eng] = (slot + 1) % NDSEM
        sk = "d_%s_%d" % (eng, slot)
        prev = self.dcount.get(sk, 0)
        waits = self._waits(eng, reads, writes)
        if prev > 0 and self.known[eng].get(sk, 0) < 16 * prev:
            waits.append((sk, 16 * prev))
            self.known[eng][sk] = 16 * prev
        self.dcount[sk] = prev + 1
        tok = (sk, 16 * (prev + 1), "dma")
        self.q[eng].append((fn, waits, (sk, 16)))
        self._commit(tok, reads, writes)
        self.ninstr += 1
        return tok

    def reg(self, en, value):
        key = (self.phase, value)
        if key not in self._regs:
            self._regs[key] = en.to_reg(value)
        return self._regs[key]

    def E(self, eng, meth, *a, R=(), W=(), **kw):
        return self.op(eng, lambda e: getattr(e, meth)(*a, **kw), R, W)

    def DMA(self, eng, out, in_, R=(), W=(), **kw):
        return self.dma(eng, lambda e: e.dma_start(out=out, in_=in_, **kw), R, W)

    def flush(self, final=False):
        nc = self.nc
        for k in list(ENGS) + list(self.dcount.keys()):
            if k not in self.sems:
                self.sems[k] = self.semctx.enter_context(nc.semaphore("s_" + k))
        sems = self.sems
        full = {e: self.cnt[e] for e in ENGS}
        for sk, c in self.dcount.items():
            full[sk] = 16 * c
        if final:
            for e in ENGS:
                self.q[e].append((None, [(sk, v) for sk, v in full.items() if v > 0], None))
        with nc.Block() as block:
            engmap = {"pe": block.tensor, "act": block.scalar, "dve": block.vector,
                      "pool": block.gpsimd, "sp": block.sync}
            for e in ENGS:
                lst = self.q[e]
                if not lst:
                    continue

                def body(engobj, lst=lst):
                    for fn, waits, inc in lst:
                        for sk, val in waits:
                            engobj.wait_ge(sems[sk], val)
                        if fn is not None:
                            ins = fn(engobj)
                            if inc is not None:
                                ins.then_inc(sems[inc[0]], inc[1])
                engmap[e](body)
        self.q = {e: [] for e in ENGS}
        self.phase += 1
        for e in ENGS:
            waits = [(sk, v) for sk, v in full.items() if v > 0 and self.known[e].get(sk, 0) < v]
            if waits:
                self.q[e].append((None, waits, None))
                for sk, v in waits:
                    self.known[e][sk] = v


def bc(ap_, shape_pairs):
    return AP(ap_.tensor, ap_.offset, [list(ap_.ap[0])] + [list(x) for x in shape_pairs])


def host_consts():
    c = {}
    L = T
    f32 = np.float32
    t = np.linspace(0.0, 1.0, L, dtype=f32)
    ang = (f32(2.0 * math.pi / L) * np.arange(L, dtype=f32)).astype(f32)
    bands = np.linspace(1e-4, 16 - 1, 16, dtype=f32)
    ph = (bands[None, :] * ang[:, None]).astype(f32)
    feat = np.concatenate([t[:, None], np.cos(ph), -np.sin(ph)], axis=-1).astype(f32)
    c["featT"] = np.ascontiguousarray(feat.T)
    c["trow"] = t.reshape(1, L).copy()
    deltas = np.abs(np.linspace(math.log(1e-2) / 1.5, math.log(1e-2) / 0.3, HYW, dtype=f32))
    nd = np.zeros((128, 8), f32)
    for j in range(8):
        nd[:, j] = -deltas[(j % 4) * 128:(j % 4) * 128 + 128]
    c["negdelta"] = nd
    n = np.arange(128)
    Fm = np.exp(-2j * np.pi * np.outer(n, n) / 128)
    Tw = np.exp(-2j * np.pi * np.outer(n, n) / (2 * L))
    Fr = Fm.real.astype(f32); Fi = Fm.imag.astype(f32)
    dft = np.stack([Fr, Fi, -Fi, Fr, -Fi, Fi, Fr], axis=1).astype(f32)
    c["dft"] = np.ascontiguousarray(dft)
    tw = np.stack([Tw.real, Tw.imag, -Tw.imag], axis=1).astype(f32)
    c["tw"] = np.ascontiguousarray(tw)
    return c


def build(taps=(), upto=99):
    nc = bass.Bass("TRN2", target_bir_lowering=False)
    G = contextlib.ExitStack()
    semctx = contextlib.ExitStack()
    P = Prog(nc, semctx)

    def din(name, shape, dt=F32):
        return nc.dram_tensor(name, list(shape), dt, kind="ExternalInput").ap()

    def dscr(name, shape, dt):
        kind = "ExternalOutput" if name in taps else "Internal"
        return nc.dram_tensor(name, list(shape), dt, kind=kind).ap()

    def gsb(name, shape, dt):
        return G.enter_context(nc.sbuf_tensor(name, list(shape), dt))

    x_in = din("x", [T, D]); ctx_in = din("ctx", [TC, D]); cvec = din("cvec", [128, 16])
    mod_w = din("mod_w", [D, 6 * D]); mod_b = din("mod_b", [1, 6 * D])
    norms = din("norms", [4, D])
    w_in = din("w_in", [D, NCOL])
    hcw = din("hcw", [128, 12, 4])
    hy_w1 = din("hy_w1", [33, 64]); hy_w2 = din("hy_w2", [64, 64]); hy_w3 = din("hy_w3", [64, 1024])
    hyp = din("hyp", [64, 3]); hy_biasc = din("hy_biasc", [128, 4])
    hy_proj = din("hy_proj", [HYW, D]); rw_proj = din("rw_proj", [RWW, D]); w_out = din("w_out", [D, D])
    router_w = din("router_w", [D, NE])
    mucol = din("mucol", [128, 14, 2]); rw_w0a0 = din("rw_w0a0", [128, 4, 4])
    rw_up = din("rw_up", [128, 2, 512]); rw_g_up = din("rw_g_up", [128, 512]); rwc = din("rwc", [128, 4, 5])
    exp_w1 = din("exp_w1", [NE, D, DE]); exp_w3 = din("exp_w3", [NE, D, DE]); exp_w2 = din("exp_w2", [NE, DE, D])
    featT = din("featT", [33, T]); trow = din("trow", [1, T]); negdelta = din("negdelta", [128, 8])
    dft_in = din("dft", [128, 7, 128]); tw_in = din("tw", [128, 3, 128])
    y_out = nc.dram_tensor("y_out", [T, D], F32, kind="ExternalOutput").ap()

    colsT = dscr("colsT", [NCOL, T], BF16)
    ccT = dscr("ccT", [1792, TC], BF16)

    identb = gsb("identb", [128, 128], BF16)
    identf = gsb("identf", [128, 128], F32)
    nhalf = gsb("nhalf", [128, 1], F32)
    invn = gsb("invn", [128, 4], F32)
    blk64 = gsb("blk64", [128, 128], F32)
    AFF = gsb("AFF", [128, T // 128, NE], F32)
    thrB = gsb("thrB", [128, NE], F32)
    pidx = gsb("pidx", [128, 1], F32)
    SLOT = gsb("SLOT", [128, T // 128, NE], I32)
    rows = {"F": gsb("RF", [128, D], F32)}
    G2 = contextlib.ExitStack()
    for k in ("A", "B", "C", "D", "E", "Ac", "Bc"):
        rows[k] = G2.enter_context(nc.sbuf_tensor("R" + k, [128, D], F32))

    P.E("pool", "memset", identf[:], 1.0, W=[identf])
    P.E("pool", "affine_select", identf[:], identf[:], [[-1, 128]], ALU.is_equal, 0.0, base=0,
        channel_multiplier=1, R=[identf], W=[identf])
    P.E("pool", "tensor_copy", identb[:], identf[:], R=[identf], W=[identb])
    P.E("pool", "memset", nhalf[:], -0.5, W=[nhalf])

    def run_pipe(genfn, n, maxlive):
        live = []
        nxt = 0
        while live or nxt < n:
            if nxt < n and len(live) < maxlive:
                live.append(genfn(nxt)); nxt += 1
            nl = []
            for g_ in live:
                try:
                    next(g_); nl.append(g_)
                except StopIteration:
                    pass
            live = nl

    def dram_bcast(ap2d, ncols):
        return AP(ap2d.tensor, ap2d.offset, [[0, 128], [1, ncols]])

    with contextlib.ExitStack() as ph:
        def sb(name, shape, dt):
            return ph.enter_context(nc.sbuf_tensor(name, list(shape), dt))

        def psum(name, shape, dt):
            P.excl.add(name)
            return ph.enter_context(nc.psum_tensor(name, list(shape), dt))
        cv = sb("A_cv", [128, 16], F32); sc = sb("A_sc", [128, 16], F32)
        SC = sb("A_SC", [128, 16, 128], F32)
        MB = sb("A_MB", [128, 6 * D], F32); M = sb("A_M", [128, 6 * D], F32); Mc = sb("A_Mc", [128, 2 * D], F32)
        Gn = sb("A_G", [128, 4, D], F32)
        wts = [sb("A_w%d" % i, [128, 8, 512], F32) for i in range(2)]
        pss = [psum("A_p%d" % i, [128, 512], F32) for i in range(4)]
        P.DMA("sp", cv[:], cvec[:, :], W=[cv])
        P.DMA("sp", MB[:], dram_bcast(mod_b[0:1, :], 6 * D), W=[MB])
        for i in range(4):
            P.DMA("sp", Gn[:, i, :], dram_bcast(norms[i:i + 1, :], D), W=[Gn])
        P.E("act", "activation", sc[:], cv[:], AF.Silu, R=[cv], W=[sc])
        P.E("dve", "tensor_copy", SC[:], bc(sc[:], [[1, 16], [0, 128]]), R=[sc], W=[SC])
        pi = 0
        for cb in range(12):
            wt = wts[cb % 2]
            P.DMA("sp", wt[:], mod_w[:, cb * 512:(cb + 1) * 512].rearrange("(k p) n -> p k n", p=128), W=[wt])
            for which in range(2 if cb < 4 else 1):
                ps = pss[pi % 4]; pi += 1
                for k in range(8):
                    P.E("pe", "matmul", ps[:], SC[:, which * 8 + k, :], wt[:, k, :], start=(k == 0), stop=(k == 7),
                        R=[SC, wt], W=[ps])
                dstM = M if which == 0 else Mc
                P.E("dve", "tensor_tensor", dstM[:, cb * 512:(cb + 1) * 512], ps[:], MB[:, cb * 512:(cb + 1) * 512],
                    ALU.add, R=[ps, MB], W=[dstM])

        def seg(t_, i):
            return t_[:, i * D:(i + 1) * D]
        P.E("dve", "scalar_tensor_tensor", rows["A"][:], seg(M, 1), 1.0, Gn[:, 0, :], ALU.add, ALU.mult, R=[M, Gn], W=[rows["A"]])
        P.E("act", "activation", rows["B"][:], seg(M, 0), AF.Copy, R=[M], W=[rows["B"]])
        P.E("dve", "tensor_tensor", rows["C"][:], seg(M, 2), Gn[:, 1, :], ALU.mult, R=[M, Gn], W=[rows["C"]])
        P.E("dve", "scalar_tensor_tensor", rows["D"][:], seg(M, 4), 1.0, Gn[:, 2, :], ALU.add, ALU.mult, R=[M, Gn], W=[rows["D"]])
        P.E("act", "activation", rows["E"][:], seg(M, 3), AF.Copy, R=[M], W=[rows["E"]])
        P.E("dve", "tensor_tensor", rows["F"][:], seg(M, 5), Gn[:, 3, :], ALU.mult, R=[M, Gn], W=[rows["F"]])
        P.E("dve", "scalar_tensor_tensor", rows["Ac"][:], seg(Mc, 1), 1.0, Gn[:, 0, :], ALU.add, ALU.mult, R=[Mc, Gn], W=[rows["Ac"]])
        P.E("act", "activation", rows["Bc"][:], seg(Mc, 0), AF.Copy, R=[Mc], W=[rows["Bc"]])
        if "dbg_rows" in taps:
            dbg_rows = dscr("dbg_rows", [8, D], F32)
            for i, k in enumerate(("A", "B", "C", "D", "E", "F", "Ac", "Bc")):
                P.DMA("sp", dbg_rows[i:i + 1, :], rows[k][0:1, :], R=[rows[k]])
        P.flush()
    if upto <= 0:
        P.flush(final=True); semctx.close(); G2.close(); G.close(); return nc

    with contextlib.ExitStack() as ph:
        def sb(name, shape, dt):
            return ph.enter_context(nc.sbuf_tensor(name, list(shape), dt))

        def psum(name, shape, dt):
            P.excl.add(name)
            return ph.enter_context(nc.psum_tensor(name, list(shape), dt))
        WIN = sb("B_win", [128, 8, NCOL], BF16)
        for k in range(8):
            for c0 in range(0, NCOL, 1792):
                P.DMA("pool", WIN[:, k, c0:c0 + 1792], w_in[k * 128:(k + 1) * 128, c0:c0 + 1792], W=[WIN])
        xts = [sb("B_x%d" % i, [128, D], F32) for i in range(2)]
        junk = sb("B_junk", [128, D], BF16)
        t1s = [sb("B_t1%d" % i, [128, D], F32) for i in range(2)]
        hbs = [sb("B_hb%d" % i, [128, D], BF16) for i in range(2)]
        sss = [sb("B_ss%d" % i, [128, 4], F32) for i in range(2)]
        hTs = [sb("B_hT%d" % i, [128, 8, 512], BF16) for i in range(2)]
        stg = [sb("B_st%d" % i, [128, 512], BF16) for i in range(4)]
        pTs = [psum("B_pT%d" % i, [128, 8, 128], BF16) for i in range(2)]
        pos = [psum("B_po%d" % i, [128, 512], F32) for i in range(4)]
        cnt = {"tile": 0, "blk": 0, "ev": 0}

        hb4 = [sb("B_hb4_%d" % i, [128, D], BF16) for i in range(8)]

        def inproj(src, ntiles, RAx, RBx, chunks, dst, row0):
            nblk = (ntiles + 3) // 4

            def tiles_of(blk):
                return list(range(blk * 4, min(ntiles, blk * 4 + 4)))

            def prep_norm(blk):
                for i, tix in enumerate(tiles_of(blk)):
                    j = cnt["tile"] % 2; cnt["tile"] += 1
                    xt, t1, ss = xts[j], t1s[j], sss[j]
                    hb = hb4[(blk % 2) * 4 + i]
                    P.DMA("sp", xt[:], src[tix * 128:(tix + 1) * 128, :], W=[xt])
                    P.E("act", "activation", junk[:], xt[:], AF.Square, accum_out=ss[:, 0:1], R=[xt], W=[junk, ss])
                    P.E("dve", "tensor_scalar", ss[:, 1:2], ss[:, 0:1], 1.0 / D, EPS, ALU.mult, ALU.add, R=[ss], W=[ss])
                    P.E("pool", "tensor_tensor", ss[:, 2:3], ss[:, 1:2], nhalf[:], ALU.pow, R=[ss, nhalf], W=[ss])
                    P.E("dve", "scalar_tensor_tensor", t1[:], xt[:], ss[:, 2:3], RAx[:], ALU.mult, ALU.mult,
                        R=[xt, ss, RAx], W=[t1])
                    P.E("dve", "tensor_tensor", hb[:], t1[:], RBx[:], ALU.add, R=[t1, RBx], W=[hb])

            def prep_T(blk):
                hT = hTs[blk % 2]
                for i, tix in enumerate(tiles_of(blk)):
                    hb = hb4[(blk % 2) * 4 + i]; pT = pTs[i % 2]
                    for k in range(8):
                        P.E("pe", "transpose", pT[:, k, :], hb[:, k * 128:(k + 1) * 128], identb[:], R=[hb, identb], W=[pT])
                    P.E("act", "activation", hT[:, :, i * 128:(i + 1) * 128], pT[:], AF.Copy, R=[pT], W=[hT])
            prep_norm(0)
            prep_T(0)
            for blk in range(nblk):
                N = 128 * len(tiles_of(blk))
                hT = hTs[blk % 2]
                if blk + 1 < nblk:
                    prep_norm(blk + 1)
                for c in chunks:
                    e = cnt["ev"]; cnt["ev"] += 1
                    po = pos[e % 4]; st = stg[e % 4]
                    for k in range(8):
                        P.E("pe", "matmul", po[:, 0:N], WIN[:, k, c * 128:(c + 1) * 128], hT[:, k, 0:N],
                            start=(k == 0), stop=(k == 7), R=[WIN, hT], W=[po])
                    if c >= 26:
                        P.E("act", "activation", st[:, 0:N], po[:, 0:N], AF.Sigmoid, R=[po], W=[st])
                    elif e % 2 == 0:
                        P.E("dve", "tensor_copy", st[:, 0:N], po[:, 0:N], R=[po], W=[st])
                    else:
                        P.E("act", "activation", st[:, 0:N], po[:, 0:N], AF.Copy, R=[po], W=[st])
                    r0 = (c - row0) * 128
                    P.DMA("sp", dst[r0:r0 + 128, blk * 512:blk * 512 + N], st[:, 0:N], R=[st])
                if blk + 1 < nblk:
                    prep_T(blk + 1)

        inproj(ctx_in, TC // 128, rows["Ac"], rows["Bc"], list(range(12, 26)), ccT, 12)
        inproj(x_in, T // 128, rows["A"], rows["B"], list(range(42)), colsT, 0)
        P.flush()
    if upto <= 1:
        P.flush(final=True); semctx.close(); G2.close(); G.close(); return nc

    zT = dscr("zT", [HYW, T], BF16); x0T = dscr("x0T", [HYW, T], BF16)
    with contextlib.ExitStack() as ph:
        def sb(name, shape, dt):
            return ph.enter_context(nc.sbuf_tensor(name, list(shape), dt))
        NB = 2048
        hc = sb("D_hcw", [128, 12, 4], F32)
        P.DMA("sp", hc[:], hcw[:, :, :], W=[hc])
        raws = [[sb("D_raw%d_%d" % (p_, i), [128, NB + 2], BF16) for i in range(2)] for p_ in range(3)]
        uus = [[sb("D_u%d_%d" % (p_, i), [128, NB], F32) for i in range(2)] for p_ in range(3)]
        zbs = [sb("D_zb%d" % i, [128, NB], BF16) for i in range(2)]
        x0bs = [sb("D_x0b%d" % i, [128, NB], BF16) for i in range(2)]
        it = 0
        for ct in range(4):
            for blk in range(T // NB):
                t0 = blk * NB
                b_ = it % 2; it += 1
                for part in range(3):
                    j = part * 4 + ct
                    raw = raws[part][b_]; u = uus[part][b_]
                    lo = max(t0 - 1, 0); hi = min(t0 + NB + 1, T)
                    P.DMA("sp", raw[:, lo - (t0 - 1):hi - (t0 - 1)], colsT[j * 128:(j + 1) * 128, lo:hi], W=[raw])
                    if blk == 0:
                        P.E("pool", "memset", raw[:, 0:1], 0.0, W=[raw])
                    if hi == T:
                        P.E("pool", "memset", raw[:, NB + 1:NB + 2], 0.0, W=[raw])
                    P.E("act", "activation", u[:], raw[:, 1:NB + 1], AF.Identity, bias=hc[:, j, 3:4], scale=hc[:, j, 1:2],
                        R=[raw, hc], W=[u])
                    P.E("dve", "scalar_tensor_tensor", u[:], raw[:, 0:NB], hc[:, j, 0:1], u[:], ALU.mult, ALU.add,
                        R=[raw, hc, u], W=[u])
                    P.E("dve", "scalar_tensor_tensor", u[:], raw[:, 2:NB + 2], hc[:, j, 2:3], u[:], ALU.mult, ALU.add,
                        R=[raw, hc, u], W=[u])
                zb = zbs[b_]; x0b = x0bs[b_]
                P.E("dve", "tensor_tensor", zb[:], uus[2][b_][:], uus[1][b_][:], ALU.mult, R=[uus[2][b_], uus[1][b_]], W=[zb])
                P.E("act", "activation", x0b[:], uus[0][b_][:], AF.Copy, R=[uus[0][b_]], W=[x0b])
                P.DMA("sp", zT[ct * 128:(ct + 1) * 128, t0:t0 + NB], zb[:], R=[zb])
                P.DMA("sp", x0T[ct * 128:(ct + 1) * 128, t0:t0 + NB], x0b[:], R=[x0b])
        P.flush()
    if upto <= 2:
        P.flush(final=True); semctx.close(); G2.close(); G.close(); return nc

    filtT = dscr("filtT", [2 * HYW, T], BF16)
    HsT = dscr("HsT", [128, HYW, 3, 128], BF16)
    ycT = dscr("ycT", [HYW, T], F32)
    TWO_PI_LO = 6.283184
    with contextlib.ExitStack() as ph:
        def sb(name, shape, dt):
            return ph.enter_context(nc.sbuf_tensor(name, list(shape), dt))

        def psum(name, shape, dt):
            P.excl.add(name)
            return ph.enter_context(nc.psum_tensor(name, list(shape), dt))
        hid2 = sb("E_hid2", [64, T], F32)
        w3 = sb("E_w3", [64, 2 * HYW], F32)
        P.DMA("sp", w3[:], hy_w3[:, :], W=[w3])
        with contextlib.ExitStack() as ph2:
            def sb2(name, shape, dt):
                return ph2.enter_context(nc.sbuf_tensor(name, list(shape), dt))
            ft = sb2("E_ft", [33, T], F32); hid1 = sb2("E_hid1", [64, T], F32)
            w1 = sb2("E_w1", [33, 64], F32); w2 = sb2("E_w2", [64, 64], F32)
            hp_ = sb2("E_hp", [64, 3], F32); cl_ = sb2("E_cl", [64, 3], F32)
            q = sb2("E_q", [64, 2048], F32); qi = sb2("E_qi", [64, 2048], I32); cm = sb2("E_cm", [64, 2048], F32)
            pq = ph2.enter_context(nc.psum_tensor("E_pq", [64, 2048], F32)); P.excl.add("E_pq")
            P.DMA("sp", ft[:], featT[:, :], W=[ft]); P.DMA("sp", w1[:], hy_w1[:, :], W=[w1])
            P.DMA("sp", w2[:], hy_w2[:, :], W=[w2]); P.DMA("sp", hp_[:], hyp[:, :], W=[hp_])
            P.E("dve", "tensor_scalar", cl_[:, 0:1], hp_[:, 0:1], 1.0 / (2.0 * math.pi), None, ALU.mult, R=[hp_], W=[cl_])
            P.E("dve", "tensor_tensor", cl_[:, 1:3], hp_[:, 1:3], bc(cl_[:, 0:1], [[0, 2]]), ALU.mult, R=[hp_, cl_], W=[cl_])
            for layer in range(2):
                src = ft if layer == 0 else hid1
                wl = w1 if layer == 0 else w2
                dsth = hid1 if layer == 0 else hid2
                for blk in range(4):
                    for s4 in range(4):
                        c0 = blk * 2048 + s4 * 512
                        P.E("pe", "matmul", pq[:, s4 * 512:(s4 + 1) * 512], wl[:], src[:, c0:c0 + 512], start=True, stop=True,
                            R=[wl, src], W=[pq])
                    P.E("dve", "tensor_scalar", q[:], pq[:], cl_[:, 0:1], cl_[:, 1 + layer:2 + layer], ALU.mult, ALU.add,
                        R=[pq, cl_], W=[q])
                    P.E("dve", "tensor_copy", qi[:], q[:], R=[q], W=[qi])
                    P.E("dve", "tensor_copy", cm[:], qi[:], R=[qi], W=[cm])
                    P.E("dve", "tensor_tensor", q[:], q[:], cm[:], ALU.subtract, R=[q, cm], W=[q])
                    P.E("dve", "tensor_single_scalar", cm[:], q[:], 0.5, ALU.is_gt, R=[q], W=[cm])
                    P.E("dve", "tensor_tensor", q[:], q[:], cm[:], ALU.subtract, R=[q, cm], W=[q])
                    P.E("dve", "tensor_single_scalar", cm[:], q[:], -0.5, ALU.is_lt, R=[q], W=[cm])
                    P.E("dve", "tensor_tensor", q[:], q[:], cm[:], ALU.add, R=[q, cm], W=[q])
                    P.E("act", "activation", dsth[:, blk * 2048:(blk + 1) * 2048], q[:], AF.Sin, scale=TWO_PI_LO, R=[q], W=[dsth])
            P.flush()
        with contextlib.ExitStack() as ph2:
            def sb2(name, shape, dt):
                return ph2.enter_context(nc.sbuf_tensor(name, list(shape), dt))
            trb = sb2("E_trb", [128, T], F32); win = sb2("E_win", [128, T], F32)
            ndl = sb2("E_ndl", [128, 8], F32)
            asum = sb2("E_asum", [128, 8, 16], F32); asr = sb2("E_asr", [128, 8], F32)
            fwbs = [sb2("E_fwb%d" % i, [128, 512], BF16) for i in range(3)]
            junk2 = sb2("E_junk", [128, 512], BF16)
            pfs = [ph2.enter_context(nc.psum_tensor("E_pf%d" % i, [128, 512], F32)) for i in range(3)]
            P.excl.update("E_pf%d" % i for i in range(3))
            P.DMA("sp", trb[:], dram_bcast(trow[0:1, :], T), W=[trb])
            P.DMA("sp", ndl[:], negdelta[:, :], W=[ndl])
            P.E("pool", "memset", asum[:], 0.0, W=[asum])
            e3 = 0
            for j in range(8):
                P.E("act", "activation", win[:], trb[:], AF.Exp, scale=ndl[:, j:j + 1], R=[trb, ndl], W=[win])
                for blk in range(16):
                    pf = pfs[e3 % 3]; fwb = fwbs[e3 % 3]; e3 += 1
                    P.E("pe", "matmul", pf[:], w3[:, j * 128:(j + 1) * 128], hid2[:, blk * 512:(blk + 1) * 512], start=True, stop=True,
                        R=[w3, hid2], W=[pf])
                    P.E("dve", "tensor_tensor", fwb[:], pf[:], win[:, blk * 512:(blk + 1) * 512], ALU.mult, R=[pf, win], W=[fwb])
                    if j >= 4 and blk == 0:
                        P.E("pool", "memset", fwb[:, 0:1], 0.0, W=[fwb])
                    P.E("dve", "reduce_sum", asum[:, j, blk:blk + 1], fwb[:], AX.X, apply_absolute_value=True,
                        R=[fwb], W=[asum])
                    P.DMA("sp", filtT[j * 128:(j + 1) * 128, blk * 512:(blk + 1) * 512], fwb[:], R=[fwb])
            P.E("dve", "reduce_sum", asr[:], asum[:], AX.X, R=[asum], W=[asr])
            P.E("dve", "tensor_tensor", asr[:, 0:4], asr[:, 0:4], asr[:, 4:8], ALU.add, R=[asr], W=[asr])
            P.E("dve", "reciprocal", invn[:], asr[:, 0:4], R=[asr], W=[invn])
            P.flush()
    if upto <= 3:
        P.flush(final=True); semctx.close(); G2.close(); G.close(); return nc

    with contextlib.ExitStack() as ph:
        def sb(name, shape, dt):
            return ph.enter_context(nc.sbuf_tensor(name, list(shape), dt))

        def psum(name, shape, dt):
            P.excl.add(name)
            return ph.enter_context(nc.psum_tensor(name, list(shape), dt))
        dftb = sb("F_dft", [128, 7, 128], BF16); twt = sb("F_tw", [128, 3, 128], BF16)
        P.DMA("pool", dftb[:], dft_in[:, :, :], W=[dftb])
        P.DMA("pool", twt[:], tw_in[:, :, :], W=[twt])
        Evs = [sb("F_Ev%d" % i, [128, 1024], BF16) for i in range(2)]
        NBUF = 6
        zins = [sb("F_zin%d" % i, [64, 4, 128], BF16) for i in range(NBUF)]
        S1s = [sb("F_S1%d" % i, [128, 1024], BF16) for i in range(2)]
        S2s = [sb("F_S2%d" % i, [128, 1024], BF16) for i in range(2)]
        Aps = [sb("F_Ap%d" % i, [128, 4, 2, 128], BF16) for i in range(NBUF)]
        Hss = [sb("F_Hs%d" % i, [128, 4, 3, 128], BF16) for i in range(NBUF)]
        Yts = [sb("F_Yt%d" % i, [128, 2, 4, 128], BF16) for i in range(NBUF)]
        Bps = [sb("F_Bp%d" % i, [128, 4, 2, 128], BF16) for i in range(NBUF)]
        ysbs = [sb("F_ys%d" % i, [64, 4, 128], F32) for i in range(NBUF)]
        Hos = [sb("F_Ho%d" % i, [128, 2, 3, 128], BF16) for i in range(NBUF)]
        Xcs = [sb("F_Xc%d" % i, [128, 2, 4, 128], F32) for i in range(NBUF)]
        EvA = [sb("F_EvA%d" % i, [128, 1024], BF16) for i in range(2)]
        EvB = [sb("F_EvB%d" % i, [128, 1024], BF16) for i in range(2)]
        EvC = [sb("F_EvC%d" % i, [128, 1024], BF16) for i in range(2)]
        pA = psum("F_pA", [128, 4, 256], F32); pX = psum("F_pX", [128, 2, 512], F32)
        pB = psum("F_pB", [128, 4, 256], F32); pY = psum("F_pY", [64, 512], F32)
        pA2 = pB

        def v4(t_, off, d1, d2, d3):
            base = t_[:]
            return AP(base.tensor, base.offset + off, [list(base.ap[0]), list(d1), list(d2), list(d3)])
        scn = {"n": 0}

        def tw_mul(src, dst, conj):
            S1 = S1s[scn["n"] % 2]; S2 = S2s[scn["n"] % 2]; scn["n"] += 1
            P.E("dve", "tensor_tensor", v4(S1, 0, [256, 4], [128, 2], [1, 128]), v4(src, 0, [256, 4], [128, 2], [1, 128]),
                v4(twt, 0, [0, 4], [0, 2], [1, 128]), ALU.mult, R=[src, twt], W=[S1])
            if not conj:
                twB = v4(twt, 256, [0, 4], [-128, 2], [1, 128])
            else:
                twB = v4(twt, 128, [0, 4], [128, 2], [1, 128])
            P.E("dve", "tensor_tensor", v4(S2, 0, [256, 4], [128, 2], [1, 128]), v4(src, 128, [256, 4], [-128, 2], [1, 128]),
                twB, ALU.mult, R=[src, twt], W=[S2])
            P.E("dve", "tensor_tensor", dst[:].rearrange("p c r k -> p (c r k)"), S1[:], S2[:], ALU.add, R=[S1, S2], W=[dst])

        def fwd_gen(g, zin, Ap, pA=pA):
            yield
            for c in range(4):
                P.E("pe", "matmul", pA[:, c, :], zin[:, c, :], dftb[0:64, 0:2, :], start=True, stop=True, R=[zin, dftb], W=[pA])
            yield
            ev = EvA[g % 2]
            P.E("act", "activation", ev[:], pA[:].rearrange("p a b -> p (a b)"), AF.Copy, R=[pA], W=[ev])
            yield
            tw_mul(ev, Ap, False)
            yield
            Ar = Ap[:, :, 0, :]; Ai = Ap[:, :, 1, :]
            P.E("pe", "matmul", pX[:, 0, :], dftb[:, 0, :], Ar, start=True, stop=False, R=[dftb, Ap], W=[pX])
            P.E("pe", "matmul", pX[:, 0, :], dftb[:, 2, :], Ai, start=False, stop=True, R=[dftb, Ap], W=[pX])
            P.E("pe", "matmul", pX[:, 1, :], dftb[:, 1, :], Ar, start=True, stop=False, R=[dftb, Ap], W=[pX])
            P.E("pe", "matmul", pX[:, 1, :], dftb[:, 0, :], Ai, start=False, stop=True, R=[dftb, Ap], W=[pX])
            yield

        def filt_gen(g):
            b_ = g % NBUF
            zin, Ap, Ho, Xc = zins[b_], Aps[b_], Hos[b_], Xcs[b_]
            c0 = 2 * g
            P.DMA("sp", zin[:, 0:2, :], filtT[c0:c0 + 2, :].rearrange("c (a b) -> a c b", b=128), W=[zin])
            P.DMA("sp", zin[:, 2:4, :], filtT[HYW + c0:HYW + c0 + 2, :].rearrange("c (a b) -> a c b", b=128), W=[zin])
            yield from fwd_gen(g, zin, Ap, pA if g % 2 == 0 else pA2)
            P.E("act", "activation", Xc[:].rearrange("p r c k -> p (r c k)"), pX[:].rearrange("p a b -> p (a b)"), AF.Copy, R=[pX], W=[Xc])
            yield
            P.E("dve", "tensor_tensor", v4(Ho, 0, [384, 2], [1, 128], [0, 1]), v4(Xc, 0, [128, 2], [1, 128], [0, 1]),
                v4(Xc, 256, [128, 2], [1, 128], [0, 1]), ALU.add, R=[Xc], W=[Ho])
            P.E("dve", "tensor_tensor", v4(Ho, 128, [384, 2], [1, 128], [0, 1]), v4(Xc, 512, [128, 2], [1, 128], [0, 1]),
                v4(Xc, 768, [128, 2], [1, 128], [0, 1]), ALU.subtract, R=[Xc], W=[Ho])
            yield
            P.E("act", "activation", v4(Ho, 256, [384, 2], [1, 128], [0, 1]), v4(Ho, 128, [384, 2], [1, 128], [0, 1]),
                AF.Copy, scale=-1.0, R=[Ho], W=[Ho])
            P.DMA("sp", HsT[:, c0:c0 + 2, :, :], Ho[:], R=[Ho])
            yield

        def conv_gen(g):
            b_ = g % NBUF
            zin, Ap, Hs, Yt, Bp, ysb = zins[b_], Aps[b_], Hss[b_], Yts[b_], Bps[b_], ysbs[b_]
            c0 = 4 * g
            P.DMA("sp", zin[:], zT[c0:c0 + 4, :].rearrange("c (a b) -> a c b", b=128), W=[zin])
            gen = fwd_gen(g, zin, Ap)
            hop = 0
            for _ in gen:
                hop += 1
                if hop == 3:
                    P.DMA("sp", Hs[:], HsT[:, c0:c0 + 4, :, :], W=[Hs])
                yield
            Xe = EvB[g % 2]
            P.E("act", "activation", Xe[:], pX[:].rearrange("p a b -> p (a b)"), AF.Copy, R=[pX], W=[Xe])
            yield
            S1 = S1s[scn["n"] % 2]; S2 = S2s[scn["n"] % 2]; scn["n"] += 1
            P.E("dve", "tensor_tensor", v4(S1, 0, [512, 2], [128, 4], [1, 128]), v4(Xe, 0, [512, 2], [128, 4], [1, 128]),
                v4(Hs, 0, [0, 2], [384, 4], [1, 128]), ALU.mult, R=[Xe, Hs], W=[S1])
            P.E("dve", "tensor_tensor", v4(S2, 0, [512, 2], [128, 4], [1, 128]), v4(Xe, 512, [-512, 2], [128, 4], [1, 128]),
                v4(Hs, 256, [-128, 2], [384, 4], [1, 128]), ALU.mult, R=[Xe, Hs], W=[S2])
            P.E("dve", "tensor_tensor", Yt[:].rearrange("p r c k -> p (r c k)"), S1[:], S2[:], ALU.add, R=[S1, S2], W=[Yt])
            yield
            for c in range(4):
                P.E("pe", "matmul", pB[:, c, :], Yt[:, 0, c, :], dftb[:, 3:5, :], start=True, stop=False, R=[Yt, dftb], W=[pB])
                P.E("pe", "matmul", pB[:, c, :], Yt[:, 1, c, :], dftb[:, 5:7, :], start=False, stop=True, R=[Yt, dftb], W=[pB])
            yield
            ev = EvC[g % 2]
            P.E("act", "activation", ev[:], pB[:].rearrange("p a b -> p (a b)"), AF.Copy, R=[pB], W=[ev])
            yield
            tw_mul(ev, Bp, True)
            yield
            P.E("pe", "matmul", pY[:], dftb[:, 0, 0:64], Bp[:, :, 0, :], start=True, stop=False, R=[dftb, Bp], W=[pY])
            P.E("pe", "matmul", pY[:], dftb[:, 1, 0:64], Bp[:, :, 1, :], start=False, stop=True, R=[dftb, Bp], W=[pY])
            yield
            P.E("act", "activation", ysb[:].rearrange("p c k -> p (c k)"), pY[:], AF.Copy, scale=1.0 / (2 * T), R=[pY], W=[ysb])
            P.DMA("sp", ycT[c0:c0 + 4, :].rearrange("c (a b) -> a c b", b=128), ysb[:], R=[ysb])
            yield

        def run_pipeline(genfn, ngroups, maxlive):
            live = []
            nxt = 0
            while live or nxt < ngroups:
                if nxt < ngroups and len(live) < maxlive:
                    live.append(genfn(nxt)); nxt += 1
                nl = []
                for g_ in live:
                    try:
                        next(g_); nl.append(g_)
                    except StopIteration:
                        pass
                live = nl

        run_pipeline(filt_gen, HYW // 2, 6)
        P.flush()
        if upto > 4:
            run_pipeline(conv_gen, HYW // 4, 6)
            P.flush()
    if upto <= 5:
        P.flush(final=True); semctx.close(); G2.close(); G.close(); return nc

    hyT = dscr("hyT", [HYW, T], BF16)
    with contextlib.ExitStack() as ph:
        def sb(name, shape, dt):
            return ph.enter_context(nc.sbuf_tensor(name, list(shape), dt))
        NB = 2048
        hbias = sb("G_hb", [128, 4], F32)
        P.DMA("sp", hbias[:], hy_biasc[:, :], W=[hbias])
        ycs = [sb("G_yc%d" % i, [128, NB], F32) for i in range(2)]
        zs = [sb("G_z%d" % i, [128, NB], BF16) for i in range(2)]
        x0s = [sb("G_x0%d" % i, [128, NB], BF16) for i in range(2)]
        hys = [sb("G_hy%d" % i, [128, NB], BF16) for i in range(2)]
        it = 0
        for ct in range(4):
            for blk in range(T // NB):
                b_ = it % 2; it += 1
                sl_ = (slice(ct * 128, (ct + 1) * 128), slice(blk * NB, (blk + 1) * NB))
                P.DMA("sp", ycs[b_][:], ycT[sl_], W=[ycs[b_]])
                P.DMA("sp", zs[b_][:], zT[sl_], W=[zs[b_]])
                P.DMA("sp", x0s[b_][:], x0T[sl_], W=[x0s[b_]])
                P.E("act", "activation", ycs[b_][:], ycs[b_][:], AF.Identity, scale=invn[:, ct:ct + 1], R=[ycs[b_], invn], W=[ycs[b_]])
                P.E("dve", "scalar_tensor_tensor", ycs[b_][:], zs[b_][:], hbias[:, ct:ct + 1], ycs[b_][:], ALU.mult, ALU.add,
                    R=[zs[b_], hbias, ycs[b_]], W=[ycs[b_]])
                P.E("dve", "tensor_tensor", hys[b_][:], ycs[b_][:], x0s[b_][:], ALU.mult, R=[ycs[b_], x0s[b_]], W=[hys[b_]])
                P.DMA("sp", hyT[sl_], hys[b_][:], R=[hys[b_]])
        P.flush()
    if upto <= 6:
        P.flush(final=True); semctx.close(); G2.close(); G.close(); return nc

    C0 = math.exp(-0.5)
    seqs = {}
    for sk, Tn in (("c", TC), ("m", T)):
        dd = {"T": Tn}
        for nm in ("Af", "Bf", "Kf", "Rf"):
            dd[nm] = [dscr("%s%d%s" % (nm, d, sk), [RWW, Tn], BF16) for d in range(2)]
        dd["V"] = dscr("VT" + sk, [RWW, Tn], BF16)
        dd["gC"] = [dscr("gC%d%s" % (d, sk), [RWW, Tn // CH], F32) for d in range(2)]
        seqs[sk] = dd
    bonT = dscr("bonT", [RWW, T], F32); grwT = dscr("grwT", [RWW, T], BF16)
    P.E("pool", "memset", blk64[:], 0.0, W=[blk64])
    P.E("pool", "memset", blk64[0:64, 0:64], 1.0, W=[blk64])
    P.E("pool", "memset", blk64[64:128, 64:128], 1.0, W=[blk64])
    with contextlib.ExitStack() as ph:
        def sb(name, shape, dt):
            return ph.enter_context(nc.sbuf_tensor(name, list(shape), dt))

        def psum(name, shape, dt):
            P.excl.add(name)
            return ph.enter_context(nc.psum_tensor(name, list(shape), dt))
        NK = 512
        muc = sb("H_mu", [128, 14, 2], F32); c0c = sb("H_c0", [128, 14], F32)
        wa = sb("H_wa", [128, 4, 4], F32); rc = sb("H_rc", [128, 4, 5], F32)
        UPb = sb("H_up", [128, 2, 512], BF16); gup = sb("H_gup", [128, 512], BF16)
        rmask = sb("H_rmask", [128, NK], F32); nhb = sb("H_nhb", [128, NK], F32)
        P.DMA("sp", muc[:], mucol[:, :, :], W=[muc]); P.DMA("sp", wa[:], rw_w0a0[:, :, :], W=[wa])
        P.DMA("sp", rc[:], rwc[:, :, :], W=[rc])
        P.DMA("pool", UPb[:], rw_up[:, :, :], W=[UPb]); P.DMA("pool", gup[:], rw_g_up[:, :], W=[gup])
        P.E("pool", "memset", rmask[:], 1.0, W=[rmask])
        P.E("pool", "memset", rmask[:].rearrange("p (c t) -> p c t", t=CH)[:, :, 0:1], 0.0, W=[rmask])
        P.E("pool", "memset", nhb[:], -0.5, W=[nhb])
        P.E("dve", "tensor_tensor", c0c[:], muc[:, :, 0], muc[:, :, 1], ALU.add, R=[muc], W=[c0c])
        P.E("dve", "tensor_scalar", c0c[:], c0c[:], -1.0, 1.0, ALU.mult, ALU.add, R=[c0c], W=[c0c])

        raw_l = [sb("H_rawl%d" % i, [128, NK + 2], BF16) for i in range(2)]
        raw_t = [sb("H_rawt%d" % i, [128, NK + 2], BF16) for i in range(4)]
        m12 = sb("H_m12", [128, NK], F32); m13 = sb("H_m13", [128, NK], F32)
        lwa = sb("H_lwa", [128, NK], BF16); lgs = sb("H_lgs", [128, NK], BF16)
        CT = [[sb("H_%s%d" % (nm, i), [128, NK], F32) for nm in ("rm", "km", "vm", "kkr", "sq", "ssb", "kk")] for i in range(2)]
        sg_ = [sb("H_sg%d" % i, [128, NK], F32) for i in range(2)]; ad_ = [sb("H_ad%d" % i, [128, NK], F32) for i in range(2)]
        cs_ = [sb("H_cs%d" % i, [128, NK], F32) for i in range(2)]; ce_ = [sb("H_ce%d" % i, [128, NK], F32) for i in range(2)]
        ci_ = [sb("H_ci%d" % i, [128, NK], F32) for i in range(2)]
        EA_ = [sb("H_EA%d" % i, [128, NK], F32) for i in range(2)]; EI_ = [sb("H_EI%d" % i, [128, NK], F32) for i in range(2)]
        EN_ = [sb("H_EN%d" % i, [128, NK], F32) for i in range(2)]
        kb_ = [sb("H_kb%d" % i, [128, NK], F32) for i in range(2)]; tt_ = [sb("H_tt%d" % i, [128, NK], F32) for i in range(2)]
        kmod_ = [sb("H_kmod%d" % i, [128, NK], F32) for i in range(2)]
        rk_ = [sb("H_rk%d" % i, [128, NK], F32) for i in range(2)]; bon = sb("H_bon", [128, NK], F32)
        gct = [sb("H_gct%d" % i, [128, 8], F32) for i in range(2)]
        ob = {nm: [sb("H_o%s%d" % (nm, i), [128, NK], BF16) for i in range(2)] for nm in ("Af", "Bf", "Kf", "Rf", "V", "g")}
        ps_w = [psum("H_pw%d" % i, [128, NK], F32) for i in range(2)]
        ps_a = [psum("H_pa%d" % i, [128, NK], F32) for i in range(2)]
        ps_ss = psum("H_pss", [128, NK], F32); ps_g = psum("H_pg", [128, NK], F32); ps_bon = psum("H_pbon", [128, NK], F32)
        cn = {"o": 0, "l": 0, "t": 0}

        def load_mix(src, row, chunk14, t0, N, Tn, raw, out, func=None):
            lo = max(t0 - 1, 0); hi = min(t0 + N + 1, Tn)
            P.DMA("sp", raw[:, lo - (t0 - 1):hi - (t0 - 1)], src[row:row + 128, lo:hi], W=[raw])
            if t0 == 0:
                P.E("pool", "memset", raw[:, 0:1], 0.0, W=[raw])
            if hi == Tn:
                P.E("pool", "memset", raw[:, N + 1:N + 2], 0.0, W=[raw])
            P.E("act", "activation", out[:, 0:N], raw[:, 1:N + 1], AF.Identity, scale=c0c[:, chunk14:chunk14 + 1], R=[raw, c0c], W=[out])
            P.E("dve", "scalar_tensor_tensor", out[:, 0:N], raw[:, 0:N], muc[:, chunk14, 0:1], out[:, 0:N], ALU.mult, ALU.add,
                R=[raw, muc, out], W=[out])
            P.E("dve", "scalar_tensor_tensor", out[:, 0:N], raw[:, 2:N + 2], muc[:, chunk14, 1:2], out[:, 0:N], ALU.mult, ALU.add,
                R=[raw, muc, out], W=[out])

        def rw_prep(src, row0, sk):
            dd = seqs[sk]; Tn = dd["T"]; main = sk == "m"
            for blk in range((Tn + NK - 1) // NK):
                t0 = blk * NK; N = min(NK, Tn - t0); nch = N // CH
                tsl = slice(t0, t0 + N)
                load_mix(src, row0 + 1536, 12, t0, N, Tn, raw_l[0], m12)
                P.E("act", "activation", lwa[0:64, 0:N], m12[0:64, 0:N], AF.Tanh, R=[m12], W=[lwa])
                P.E("dve", "tensor_copy", lwa[64:128, 0:N], m12[64:128, 0:N], R=[m12], W=[lwa])
                if main:
                    load_mix(src, row0 + 1664, 13, t0, N, Tn, raw_l[1], m13)
                    P.E("act", "activation", lgs[:, 0:N], m13[:, 0:N], AF.Sigmoid, R=[m13], W=[lgs])
                def common_gen(hp, par):
                    rm, km, vm, kkr, sq, ssb, kk = CT[par]
                    rsl = slice(hp * 128, (hp + 1) * 128)
                    if main:
                        load_mix(src, row0 + hp * 128, hp, t0, N, Tn, raw_t[0], rm)
                    load_mix(src, row0 + 512 + hp * 128, 4 + hp, t0, N, Tn, raw_t[1], km)
                    yield
                    load_mix(src, row0 + 1024 + hp * 128, 8 + hp, t0, N, Tn, raw_t[2], vm)
                    o = cn["o"] % 2; cn["o"] += 1
                    P.E("act", "activation", ob["V"][o][:, 0:N], vm[:, 0:N], AF.Copy, R=[vm], W=[ob["V"][o]])
                    P.DMA("sp", dd["V"][rsl, tsl], ob["V"][o][:, 0:N], R=[ob["V"][o]])
                    yield
                    P.E("dve", "tensor_scalar", kkr[:, 0:N], km[:, 0:N], rc[:, hp, 0:1], None, ALU.mult, R=[km, rc], W=[kkr])
                    P.E("act", "activation", sq[:, 0:N], kkr[:, 0:N], AF.Square, R=[kkr], W=[sq])
                    yield
                    P.E("pe", "matmul", ps_ss[:, 0:N], blk64[:], sq[:, 0:N], start=True, stop=True, R=[blk64, sq], W=[ps_ss])
                    P.E("dve", "tensor_scalar", ssb[:, 0:N], ps_ss[:, 0:N], 1e-24, None, ALU.max, R=[ps_ss], W=[ssb])
                    P.E("act", "activation", ssb[:, 0:N], ssb[:, 0:N], AF.Sqrt, R=[ssb], W=[ssb])
                    yield
                    P.E("dve", "reciprocal", ssb[:, 0:N], ssb[:, 0:N], R=[ssb], W=[ssb])
                    P.E("dve", "tensor_tensor", kk[:, 0:N], kkr[:, 0:N], ssb[:, 0:N], ALU.mult, R=[kkr, ssb], W=[kk])
                    if main:
                        P.E("pe", "matmul", ps_g[:, 0:N], gup[:, rsl], lgs[:, 0:N], start=True, stop=True, R=[gup, lgs], W=[ps_g])
                        P.E("act", "activation", ob["g"][o][:, 0:N], ps_g[:, 0:N], AF.Copy, R=[ps_g], W=[ob["g"][o]])
                        P.DMA("sp", grwT[rsl, tsl], ob["g"][o][:, 0:N], R=[ob["g"][o]])
                    yield
                for _ in common_gen(0, 0):
                    pass
                for hp in range(4):
                    par = hp % 2
                    rsl = slice(hp * 128, (hp + 1) * 128)
                    o = 0
                    def dgen(d, hp=hp, N=N, nch=nch, rsl=rsl, tsl=tsl, t0=t0, o=o, par=par):
                        pw = ps_w[d]; pa = ps_a[d]
                        rm, km, vm, kkr, sq, ssb, kk = CT[par]
                        P.E("pe", "matmul", pw[:, 0:N], UPb[0:64, d, rsl], lwa[0:64, 0:N], start=True, stop=True, R=[UPb, lwa], W=[pw])
                        P.E("pe", "matmul", pa[:, 0:N], UPb[64:128, d, rsl], lwa[64:128, 0:N], start=True, stop=True, R=[UPb, lwa], W=[pa])
                        yield
                        P.E("act", "activation", sg_[d][:, 0:N], pw[:, 0:N], AF.Sigmoid, bias=wa[:, hp, d:d + 1], R=[pw, wa], W=[sg_[d]])
                        P.E("act", "activation", ad_[d][:, 0:N], pa[:, 0:N], AF.Sigmoid, bias=wa[:, hp, 2 + d:3 + d], R=[pa, wa], W=[ad_[d]])
                        yield
                        P.E("dve", "tensor_tensor_scan", cs_[d][:, 0:N], rmask[:, 0:N], sg_[d][:, 0:N], 0.0, ALU.mult, ALU.add, R=[rmask, sg_[d]], W=[cs_[d]])
                        if d == 0:
                            cis = cs_[d]
                            P.E("dve", "tensor_tensor", ce_[d][:, 0:N], cs_[d][:, 0:N], sg_[d][:, 0:N], ALU.subtract, R=[cs_[d], sg_[d]], W=[ce_[d]])
                        else:
                            cis = ci_[d]
                            tot = bc(cs_[d][:, CH - 1:CH], [[CH, nch], [0, CH]])
                            P.E("dve", "tensor_tensor", ce_[d][:, 0:N].rearrange("p (c t) -> p c t", t=CH), tot,
                                cs_[d][:, 0:N].rearrange("p (c t) -> p c t", t=CH), ALU.subtract, R=[cs_[d]], W=[ce_[d]])
                            P.E("dve", "tensor_tensor", ci_[d][:, 0:N], ce_[d][:, 0:N], sg_[d][:, 0:N], ALU.add, R=[ce_[d], sg_[d]], W=[ci_[d]])
                        g_ = gct[d]
                        P.E("act", "activation", g_[:, 0:nch], bc(cs_[d][:, CH - 1:CH], [[CH, nch]]), AF.Exp, scale=-C0, R=[cs_[d]], W=[g_])
                        P.DMA("sp", dd["gC"][d][rsl, t0 // CH:t0 // CH + nch], g_[:, 0:nch], R=[g_])
                        yield
                        P.E("act", "activation", EA_[d][:, 0:N], ce_[d][:, 0:N], AF.Exp, scale=-C0, R=[ce_[d]], W=[EA_[d]])
                        P.E("act", "activation", EN_[d][:, 0:N], cis[:, 0:N], AF.Exp, scale=C0, R=[cis], W=[EN_[d]])
                        yield
                        if main:
                            P.E("act", "activation", EI_[d][:, 0:N], cis[:, 0:N], AF.Exp, scale=-C0, R=[cis], W=[EI_[d]])
                        oA, oB, oK, oR = ob["Af"][d], ob["Bf"][d], ob["Kf"][d], ob["Rf"][d]
                        P.E("dve", "scalar_tensor_tensor", oA[:, 0:N], kk[:, 0:N], -1.0, EA_[d][:, 0:N], ALU.mult, ALU.mult, R=[kk, EA_[d]], W=[oA])
                        P.E("dve", "tensor_tensor", kb_[d][:, 0:N], kk[:, 0:N], ad_[d][:, 0:N], ALU.mult, R=[kk, ad_[d]], W=[kb_[d]])
                        P.E("dve", "tensor_tensor", oB[:, 0:N], kb_[d][:, 0:N], EN_[d][:, 0:N], ALU.mult, R=[kb_[d], EN_[d]], W=[oB])
                        P.E("dve", "tensor_scalar", tt_[d][:, 0:N], ad_[d][:, 0:N], -1.0, rc[:, hp, 1:2], ALU.add, ALU.mult, R=[ad_[d], rc], W=[tt_[d]])
                        P.E("dve", "scalar_tensor_tensor", kmod_[d][:, 0:N], tt_[d][:, 0:N], 1.0, km[:, 0:N], ALU.add, ALU.mult, R=[tt_[d], km], W=[kmod_[d]])
                        P.E("dve", "tensor_tensor", oK[:, 0:N], kmod_[d][:, 0:N], EN_[d][:, 0:N], ALU.mult, R=[kmod_[d], EN_[d]], W=[oK])
                        P.DMA("sp", dd["Af"][d][rsl, tsl], oA[:, 0:N], R=[oA])
                        P.DMA("sp", dd["Bf"][d][rsl, tsl], oB[:, 0:N], R=[oB])
                        P.DMA("sp", dd["Kf"][d][rsl, tsl], oK[:, 0:N], R=[oK])
                        if main:
                            P.E("dve", "tensor_tensor", oR[:, 0:N], rm[:, 0:N], EI_[d][:, 0:N], ALU.mult, R=[rm, EI_[d]], W=[oR])
                            P.DMA("sp", dd["Rf"][d][rsl, tsl], oR[:, 0:N], R=[oR])
                            P.E("dve", "scalar_tensor_tensor", rk_[d][:, 0:N], rm[:, 0:N], rc[:, hp, 2:3], kmod_[d][:, 0:N], ALU.mult, ALU.mult,
                                R=[rm, rc, kmod_[d]], W=[rk_[d]])
                            P.E("pe", "matmul", ps_bon[:, 0:N], blk64[:], rk_[d][:, 0:N], start=(d == 0), stop=(d == 1), R=[blk64, rk_[d]], W=[ps_bon])
                    gens = [dgen(0), dgen(1)] + ([common_gen(hp + 1, 1 - par)] if hp + 1 < 4 else [])
                    alive = True
                    while alive:
                        alive = False
                        for g_ in gens:
                            try:
                                next(g_); alive = True
                            except StopIteration:
                                pass
                    vm = CT[par][2]
                    if main:
                        P.E("dve", "tensor_tensor", bon[:, 0:N], ps_bon[:, 0:N], vm[:, 0:N], ALU.mult, R=[ps_bon, vm], W=[bon])
                        P.DMA("sp", bonT[rsl, tsl], bon[:, 0:N], R=[bon])

        rw_prep(ccT, 0, "c")
        rw_prep(colsT, 1536, "m")
        P.flush()
    if upto <= 7:
        P.flush(final=True); semctx.close(); G2.close(); G.close(); return nc

    yT = [dscr("yT%d" % d, [RWW, T], F32) for d in range(2)]
    SCH = 8
    with contextlib.ExitStack() as ph:
        def sb(name, shape, dt):
            return ph.enter_context(nc.sbuf_tensor(name, list(shape), dt))

        def psum(name, shape, dt):
            P.excl.add(name)
            return ph.enter_context(nc.psum_tensor(name, list(shape), dt))
        ones_f = sb("I_ones", [128, 256], F32)
        P.E("pool", "memset", ones_f[:], 1.0, W=[ones_f])
        MA = [sb("I_MA%d" % d, [128, 256], F32) for d in range(2)]
        MB = [sb("I_MB%d" % d, [128, 256], F32) for d in range(2)]
        MC = [sb("I_MC%d" % d, [128, 2, 64], F32) for d in range(2)]

        def asel(out, inp, pat, cm, cmp, res):
            P.E("pool", "affine_select", out, inp, pat, cmp, 0.0, base=0, channel_multiplier=cm, R=[ones_f], W=[res])
        for d in range(2):
            pf, cf = ([[1, 128]], -1) if d == 0 else ([[-1, 128]], 1)
            pr, cr = ([[-1, 128]], 1) if d == 0 else ([[1, 128]], -1)
            asel(MA[d][:, 0:128], ones_f[:, 0:128], pf, cf, ALU.is_gt, MA[d])
            asel(MA[d][:, 128:256], ones_f[:, 0:128], pr, cr, ALU.is_gt, MA[d])
            P.E("pool", "memset", MB[d][:, 0:128], 1.0, W=[MB[d]])
            asel(MB[d][:, 128:256], ones_f[:, 0:128], pr, cr, ALU.is_gt, MB[d])
            pi_, ci_ = ([[1, 64]], -1) if d == 0 else ([[-1, 64]], 1)
            for half in range(2):
                for w_ in range(2):
                    asel(MC[d][half * 64:(half + 1) * 64, w_, :], ones_f[half * 64:(half + 1) * 64, 0:64], pi_, ci_, ALU.is_ge, MC[d])

        NS = SCH * CH
        lanes = []
        for l in range(4):
            L_ = {"d": l % 2}
            L_["st"] = [{nm: sb("I_st%s%d_%d" % (nm, l, i), [128, NS], BF16) for nm in ("Rf",)} for i in range(2)]
            L_["bd"] = [{nm: sb("I_bd%s%d_%d" % (nm, l, i), [128, SCH, 128], BF16) for nm in ("Af", "Bf", "Kf", "V")} for i in range(2)]
            L_["gc"] = [sb("I_gc%d_%d" % (l, i), [128, SCH], F32) for i in range(2)]
            L_["yb"] = [sb("I_yb%d_%d" % (l, i), [128, NS], F32) for i in range(2)]
            L_["XX"] = [sb("I_XX%d_%d" % (l, i), [128, 2, 128], BF16) for i in range(2)]
            L_["YY"] = [sb("I_YY%d_%d" % (l, i), [128, 2, 128], BF16) for i in range(2)]
            L_["TM"] = [sb("I_TM%d_%d" % (l, i), [128, 3, 128], BF16) for i in range(2)]
            L_["BRKR"] = [sb("I_BK%d_%d" % (l, i), [128, 2, 64], BF16) for i in range(2)]
            L_["GQ"] = [sb("I_GQ%d_%d" % (l, i), [128, 192], BF16) for i in range(2)]
            L_["HZ"] = [sb("I_HZ%d_%d" % (l, i), [128, 192], BF16) for i in range(2)]
            L_["S"] = [sb("I_S%d_%d" % (l, i), [128, 128], BF16) for i in range(2)]
            L_["ps"] = [psum("I_ps%d_%d" % (l, i), [128, 512], F32) for i in range(2)]
            for i in range(2):
                for nm in ("Af", "Bf", "Kf", "V"):
                    P.E("pool", "memset", L_["bd"][i][nm][:], 0.0, W=[L_["bd"][i][nm]])
            lanes.append(L_)

        def load_super(L_, hp, sk, s_idx, setidx):
            dd = seqs[sk]; d = L_["d"]; main = sk == "m"
            Tn = dd["T"]; N = min(NS, Tn); nch = N // CH
            t0 = s_idx * NS
            rsl = slice(hp * 128, (hp + 1) * 128)
            st = L_["st"][setidx]; bd = L_["bd"][setidx]
            for nm in ("Af", "Bf", "Kf", "V"):
                srcT = dd["V"] if nm == "V" else dd[nm][d]
                for half in range(2):
                    r0 = hp * 128 + half * 64
                    P.DMA("sp", bd[nm][half * 64:(half + 1) * 64, 0:nch, half * 64:(half + 1) * 64],
                          srcT[r0:r0 + 64, t0:t0 + N].rearrange("p (c t) -> p c t", t=CH), W=[bd[nm]])
            if main:
                P.DMA("sp", st["Rf"][:, 0:N], dd["Rf"][d][rsl, t0:t0 + N], W=[st["Rf"]])
            P.DMA("sp", L_["gc"][setidx][:, 0:nch], dd["gC"][d][rsl, t0 // CH:t0 // CH + nch], W=[L_["gc"][setidx]])

        cstate = [{"n": 0, "s": 0} for _ in range(4)]

        def chunk_gen(L_, l, sk, setidx, c, main):
            d = L_["d"]; cs_ = cstate[l]
            par = cs_["n"] % 2; cs_["n"] += 1
            st = L_["st"][setidx]; bd = L_["bd"][setidx]
            psA, psB = L_["ps"]
            rA0 = rA1 = psA; rB0 = rB1 = psB; rC1 = psA; rC0 = psA
            Af = bd["Af"][:, c, :]; Bf = bd["Bf"][:, c, :]; Kf = bd["Kf"][:, c, :]; Vf = bd["V"][:, c, :]
            Rfc = st["Rf"][:, c * CH:(c + 1) * CH]
            XX = L_["XX"]; YY = L_["YY"]; TM = L_["TM"][par]; BRKR = L_["BRKR"][par]; GQ = L_["GQ"][par]; HZ = L_["HZ"][par]
            mm = lambda out, lhsT, rhs, st_, sp_, R, W: P.E("pe", "matmul", out, lhsT, rhs, start=st_, stop=sp_, R=R, W=W)
            mm(psA[:, 0:128], Bf, Af, True, True, [bd["Bf"], bd["Af"]], [rA0])
            mm(psA[:, 128:256], Af, Bf, True, True, [bd["Bf"], bd["Af"]], [rA0])
            mm(psA[:, 256:384], Af, identb[:], True, True, [bd["Af"], identb], [rA1])
            mm(psA[:, 384:512], Af, Kf, True, True, [bd["Af"], bd["Kf"]], [rA1])
            if main:
                mm(psB[:, 0:64], Bf, Rfc, True, True, [bd["Bf"], st["Rf"]], [rB0])
                mm(psB[:, 64:128], Kf, Rfc, True, True, [bd["Kf"], st["Rf"]], [rB0])
            mm(psB[:, 128:256], Bf, identb[:], True, True, [bd["Bf"], identb], [rB1])
            mm(psB[:, 256:384], Kf, identb[:], True, True, [bd["Kf"], identb], [rB1])
            mm(psB[:, 384:512], Vf, identb[:], True, True, [bd["V"], identb], [rB1])
            x0 = XX[0]; y0 = YY[0]
            P.E("dve", "tensor_tensor", x0[:].rearrange("p a b -> p (a b)"), psA[:, 0:256], MA[d][:], ALU.mult, R=[rA0, MA[d]], W=[x0])
            P.E("dve", "tensor_tensor", y0[:].rearrange("p a b -> p (a b)"), psA[:, 256:512], MB[d][:], ALU.mult, R=[rA1, MB[d]], W=[y0])
            if main:
                P.E("dve", "tensor_tensor", BRKR[:].rearrange("p a b -> p (a b)"), psB[:, 0:128], MC[d][:].rearrange("p a b -> p (a b)"),
                    ALU.mult, R=[rB0, MC[d]], W=[BRKR])
            P.E("act", "activation", TM[:].rearrange("p a b -> p (a b)"), psB[:, 128:512], AF.Copy, R=[rB1], W=[TM])
            yield
            for k in range(6):
                xk = XX[k % 2]; xn = XX[(k + 1) % 2]; yk = YY[k % 2]; yn = YY[(k + 1) % 2]
                ykf = yk[:].rearrange("p a b -> p (a b)")
                mm(psA[:, 0:256], xk[:, 0, :], ykf, True, k < 5, [xk, yk], [rC1])
                if k == 5:
                    mm(psA[:, 0:256], identb[:], ykf, False, True, [identb, yk], [rC1])
                if k < 5:
                    mm(psA[:, 256:384], xk[:, 1, :], xk[:, 0, :], True, True, [xk], [rC0])
                    if k < 4:
                        mm(psA[:, 384:512], xk[:, 0, :], xk[:, 1, :], True, True, [xk], [rC0])
                if k < 5:
                    P.E("dve", "tensor_tensor", yn[:].rearrange("p a b -> p (a b)"), psA[:, 0:256], ykf, ALU.add, R=[rC1, yk], W=[yn])
                else:
                    P.E("act", "activation", yn[:].rearrange("p a b -> p (a b)"), psA[:, 0:256], AF.Copy, R=[rC1], W=[yn])
                if k < 5:
                    w_ = 256 if k < 4 else 128
                    P.E("act", "activation", xn[:].rearrange("p a b -> p (a b)")[:, 0:w_], psA[:, 256:256 + w_], AF.Copy, R=[rC0], W=[xn])
                yield
            yf = YY[0]
            ApT = yf[:, 0, :]; WT = yf[:, 1, :]
            Bt = TM[:, 0, :]; Kt = TM[:, 1, :]; Vt = TM[:, 2, :]
            mm(psB[:, 0:128], ApT, Bt, True, False, [yf, TM], [psB])
            mm(psB[:, 0:128], identb[:], identb[:], False, True, [identb], [psB])
            if main:
                mm(psB[:, 128:192], ApT, BRKR[:, 0, :], True, False, [yf, BRKR], [psB])
                mm(psB[:, 128:192], identb[:], Rfc, False, True, [identb, st["Rf"]], [psB])
            mm(psB[:, 192:320], WT, Bt, True, False, [yf, TM], [psB])
            mm(psB[:, 192:320], identb[:], Kt, False, True, [identb, TM], [psB])
            if main:
                mm(psB[:, 320:384], WT, BRKR[:, 0, :], True, False, [yf, BRKR], [psB])
                mm(psB[:, 320:384], identb[:], BRKR[:, 1, :], False, True, [identb, BRKR], [psB])
            wq = 192 if main else 128
            P.E("act", "activation", GQ[:, 0:wq], psB[:, 0:wq], AF.Copy, R=[psB], W=[GQ])
            P.E("dve", "tensor_copy", HZ[:, 0:wq], psB[:, 192:192 + wq], R=[psB], W=[HZ])
            yield
            Sc = L_["S"][cs_["s"] % 2]; Sn = L_["S"][(cs_["s"] + 1) % 2]; cs_["s"] += 1
            if main:
                mm(psA[:, 0:64], Sc[:], GQ[:, 128:192], True, False, [Sc, GQ], [psA])
                mm(psA[:, 0:64], Vt, HZ[:, 128:192], False, True, [TM, HZ], [psA])
                yb = L_["yb"][setidx]
                P.E("dve", "tensor_copy", yb[:, c * CH:(c + 1) * CH], psA[:, 0:64], R=[psA], W=[yb])
            mm(psB[:, 384:512], GQ[:, 0:128], Sc[:], True, False, [GQ, Sc], [psB])
            mm(psB[:, 384:512], HZ[:, 0:128], Vt, False, True, [HZ, TM], [psB])
            P.E("act", "activation", Sn[:], psB[:, 384:512], AF.Identity, scale=L_["gc"][setidx][:, c:c + 1], R=[psB, L_["gc"][setidx]], W=[Sn])
            yield

        import os
        for hpp in range(int(os.environ.get('SCAN_NHP', 2))):
            sched = []
            for l in range(4):
                items = []
                nsup = T // NS
                if l % 2 == 0:
                    items.append(("c", 0, list(range(TC // CH))))
                    for s_ in range(nsup):
                        items.append(("m", s_, list(range(SCH))))
                else:
                    items.append(("c", 0, list(range(TC // CH - 1, -1, -1))))
                    for s_ in range(nsup - 1, -1, -1):
                        items.append(("m", s_, list(range(SCH - 1, -1, -1))))
                sched.append(items)
                cstate[l]["s"] = 0
                P.E("pool", "memset", lanes[l]["S"][0][:], 0.0, W=[lanes[l]["S"][0]])
            hps = [2 * hpp + l // 2 for l in range(4)]
            nitems = min(len(sched[0]), int(os.environ.get('SCAN_NITEMS', 999)))
            for l in range(4):
                load_super(lanes[l], hps[l], sched[l][0][0], sched[l][0][1], 0)
            for it_ in range(nitems):
                setidx = it_ % 2
                if it_ + 1 < nitems:
                    for l in range(4):
                        load_super(lanes[l], hps[l], sched[l][it_ + 1][0], sched[l][it_ + 1][1], (it_ + 1) % 2)
                nchunks = len(sched[0][it_][2])
                for ci_ in range(nchunks):
                    gens = []
                    for l in range(4):
                        sk, s_idx, order = sched[l][it_]
                        gens.append(chunk_gen(lanes[l], l, sk, setidx, order[ci_], sk == "m"))
                    alive = True
                    while alive:
                        alive = False
                        for g_ in gens:
                            try:
                                next(g_); alive = True
                            except StopIteration:
                                pass
                for l in range(4):
                    sk, s_idx, order = sched[l][it_]
                    if sk == "m":
                        rsl = slice(hps[l] * 128, (hps[l] + 1) * 128)
                        P.DMA("sp", yT[l % 2][rsl, s_idx * NS:(s_idx + 1) * NS], lanes[l]["yb"][setidx][:], R=[lanes[l]["yb"][setidx]])
        P.flush()
    if upto <= 8:
        P.flush(final=True); semctx.close(); G2.close(); G.close(); return nc

    xe = [dscr("xe%d" % e, [CAP, ROWP], BF16) for e in range(NE)]; acc = dscr("acc", [T, D], F32)
    rwoT = dscr("rwoT", [RWW, T], BF16)
    with contextlib.ExitStack() as ph:
        def sb(name, shape, dt):
            return ph.enter_context(nc.sbuf_tensor(name, list(shape), dt))

        def psum(name, shape, dt):
            P.excl.add(name)
            return ph.enter_context(nc.psum_tensor(name, list(shape), dt))
        NK = 512
        rc = sb("J_rc", [128, 4, 5], F32); nhb = sb("J_nhb", [128, NK], F32)
        P.DMA("sp", rc[:], rwc[:, :, :], W=[rc]); P.E("pool", "memset", nhb[:], -0.5, W=[nhb])
        zb = sb("K_zb", [128, 8 * ROWP], BF16); zf = sb("K_zf", [128, 4096], F32)
        P.E("pool", "memset", zb[:], 0.0, W=[zb]); P.E("pool", "memset", zf[:], 0.0, W=[zf])
        acc_flat = acc.rearrange("t d -> (t d)").rearrange("(p q) -> p q", p=128)
        for i in range(16):
            P.DMA("act", xe[i].rearrange("s r -> (s r)").rearrange("(p q) -> p q", p=128), zb[:], R=[zb])
            P.DMA("act", acc_flat[:, i * 4096:(i + 1) * 4096], zf[:], R=[zf])

        pm = psum("J_pm", [128, NK], F32); pq2 = psum("J_pq", [128, NK], F32)
        NR = 3
        y0s = [sb("J_y0r%d" % i, [128, NK], F32) for i in range(NR)]; y1s = [sb("J_y1r%d" % i, [128, NK], F32) for i in range(NR)]
        bns = [sb("J_bnr%d" % i, [128, NK], F32) for i in range(NR)]; gts = [sb("J_gtr%d" % i, [128, NK], BF16) for i in range(NR)]
        outs = [sb("J_outr%d" % i, [128, NK], BF16) for i in range(NR)]
        sqs = [sb("J_sqr%d" % i, [128, NK], F32) for i in range(NR)]; mns = [sb("J_mnr%d" % i, [128, NK], F32) for i in range(NR)]
        msqs = [sb("J_msqr%d" % i, [128, NK], F32) for i in range(NR)]; vars_ = [sb("J_varr%d" % i, [128, NK], F32) for i in range(NR)]
        yns = [sb("J_ynr%d" % i, [128, NK], F32) for i in range(NR)]
        nbj = T // NK

        def j_gen(it):
            hp = it // nbj; blk = it % nbj; b_ = it % NR
            sl_ = (slice(hp * 128, (hp + 1) * 128), slice(blk * NK, (blk + 1) * NK))
            y0, y1, bn, gt, ot = y0s[b_], y1s[b_], bns[b_], gts[b_], outs[b_]
            sq, mn, msq, var, yn = sqs[b_], mns[b_], msqs[b_], vars_[b_], yns[b_]
            P.DMA("sp", y0[:], yT[0][sl_], W=[y0]); P.DMA("sp", y1[:], yT[1][sl_], W=[y1])
            P.DMA("sp", bn[:], bonT[sl_], W=[bn]); P.DMA("sp", gt[:], grwT[sl_], W=[gt])
            yield
            P.E("dve", "tensor_tensor", y0[:], y0[:], y1[:], ALU.add, R=[y0, y1], W=[y0])
            yield
            P.E("pe", "matmul", pm[:], blk64[:], y0[:], start=True, stop=True, R=[blk64, y0], W=[pm])
            P.E("act", "activation", sq[:], y0[:], AF.Square, R=[y0], W=[sq])
            yield
            P.E("pe", "matmul", pq2[:], blk64[:], sq[:], start=True, stop=True, R=[blk64, sq], W=[pq2])
            P.E("act", "activation", mn[:], pm[:], AF.Copy, scale=1.0 / 64, R=[pm], W=[mn])
            P.E("act", "activation", msq[:], pm[:], AF.Square, scale=1.0 / 64, R=[pm], W=[msq])
            yield
            P.E("dve", "scalar_tensor_tensor", var[:], pq2[:], 1.0 / 64, msq[:], ALU.mult, ALU.subtract, R=[pq2, msq], W=[var])
            P.E("dve", "tensor_scalar", var[:], var[:], LN_EPS, None, ALU.add, R=[var], W=[var])
            yield
            P.E("act", "activation", var[:], var[:], AF.Sqrt, R=[var], W=[var])
            yield
            P.E("dve", "reciprocal", var[:], var[:], R=[var], W=[var])
            P.E("dve", "tensor_tensor", yn[:], y0[:], mn[:], ALU.subtract, R=[y0, mn], W=[yn])
            P.E("dve", "tensor_tensor", yn[:], yn[:], var[:], ALU.mult, R=[yn, var], W=[yn])
            yield
            P.E("act", "activation", yn[:], yn[:], AF.Identity, scale=rc[:, hp, 3:4], bias=rc[:, hp, 4:5], R=[yn, rc], W=[yn])
            yield
            P.E("dve", "tensor_tensor", yn[:], yn[:], bn[:], ALU.add, R=[yn, bn], W=[yn])
            P.E("dve", "tensor_tensor", ot[:], yn[:], gt[:], ALU.mult, R=[yn, gt], W=[ot])
            P.DMA("sp", rwoT[sl_], ot[:], R=[ot])
            yield
        run_pipe(j_gen, 4 * nbj, NR)
        P.flush()
    if upto <= 9:
        P.flush(final=True); semctx.close(); G2.close(); G.close(); return nc

    x1d = dscr("x1d", [T, D], F32); h2p = dscr("h2p", [T, ROWP], BF16)
    affd = dscr("affd", [T, NE], F32); affTd = dscr("affTd", [NE, T], F32)
    P.E("pool", "iota", pidx[:], [[0, 1]], base=0, channel_multiplier=1, allow_small_or_imprecise_dtypes=True, W=[pidx])
    with contextlib.ExitStack() as ph:
        def sb(name, shape, dt):
            return ph.enter_context(nc.sbuf_tensor(name, list(shape), dt))

        def psum(name, shape, dt):
            P.excl.add(name)
            return ph.enter_context(nc.psum_tensor(name, list(shape), dt))
        hpj = sb("K_hpj", [128, 4, D], BF16); rpj = sb("K_rpj", [128, 4, D], BF16); wo = sb("K_wo", [128, 8, D], BF16)
        rtw = sb("K_rtw", [128, 8, NE], F32)
        for c in range(4):
            P.DMA("pool", hpj[:, c, :], hy_proj[c * 128:(c + 1) * 128, :], W=[hpj])
            P.DMA("pool", rpj[:, c, :], rw_proj[c * 128:(c + 1) * 128, :], W=[rpj])
        for k in range(8):
            P.DMA("pool", wo[:, k, :], w_out[k * 128:(k + 1) * 128, :], W=[wo])
        P.DMA("sp", rtw[:], router_w.rearrange("(k p) e -> p k e", p=128), W=[rtw])
        hyb = [sb("K_hy%d" % i, [128, 4, 512], BF16) for i in range(2)]
        rwb = [sb("K_rw%d" % i, [128, 4, 512], BF16) for i in range(2)]
        gtb = [sb("K_gt0", [128, 16, 512], BF16)] * 2
        m1 = sb("K_m1", [128, 512], F32); m2 = sb("K_m2", [128, 512], F32)
        mp = [sb("K_mp%d" % i, [128, 8, 512], BF16) for i in range(2)]
        xts = [sb("K_x%d" % i, [128, D], F32) for i in range(2)]
        t1r = [sb("K_t1%d" % i, [128, D], F32) for i in range(2)]; x1tr = [sb("K_x1t%d" % i, [128, D], F32) for i in range(2)]
        h2tr = [sb("K_h2t%d" % i, [128, D], F32) for i in range(2)]
        junkr = [sb("K_junk%d" % i, [128, D], BF16) for i in range(2)]
        h2pt = [sb("K_h2p%d" % i, [128, ROWP], BF16) for i in range(2)]
        ssr = [sb("K_ss%d" % i, [128, 8], F32) for i in range(2)]; smr = [sb("K_sm%d" % i, [128, 4], F32) for i in range(2)]
        h2Tr = [sb("K_h2T%d" % i, [128, 8, 128], F32) for i in range(2)]; e16r = [sb("K_e16%d" % i, [128, NE], F32) for i in range(2)]
        affTs = [sb("K_affT%d" % i, [NE, 128], F32) for i in range(2)]
        ps_h = psum("K_ph", [128, 512], F32); ps_r = psum("K_pr", [128, 512], F32)
        ps_o = psum("K_po", [128, 1024], F32); pTr = psum("K_pTr", [128, 8, 128], F32)
        ps_l = psum("K_pl", [128, 512], F32); psT = psum("K_pT", [128, 512], F32)
        def proj_gen(blk):
            b_ = blk % 2
            tsl = slice(blk * 512, (blk + 1) * 512)
            P.DMA("sp", hyb[b_][:], hyT[:, tsl].rearrange("(c p) t -> p c t", p=128), W=[hyb[b_]])
            P.DMA("sp", rwb[b_][:], rwoT[:, tsl].rearrange("(c p) t -> p c t", p=128), W=[rwb[b_]])
            P.DMA("sp", gtb[b_][:], colsT[3328:5376, tsl].rearrange("(c p) t -> p c t", p=128), W=[gtb[b_]])
            for dc in range(8):
                for c in range(4):
                    P.E("pe", "matmul", ps_h[:], hpj[:, c, dc * 128:(dc + 1) * 128], hyb[b_][:, c, :], start=(c == 0), stop=(c == 3),
                        R=[hpj, hyb[b_]], W=[ps_h])
                for c in range(4):
                    P.E("pe", "matmul", ps_r[:], rpj[:, c, dc * 128:(dc + 1) * 128], rwb[b_][:, c, :], start=(c == 0), stop=(c == 3),
                        R=[rpj, rwb[b_]], W=[ps_r])
                P.E("dve", "tensor_tensor", m1[:], ps_h[:], gtb[b_][:, dc, :], ALU.mult, R=[ps_h, gtb[b_]], W=[m1])
                P.E("dve", "tensor_tensor", m2[:], ps_r[:], gtb[b_][:, 8 + dc, :], ALU.mult, R=[ps_r, gtb[b_]], W=[m2])
                P.E("dve", "tensor_tensor", mp[b_][:, dc, :], m1[:], m2[:], ALU.add, R=[m1, m2], W=[mp[b_]])
                yield
        def tile_gen(tau):
            if True:
                blk = tau // 4; i = tau % 4; b_ = blk % 2; r_ = tau % 2
                t1 = t1r[r_]; x1t = x1tr[r_]; h2t = h2tr[r_]; junkb = junkr[r_]; ss = ssr[r_]; sm = smr[r_]; h2T = h2Tr[r_]; e16 = e16r[r_]
                xt = xts[tau % 2]; hp_ = h2pt[tau % 2]
                rsl = slice(tau * 128, (tau + 1) * 128)
                P.DMA("sp", xt[:], x_in[rsl, :], W=[xt])
                for half in range(2):
                    for k in range(8):
                        P.E("pe", "matmul", ps_o[:, half * 512:(half + 1) * 512], mp[b_][:, k, i * 128:(i + 1) * 128],
                            wo[:, k, half * 512:(half + 1) * 512], start=(k == 0), stop=(k == 7), R=[mp[b_], wo], W=[ps_o])
                P.E("act", "activation", junkb[:], ps_o[:], AF.Square, accum_out=ss[:, 0:1], R=[ps_o], W=[junkb, ss])
                P.E("dve", "tensor_scalar", ss[:, 1:2], ss[:, 0:1], 1.0 / D, EPS, ALU.mult, ALU.add, R=[ss], W=[ss])
                P.E("pool", "tensor_tensor", ss[:, 2:3], ss[:, 1:2], nhalf[:], ALU.pow, R=[ss, nhalf], W=[ss])
                P.E("dve", "scalar_tensor_tensor", t1[:], ps_o[:], ss[:, 2:3], rows["C"][:], ALU.mult, ALU.mult, R=[ps_o, ss, rows["C"]], W=[t1])
                yield
                P.E("dve", "tensor_tensor", x1t[:], t1[:], xt[:], ALU.add, R=[t1, xt], W=[x1t])
                P.DMA("sp", x1d[rsl, :], x1t[:], R=[x1t])
                yield
                P.E("act", "activation", junkb[:], x1t[:], AF.Square, accum_out=ss[:, 3:4], R=[x1t], W=[junkb, ss])
                P.E("dve", "tensor_scalar", ss[:, 4:5], ss[:, 3:4], 1.0 / D, EPS, ALU.mult, ALU.add, R=[ss], W=[ss])
                P.E("pool", "tensor_tensor", ss[:, 5:6], ss[:, 4:5], nhalf[:], ALU.pow, R=[ss, nhalf], W=[ss])
                P.E("dve", "scalar_tensor_tensor", t1[:], x1t[:], ss[:, 5:6], rows["D"][:], ALU.mult, ALU.mult, R=[x1t, ss, rows["D"]], W=[t1])
                P.E("dve", "tensor_tensor", h2t[:], t1[:], rows["E"][:], ALU.add, R=[t1, rows["E"]], W=[h2t])
                yield
                P.E("act", "activation", hp_[:, 0:D], h2t[:], AF.Copy, R=[h2t], W=[hp_])
                P.E("pool", "memset", hp_[:, D:D + 1], float(tau), W=[hp_])
                P.E("pool", "tensor_copy", hp_[:, D + 1:D + 2], pidx[:], R=[pidx], W=[hp_])
                P.DMA("sp", h2p[rsl, :], hp_[:], R=[hp_])
                for k in range(8):
                    P.E("pe", "transpose", pTr[:, k, :], h2t[:, k * 128:(k + 1) * 128], identf[:], R=[h2t, identf], W=[pTr])
                P.E("act", "activation", h2T[:].rearrange("p a b -> p (a b)"), pTr[:].rearrange("p a b -> p (a b)"), AF.Copy, R=[pTr], W=[h2T])
                yield
                for k in range(8):
                    P.E("pe", "matmul", ps_l[:, 0:NE], h2T[:, k, :], rtw[:, k, :], start=(k == 0), stop=(k == 7), R=[h2T, rtw], W=[ps_l])
                P.E("dve", "reduce_max", sm[:, 0:1], ps_l[:, 0:NE], AX.X, R=[ps_l], W=[sm])
                P.E("dve", "tensor_scalar", sm[:, 1:2], sm[:, 0:1], -1.0, None, ALU.mult, R=[sm], W=[sm])
                P.E("act", "activation", e16[:], ps_l[:, 0:NE], AF.Exp, bias=sm[:, 1:2], accum_out=sm[:, 2:3], R=[ps_l, sm], W=[e16, sm])
                P.E("dve", "reciprocal", sm[:, 3:4], sm[:, 2:3], R=[sm], W=[sm])
                P.E("dve", "tensor_scalar", AFF[:, tau, :], e16[:], sm[:, 3:4], None, ALU.mult, R=[e16, sm], W=[AFF])
                yield
                P.DMA("sp", affd[rsl, :], AFF[:, tau, :], R=[AFF])
                P.E("pe", "transpose", psT[0:NE, 0:128], AFF[:, tau, :], identf[:], R=[AFF, identf], W=[psT])
                P.E("act", "activation", affTs[tau % 2][:], psT[0:NE, 0:128], AF.Copy, R=[psT], W=[affTs[tau % 2]])
                P.DMA("sp", affTd[:, tau * 128:(tau + 1) * 128], affTs[tau % 2][:], R=[affTs[tau % 2]])
                yield
        nblk_ = T // 512
        for _ in proj_gen(0):
            pass
        jobs = []
        for blk in range(nblk_):
            if blk + 1 < nblk_:
                jobs.append(("p", blk + 1))
            for i in range(4):
                jobs.append(("t", blk * 4 + i))

        def kjob(n):
            kind, idx = jobs[n]
            return proj_gen(idx) if kind == "p" else tile_gen(idx)
        run_pipe(kjob, len(jobs), 2)
        P.flush()
    if upto <= 10:
        P.flush(final=True); semctx.close(); G2.close(); G.close(); return nc

    with contextlib.ExitStack() as ph:
        def sb(name, shape, dt):
            return ph.enter_context(nc.sbuf_tensor(name, list(shape), dt))

        def psum(name, shape, dt):
            P.excl.add(name)
            return ph.enter_context(nc.psum_tensor(name, list(shape), dt))
        A2 = sb("L_A2", [128, 1024], F32); cmpt = sb("L_cmp", [128, 1024], F32)
        blk8 = sb("L_blk8", [128, 128], F32)
        ri = sb("L_ri", [128, 1], I32); rf = sb("L_rf", [128, 1], F32)
        cix = sb("L_ci", [128, 128], I32); cf = sb("L_cf", [128, 128], F32)
        lo = sb("L_lo", [128, 1], F32); mid = sb("L_mid", [128, 1], F32); cntp = sb("L_cnt", [128, 1], F32); pred = sb("L_pred", [128, 1], F32)
        thrM = sb("L_thrM", [128, 128], F32)
        pc = psum("L_pc", [128, 512], F32)
        P.DMA("sp", A2[:], affTd.rearrange("e (g i) -> (e g) i", g=8), W=[A2])
        P.E("pool", "iota", ri[:], [[0, 1]], base=0, channel_multiplier=1, W=[ri])
        P.E("pool", "iota", cix[:], [[1, 128]], base=0, channel_multiplier=0, W=[cix])
        P.E("dve", "tensor_single_scalar", ri[:], ri[:], 3, ALU.arith_shift_right, R=[ri], W=[ri])
        P.E("dve", "tensor_single_scalar", cix[:], cix[:], 3, ALU.arith_shift_right, R=[cix], W=[cix])
        P.E("dve", "tensor_copy", rf[:], ri[:], R=[ri], W=[rf])
        P.E("dve", "tensor_copy", cf[:], cix[:], R=[cix], W=[cf])
        P.E("dve", "tensor_scalar", blk8[:], cf[:], rf[:, 0:1], None, ALU.is_equal, R=[cf, rf], W=[blk8])
        P.E("pool", "memset", lo[:], 0.0, W=[lo])
        for it_ in range(34):
            wk = 0.5 ** (it_ + 1)
            P.E("dve", "tensor_scalar", mid[:], lo[:], wk, None, ALU.add, R=[lo], W=[mid])
            P.E("dve", "tensor_scalar", cmpt[:], A2[:], mid[:, 0:1], None, ALU.is_ge, R=[A2, mid], W=[cmpt])
            P.E("dve", "reduce_sum", cntp[:], cmpt[:], AX.X, R=[cmpt], W=[cntp])
            P.E("pe", "matmul", pc[:, 0:1], blk8[:], cntp[:], start=True, stop=True, R=[blk8, cntp], W=[pc])
            P.E("dve", "tensor_single_scalar", pred[:], pc[:, 0:1], CAP - 0.5, ALU.is_ge, R=[pc], W=[pred])
            P.E("dve", "scalar_tensor_tensor", lo[:], pred[:], wk, lo[:], ALU.mult, ALU.add, R=[pred, lo], W=[lo])
        P.E("dve", "tensor_copy", thrM[:], bc(lo[:, 0:1], [[0, 128]]), R=[lo], W=[thrM])
        P.E("pe", "matmul", pc[:, 0:NE], thrM[:], bc(identf[:, 0:1], [[8, NE]]), start=True, stop=True, R=[thrM, identf], W=[pc])
        P.E("dve", "tensor_copy", thrB[:], pc[:, 0:NE], R=[pc], W=[thrB])
        if "dbg_thr" in taps:
            dbg_thr = dscr("dbg_thr", [128, NE], F32)
            P.DMA("sp", dbg_thr[:, :], thrB[:], R=[thrB])
        P.flush()
    if upto <= 11:
        P.flush(final=True); semctx.close(); G2.close(); G.close(); return nc

    with contextlib.ExitStack() as ph:
        def sb(name, shape, dt):
            return ph.enter_context(nc.sbuf_tensor(name, list(shape), dt))

        def psum(name, shape, dt):
            P.excl.add(name)
            return ph.enter_context(nc.psum_tensor(name, list(shape), dt))
        triu = sb("M_triu", [128, 128], BF16); onesb = sb("M_ones", [128, 128], BF16)
        onesf = sb("M_onesf", [128, 128], F32)
        Rrun = sb("M_Rrun", [128, NE], BF16)
        maskb = [sb("M_mask%d" % i, [128, NE], BF16) for i in range(2)]
        slotf = [sb("M_slotf%d" % i, [128, NE], F32) for i in range(2)]
        pcs = [psum("M_pc%d" % i, [128, 512], F32) for i in range(2)]
        P.E("pool", "memset", onesf[:], 1.0, W=[onesf])
        P.E("pool", "tensor_copy", onesb[:], onesf[:], R=[onesf], W=[onesb])
        P.E("pool", "affine_select", triu[:], onesf[:], [[1, 128]], ALU.is_ge, 0.0, base=0, channel_multiplier=-1, R=[onesf], W=[triu])
        P.E("pool", "memset", Rrun[:], 0.0, W=[Rrun])
        for tau in range(T // 128):
            b_ = tau % 2
            mk = maskb[b_]; sf = slotf[b_]; pc_ = pcs[b_]
            P.E("dve", "tensor_tensor", mk[:], AFF[:, tau, :], thrB[:], ALU.is_ge, R=[AFF, thrB], W=[mk])
            P.E("pe", "matmul", pc_[:, 0:NE], triu[:], mk[:], start=True, stop=False, R=[triu, mk], W=[pc_])
            P.E("pe", "matmul", pc_[:, 0:NE], onesb[:], Rrun[:], start=False, stop=True, R=[onesb, Rrun], W=[pc_])
            P.E("dve", "scalar_tensor_tensor", sf[:], mk[:], -4096.0, pc_[:, 0:NE], ALU.mult, ALU.add, R=[mk, pc_], W=[sf])
            P.E("dve", "tensor_scalar", sf[:], sf[:], 4095.0, None, ALU.add, R=[sf], W=[sf])
            P.E("dve", "tensor_copy", SLOT[:, tau, :], sf[:], R=[sf], W=[SLOT])
            P.E("dve", "tensor_tensor", Rrun[:], Rrun[:], mk[:], ALU.add, R=[Rrun, mk], W=[Rrun])
        P.flush()
    if upto <= 12:
        P.flush(final=True); semctx.close(); G2.close(); G.close(); return nc

    G2.close()
    with contextlib.ExitStack() as ph:
        def sb(name, shape, dt):
            return ph.enter_context(nc.sbuf_tensor(name, list(shape), dt))

        def psum(name, shape, dt):
            P.excl.add(name)
            return ph.enter_context(nc.psum_tensor(name, list(shape), dt))
        w1b = sb("N_w1", [128, 8, DE], BF16); w3b = sb("N_w3", [128, 8, DE], BF16); w2b = sb("N_w2", [128, 16, D], BF16)
        xeT = sb("N_xeT", [128, 8, CAP], BF16); hidT = sb("N_hidT", [128, 16, CAP], BF16)
        xts = [sb("N_xt%d" % i, [128, ROWP], BF16) for i in range(2)]
        s1s = [sb("N_s1%d" % i, [128, 512], BF16) for i in range(2)]
        yos = [sb("N_yo%d" % i, [128, D], BF16) for i in range(2)]
        stg = [sb("N_stg%d" % i, [128, 1024], F32) for i in range(3)]
        ps1 = [psum("N_p1%d" % i, [128, 512], F32) for i in range(2)]
        ps3 = [psum("N_p3%d" % i, [128, 512], F32) for i in range(2)]
        pso = [psum("N_po%d" % i, [128, 512], F32) for i in range(4)]
        lc = {"n": 0}
        dtl = [sb("N_dt%d" % i, [128, ROWP], BF16) for i in range(5)]
        tidfs = [sb("N_tidf%d" % i, [128, 8], F32) for i in range(2)]
        tidis = [sb("N_tidi%d" % i, [128, 8], I32) for i in range(2)]
        garrs = [sb("N_garr%d" % i, [128, 8, NE], F32) for i in range(2)]
        dcount = {"n": 0}
        dtoks = {}

        def load_cast(dst, src, wres):
            i = lc["n"]; lc["n"] += 1
            st = stg[i % 3]
            P.DMA("sp", st[:], src, W=[st])
            if i % 2 == 0:
                P.E("act", "activation", dst, st[:], AF.Copy, R=[st], W=[wres])
            else:
                P.E("dve", "tensor_copy", dst, st[:], R=[st], W=[wres])

        def w13_jobs(e):
            jobs = []
            for k in range(8):
                for h_ in range(2):
                    jobs.append((w1b[:, k, h_ * 1024:(h_ + 1) * 1024], exp_w1[e, k * 128:(k + 1) * 128, h_ * 1024:(h_ + 1) * 1024], w1b))
                    jobs.append((w3b[:, k, h_ * 1024:(h_ + 1) * 1024], exp_w3[e, k * 128:(k + 1) * 128, h_ * 1024:(h_ + 1) * 1024], w3b))
            return jobs

        def w2_jobs(e):
            return [(w2b[:, kk, :], exp_w2[e, kk * 128:(kk + 1) * 128, :], w2b) for kk in range(16)]

        def dispatch_one(e, tau):
            i = dcount["n"]; dcount["n"] += 1
            tl = dtl[i % 5]
            P.DMA("sp", tl[:], h2p[tau * 128:(tau + 1) * 128, :], W=[tl])
            tok = P.dma("pool", (lambda en, e=e, tau=tau, tl=tl: en.indirect_dma_start(
                out=xe[e][:, :], out_offset=bass.IndirectOffsetOnAxis(ap=SLOT[:, tau, e:e + 1], axis=0),
                in_=tl[:], in_offset=None, bounds_check=P.reg(en, CAP - 1), oob_is_err=False)), R=[SLOT.name, tl.name], W=[])
            d_ = dtoks.setdefault(e, {})
            d_[tok[0]] = max(d_.get(tok[0], 0), tok[1])

        def wait_dispatch(e):
            waits = [(sk, v) for sk, v in dtoks.get(e, {}).items() if P.known["sp"].get(sk, 0) < v]
            for sk, v in waits:
                P.known["sp"][sk] = v
            if waits:
                P.q["sp"].append((None, waits, None))

        def prologue_step(e, st_):
            par = e % 2
            tidf = tidfs[par]; tidi = tidis[par]; garr = garrs[par]
            xt = xts[st_ % 2]
            if st_ == 0:
                wait_dispatch(e)
            P.DMA("sp", xt[:], xe[e][st_ * 128:(st_ + 1) * 128, :], W=[xt])
            P.E("dve", "scalar_tensor_tensor", tidf[:, st_:st_ + 1], xt[:, D:D + 1], 128.0, xt[:, D + 1:D + 2], ALU.mult, ALU.add,
                R=[xt], W=[tidf])
            P.E("dve", "tensor_copy", tidi[:, st_:st_ + 1], tidf[:, st_:st_ + 1], R=[tidf], W=[tidi])
            P.dma("pool", (lambda en, st_=st_, garr=garr, tidi=tidi: en.indirect_dma_start(
                out=garr[:, st_, :], out_offset=None, in_=affd[:, :],
                in_offset=bass.IndirectOffsetOnAxis(ap=tidi[:, st_:st_ + 1], axis=0),
                bounds_check=P.reg(en, T - 1), oob_is_err=False)), R=[tidi.name], W=[garr.name])
            for half in range(2):
                pp = ps1[half]
                for k4 in range(4):
                    k = half * 4 + k4
                    P.E("pe", "matmul", pp[:, k4 * 128:(k4 + 1) * 128], xt[:, k * 128:(k + 1) * 128], identb[:], start=True, stop=True,
                        R=[xt, identb], W=[pp])
                dst = xeT[:, half * 4:(half + 1) * 4, st_ * 128:(st_ + 1) * 128]
                if half == 0:
                    P.E("act", "activation", dst, pp[:].rearrange("p (a b) -> p a b", b=128), AF.Copy, R=[pp], W=[xeT])
                else:
                    P.E("dve", "tensor_copy", dst, pp[:].rearrange("p (a b) -> p a b", b=128), R=[pp], W=[xeT])
        import os
        nexp = int(os.environ.get("NEXP", NE))
        for tau in range(T // 128):
            dispatch_one(0, tau)
        for j in w13_jobs(0):
            load_cast(*j)
        for st_ in range(8):
            prologue_step(0, st_)
        ev = 0
        for e in range(nexp):
            par = e % 2
            tidi = tidis[par]; garr = garrs[par]
            jobs2 = w2_jobs(e)
            djobs = [(e + 1, tau) for tau in range(T // 128)] if e + 1 < nexp else []
            for sh in range(2):
                ssl = slice(sh * 512, (sh + 1) * 512)
                for fc in range(16):
                    if jobs2:
                        load_cast(*jobs2.pop(0))
                    for _ in range(2):
                        if djobs:
                            dispatch_one(*djobs.pop(0))
                    p1 = ps1[ev % 2]; p3 = ps3[ev % 2]; s1 = s1s[ev % 2]; ev += 1
                    for k in range(8):
                        P.E("pe", "matmul", p1[:], w1b[:, k, fc * 128:(fc + 1) * 128], xeT[:, k, ssl], start=(k == 0), stop=(k == 7),
                            R=[w1b, xeT], W=[p1])
                    for k in range(8):
                        P.E("pe", "matmul", p3[:], w3b[:, k, fc * 128:(fc + 1) * 128], xeT[:, k, ssl], start=(k == 0), stop=(k == 7),
                            R=[w3b, xeT], W=[p3])
                    P.E("act", "activation", s1[:], p1[:], AF.Silu, R=[p1], W=[s1])
                    P.E("dve", "tensor_tensor", hidT[:, fc, ssl], s1[:], p3[:], ALU.mult, R=[s1, p3], W=[hidT])
            jobs13 = w13_jobs(e + 1) if e + 1 < nexp else []
            for st_ in range(8):
                yo = yos[st_ % 2]
                for dh in range(2):
                    for _ in range(2):
                        if jobs13:
                            load_cast(*jobs13.pop(0))
                    po = pso[(st_ % 2) * 2 + dh]
                    for fc in range(16):
                        P.E("pe", "matmul", po[:], hidT[:, fc, st_ * 128:(st_ + 1) * 128], w2b[:, fc, dh * 512:(dh + 1) * 512],
                            start=(fc == 0), stop=(fc == 15), R=[hidT, w2b], W=[po])
                    if dh == 0:
                        P.E("act", "activation", yo[:, 0:512], po[:], AF.Identity, scale=garr[:, st_, e:e + 1], R=[po, garr], W=[yo])
                    else:
                        P.E("dve", "tensor_scalar", yo[:, 512:1024], po[:], garr[:, st_, e:e + 1], None, ALU.mult, R=[po, garr], W=[yo])
                P.dma("pool", (lambda en, st_=st_, yo=yo, tidi=tidi: en.indirect_dma_start(
                    out=acc[:, :], out_offset=bass.IndirectOffsetOnAxis(ap=tidi[:, st_:st_ + 1], axis=0),
                    in_=yo[:], in_offset=None, bounds_check=P.reg(en, T - 1), oob_is_err=True, compute_op=ALU.add)),
                    R=[yo.name, tidi.name] + (["accE%d_%d" % (e - 1, j_) for j_ in range(8)] if e > 0 else []),
                    W=["accE%d_%d" % (e, st_)])
                if e + 1 < nexp:
                    prologue_step(e + 1, st_)
            for j in jobs13:
                load_cast(*j)
        P.flush()
    if upto <= 13:
        P.flush(final=True); semctx.close(); G2.close(); G.close(); return nc

    with contextlib.ExitStack() as ph:
        def sb(name, shape, dt):
            return ph.enter_context(nc.sbuf_tensor(name, list(shape), dt))
        NR = 4
        ats = [sb("O_a%d" % i, [128, D], F32) for i in range(NR)]
        xs_ = [sb("O_x%d" % i, [128, D], F32) for i in range(NR)]
        os_ = [sb("O_o%d" % i, [128, D], F32) for i in range(NR)]
        junkb = sb("O_junk", [128, D], BF16); sss = [sb("O_ss%d" % i, [128, 4], F32) for i in range(NR)]

        def o_gen(tau):
            b_ = tau % NR; rsl = slice(tau * 128, (tau + 1) * 128)
            at, xt, ot, ss = ats[b_], xs_[b_], os_[b_], sss[b_]
            P.DMA("sp", at[:], acc[rsl, :], W=[at]); P.DMA("act", xt[:], x1d[rsl, :], W=[xt])
            yield
            P.E("act", "activation", junkb[:], at[:], AF.Square, accum_out=ss[:, 0:1], R=[at], W=[junkb, ss])
            yield
            P.E("dve", "tensor_scalar", ss[:, 1:2], ss[:, 0:1], 1.0 / D, EPS, ALU.mult, ALU.add, R=[ss], W=[ss])
            yield
            P.E("pool", "tensor_tensor", ss[:, 2:3], ss[:, 1:2], nhalf[:], ALU.pow, R=[ss, nhalf], W=[ss])
            yield
            P.E("dve", "scalar_tensor_tensor", at[:], at[:], ss[:, 2:3], rows["F"][:], ALU.mult, ALU.mult, R=[at, ss, rows["F"]], W=[at])
            P.E("dve", "tensor_tensor", ot[:], at[:], xt[:], ALU.add, R=[at, xt], W=[ot])
            yield
            P.DMA("sp", y_out[rsl, :], ot[:], R=[ot])
            yield
        run_pipe(o_gen, T // 128, NR)
        P.flush()

    P.flush(final=True)
    semctx.close(); G.close()
    return nc


def prep_inputs(inp, consts):
    f = np.float32
    L0 = lambda k: np.ascontiguousarray(inp[k][0], dtype=f)
    sh = {}
    sh["mod_w"] = L0("mod_w"); sh["mod_b"] = L0("mod_b").reshape(1, -1)
    sh["norms"] = np.stack([L0("norm1_pre"), L0("norm1_post"), L0("norm2_pre"), L0("norm2_post")])
    sh["w_in"] = L0("w_in")
    cw = L0("hy_conv_w"); cb = L0("hy_conv_b")
    hc = np.zeros((128, 12, 4), f)
    for k in range(3):
        hc[:, :, k] = cw[k].reshape(12, 128).T
    hc[:, :, 3] = cb.reshape(12, 128).T
    sh["hcw"] = hc
    sh["hy_w1"] = L0("hy_ffn_w1"); sh["hy_w2"] = L0("hy_ffn_w2"); sh["hy_w3"] = L0("hy_ffn_w3")
    sh["hyp"] = np.stack([L0("hy_freq"), L0("hy_ffn_b1"), L0("hy_ffn_b2")], axis=1)
    sh["hy_biasc"] = np.ascontiguousarray(L0("hy_bias").reshape(4, 128).T)
    sh["hy_proj"] = L0("hy_proj"); sh["rw_proj"] = L0("rw_proj"); sh["w_out"] = L0("w_out")
    sh["router_w"] = L0("router_w")
    mu = L0("rw_mu")
    sh["mucol"] = np.ascontiguousarray(mu.reshape(2, 14, 128).transpose(2, 1, 0))
    w0 = L0("rw_w0"); a0 = L0("rw_a0")
    wa = np.zeros((128, 4, 4), f)
    for d in range(2):
        wa[:, :, d] = w0[d].reshape(4, 128).T
        wa[:, :, 2 + d] = a0[d].reshape(4, 128).T
    sh["rw_w0a0"] = wa
    up = np.zeros((128, 2, 512), f)
    up[0:64] = L0("rw_w_up").transpose(1, 0, 2)
    up[64:128] = L0("rw_a_up").transpose(1, 0, 2)
    sh["rw_up"] = up
    sh["rw_g_up"] = L0("rw_g_up")
    rc = np.zeros((128, 4, 5), f)
    for i, k in enumerate(["rw_k_k", "rw_k_a", "rw_r_k", "rw_ln_g", "rw_ln_b"]):
        rc[:, :, i] = L0(k).reshape(4, 128).T
    sh["rwc"] = rc
    sh["exp_w1"] = L0("exp_w1"); sh["exp_w3"] = L0("exp_w3"); sh["exp_w2"] = L0("exp_w2")
    sh.update(consts)
    maps = []
    for core in range(8):
        b = core // 2
        m = dict(sh)
        m["x"] = np.ascontiguousarray(inp["x"][b], dtype=f)
        m["ctx"] = np.ascontiguousarray(inp["ctx"][b], dtype=f)
        cv = np.zeros((128, 16), f)
        cv[:, 0:8] = np.asarray(inp["c"][b], f).reshape(8, 128).T
        cv[:, 8:16] = np.asarray(inp["c_ctx"], f).reshape(8, 128).T
        m["cvec"] = cv
        maps.append(m)
    return maps


_CACHE = {}


def kernel(**inputs):
    inp = {k: np.asarray(v) for k, v in inputs.items()}
    if "nc" not in _CACHE:
        _CACHE["nc"] = build()
        _CACHE["consts"] = host_consts()
    nc = _CACHE["nc"]
    maps = prep_inputs(inp, _CACHE["consts"])
    res = run_bass_kernel_spmd(nc, maps, core_ids=list(range(8)))
    out = np.stack([np.asarray(res.results[2 * b]["y_out"], dtype=np.float32) for b in range(4)], axis=0)
    return out
```
